# Optimizing a Trainium2 kernel written in Bass

```python
import math
import jax, jax.numpy as jnp
from jax import lax
import numpy as np

D_MODEL = 1024
BATCH = 8
SEQ = 8192
DEPTH = 2

HEAD_DIM = 64
ROPE_THETA = 500000.0
ROPE_DIM = HEAD_DIM // 4
NORM_EPS = 1e-6
Q_BLOCK = 128

A_PAIRS = ((128, 1), (512, 4), (2048, 16))
A_GROUPS = len(A_PAIRS)
A_HEADS = 4
A_OUT = A_HEADS * HEAD_DIM
B_HEADS = 4
B_OUT = B_HEADS * HEAD_DIM
IDX_HEADS = 8
IDX_DIM = 64
TOPK_MAX = 256
C_HEADS = 4
C_VDIM = 2 * HEAD_DIM
C_OUT = C_HEADS * C_VDIM
N_BRANCH = 3

A_IN = A_GROUPS * 3 * A_HEADS * HEAD_DIM
B_IN = 3 * B_HEADS * HEAD_DIM
IDX_IN = IDX_HEADS * IDX_DIM + IDX_DIM + IDX_HEADS
C_QK = C_HEADS * 2 * HEAD_DIM
C_IN = 2 * C_QK + C_OUT
GATE_IN = N_BRANCH * D_MODEL
IN_WIDTH = A_IN + B_IN + IDX_IN + C_IN + GATE_IN
IN_OFFSETS = [A_IN, A_IN + B_IN, A_IN + B_IN + IDX_IN, A_IN + B_IN + IDX_IN + C_IN]

D_FF = ((8 * D_MODEL // 3 + 127) // 128) * 128
CONV_WIDTH = 3

kernel_name = "hybrid_gated_dilated_dsa_diff_block"


def rms_norm(x, g):
    xf = x.astype(jnp.float32)
    y = xf * lax.rsqrt(jnp.mean(xf * xf, axis=-1, keepdims=True) + NORM_EPS)
    return (y * g.astype(jnp.float32)).astype(x.dtype)


def rope_tables(positions):
    inv = ROPE_THETA ** (-jnp.arange(0, ROPE_DIM, 2, dtype=jnp.float32) / ROPE_DIM)
    ang = positions.astype(jnp.float32)[..., None] * inv
    return jnp.cos(ang)[:, :, None, :], jnp.sin(ang)[:, :, None, :]


def apply_partial_rope(x, cos, sin):
    half = ROPE_DIM // 2
    x1 = x[..., :half].astype(jnp.float32)
    x2 = x[..., half:ROPE_DIM].astype(jnp.float32)
    rot = jnp.concatenate([x1 * cos - x2 * sin, x2 * cos + x1 * sin], axis=-1).astype(x.dtype)
    return jnp.concatenate([rot, x[..., ROPE_DIM:]], axis=-1)


def to_blocks(t):
    b, s = t.shape[:2]
    return jnp.moveaxis(t.reshape(b, s // Q_BLOCK, Q_BLOCK, *t.shape[2:]), 1, 0)


def from_blocks(t):
    t = jnp.moveaxis(t, 0, 1)
    return t.reshape(t.shape[0], t.shape[1] * t.shape[2], *t.shape[3:])


def banded_causal_attention(q, k, v, window):
    n, l, h, dh = q.shape
    nb = -(-l // Q_BLOCK)
    pad = nb * Q_BLOCK - l
    padf = lambda t: jnp.pad(t, ((0, 0), (0, pad), (0, 0), (0, 0))).reshape(n, nb, Q_BLOCK, h, dh)
    qb, kb, vb = padf(q), padf(k), padf(v)

    def with_prev(t):
        prev = jnp.pad(t[:, :-1], ((0, 0), (1, 0), (0, 0), (0, 0), (0, 0)))
        return jnp.concatenate([prev, t], axis=2)

    kk, vv = with_prev(kb), with_prev(vb)
    s = jnp.einsum('nbqhd,nbkhd->nbhqk', qb, kk).astype(jnp.float32) * (dh ** -0.5)
    qi = jnp.arange(Q_BLOCK)[:, None] + Q_BLOCK
    kj = jnp.arange(2 * Q_BLOCK)[None, :]
    dist = qi - kj
    band = (dist >= 0) & (dist <= window)
    inside = (jnp.arange(nb)[:, None] * Q_BLOCK - Q_BLOCK + kj) >= 0
    mask = band[None, :, :] & inside[:, None, :]
    s = jnp.where(mask[None, :, None], s, -jnp.inf)
    lse = jax.nn.logsumexp(s, axis=-1)
    p = jnp.exp(s - lse[..., None]).astype(v.dtype)
    o = jnp.einsum('nbhqk,nbkhd->nbqhd', p, vv).reshape(n, nb * Q_BLOCK, h, dh)[:, :l]
    lse = jnp.moveaxis(lse, 2, 3).reshape(n, nb * Q_BLOCK, h)[:, :l]
    return o, lse


def dilated_causal_attention(q, k, v, window, dilation):
    b, s, h, dh = q.shape
    m = s // dilation
    fold = lambda t: t.reshape(b, m, dilation, h, dh).transpose(0, 2, 1, 3, 4).reshape(b * dilation, m, h, dh)
    o, lse = banded_causal_attention(fold(q), fold(k), fold(v), window // dilation)
    o = o.reshape(b, dilation, m, h, dh).transpose(0, 2, 1, 3, 4).reshape(b, s, h, dh)
    lse = lse.reshape(b, dilation, m, h).transpose(0, 2, 1, 3).reshape(b, s, h)
    return o, lse


def mixer_dilated(qkv_a, cos, sin):
    b, s = qkv_a.shape[:2]
    outs, lses = [], []
    for g, (window, dilation) in enumerate(A_PAIRS):
        q = apply_partial_rope(qkv_a[:, :, g, 0], cos, sin)
        k = apply_partial_rope(qkv_a[:, :, g, 1], cos, sin)
        o, lse = dilated_causal_attention(q, k, qkv_a[:, :, g, 2], window, dilation)
        outs.append(o)
        lses.append(lse)
    alpha = jax.nn.softmax(jnp.stack(lses), axis=0).astype(qkv_a.dtype)
    o = jnp.einsum('gbsh,gbshd->bshd', alpha, jnp.stack(outs))
    return o.reshape(b, s, A_OUT)


def mixer_sparse(q, k, v, q_idx, k_idx, w_idx):
    b, s = q.shape[:2]
    topk = min(TOPK_MAX, s // 4)
    kpos = jnp.arange(s)
    starts = jnp.arange(s // Q_BLOCK) * Q_BLOCK

    def block(args):
        qb, qib, wb, start = args
        qpos = start + jnp.arange(Q_BLOCK)
        logits = jnp.einsum('bqhd,bsd->bqhs', qib, k_idx).astype(jnp.float32)
        score = jnp.einsum('bqhs,bqh->bqs', jax.nn.relu(logits), wb.astype(jnp.float32)) * (IDX_DIM ** -0.5)
        score = jnp.where(kpos[None, None, :] <= qpos[None, :, None], score, -jnp.inf)
        _, idx = lax.top_k(score, topk)
        k_sel = jax.vmap(lambda kk, ii: kk[ii])(k, idx)
        v_sel = jax.vmap(lambda vv, ii: vv[ii])(v, idx)
        att = jnp.einsum('bqhd,bqkhd->bhqk', qb, k_sel).astype(jnp.float32) * (HEAD_DIM ** -0.5)
        valid = idx <= qpos[None, :, None]
        att = jnp.where(valid[:, None], att, -jnp.inf)
        p = jax.nn.softmax(att, axis=-1).astype(v.dtype)
        return jnp.einsum('bhqk,bqkhd->bqhd', p, v_sel)

    o = lax.map(block, (to_blocks(q), to_blocks(q_idx), to_blocks(w_idx), starts))
    return from_blocks(o).reshape(b, s, B_OUT)


def mixer_diff(q, k, v, lam, lam_init, subln_g):
    b, s = q.shape[:2]
    kpos = jnp.arange(s)
    starts = jnp.arange(s // Q_BLOCK) * Q_BLOCK

    def block(args):
        qb, start = args
        qpos = start + jnp.arange(Q_BLOCK)
        sc = jnp.einsum('bqhcd,bkhcd->bhcqk', qb, k).astype(jnp.float32) * (HEAD_DIM ** -0.5)
        sc = jnp.where(kpos[None, :] <= qpos[:, None], sc, -jnp.inf)
        p = jax.nn.softmax(sc, axis=-1)
        a = (p[:, :, 0] - lam * p[:, :, 1]).astype(v.dtype)
        return jnp.einsum('bhqk,bkhe->bqhe', a, v)

    o = from_blocks(lax.map(block, (to_blocks(q), starts)))
    o = rms_norm(o, subln_g) * (1.0 - lam_init)
    return o.reshape(b, s, C_OUT)


def mixer_block(xn, cos, sin, w_in, w_br_a, w_br_b, w_br_c, w_out,
                lam_q1, lam_k1, lam_q2, lam_k2, lam_init, subln_g):
    b, s, _ = xn.shape
    proj = xn @ w_in
    pa, pb, pidx, pc, pg = jnp.split(proj, IN_OFFSETS, axis=-1)
    o_a = mixer_dilated(pa.reshape(b, s, A_GROUPS, 3, A_HEADS, HEAD_DIM), cos, sin)
    qkv_b = pb.reshape(b, s, 3, B_HEADS, HEAD_DIM)
    q_b = apply_partial_rope(qkv_b[:, :, 0], cos, sin)
    k_b = apply_partial_rope(qkv_b[:, :, 1], cos, sin)
    q_idx, k_idx, w_idx = jnp.split(pidx, [IDX_HEADS * IDX_DIM, IDX_HEADS * IDX_DIM + IDX_DIM], axis=-1)
    q_idx = apply_partial_rope(q_idx.reshape(b, s, IDX_HEADS, IDX_DIM), cos, sin)
    k_idx = apply_partial_rope(k_idx.reshape(b, s, 1, IDX_DIM), cos, sin)[:, :, 0]
    w_idx = w_idx * (IDX_HEADS ** -0.5)
    o_b = mixer_sparse(q_b, k_b, qkv_b[:, :, 2], q_idx, k_idx, w_idx)
    qc, kc, vc = jnp.split(pc, [C_QK, 2 * C_QK], axis=-1)
    qc = apply_partial_rope(qc.reshape(b, s, C_HEADS * 2, HEAD_DIM), cos, sin).reshape(b, s, C_HEADS, 2, HEAD_DIM)
    kc = apply_partial_rope(kc.reshape(b, s, C_HEADS * 2, HEAD_DIM), cos, sin).reshape(b, s, C_HEADS, 2, HEAD_DIM)
    vc = vc.reshape(b, s, C_HEADS, C_VDIM)
    f32 = jnp.float32
    lam = (jnp.exp(jnp.sum(lam_q1.astype(f32) * lam_k1.astype(f32)))
           - jnp.exp(jnp.sum(lam_q2.astype(f32) * lam_k2.astype(f32))) + lam_init)
    o_c = mixer_diff(qc, kc, vc, lam, lam_init, subln_g)
    gates = jax.nn.sigmoid(pg.reshape(b, s, N_BRANCH, D_MODEL))
    y = (gates[:, :, 0] * (o_a @ w_br_a) + gates[:, :, 1] * (o_b @ w_br_b)
         + gates[:, :, 2] * (o_c @ w_br_c))
    return y @ w_out


def conv_ffn(x, w_up, conv_w, conv_b, w_down):
    s = x.shape[1]
    h = x @ w_up
    hp = jnp.pad(h, ((0, 0), (CONV_WIDTH - 1, 0), (0, 0)))
    h = sum(hp[:, j:j + s] * conv_w[j] for j in range(CONV_WIDTH)) + conv_b
    g, u = jnp.split(h, 2, axis=-1)
    return (jax.nn.gelu(g, approximate=True) * u) @ w_down


def setup_inputs(seed: int = 0) -> dict:
    key = jax.random.key(seed)
    ks = jax.random.split(key, 24)
    f32 = jnp.float32
    dense = lambda k, shape, fan_in: jax.random.normal(k, shape, f32) * (fan_in ** -0.5)
    gain = lambda k, shape: 1.0 + 0.02 * jax.random.normal(k, shape, f32)
    return {
        "x": jax.random.normal(ks[0], (BATCH, SEQ, D_MODEL), f32),
        "positions": jnp.broadcast_to(jnp.arange(SEQ, dtype=jnp.int32), (BATCH, SEQ)),
        "w_in": dense(ks[1], (DEPTH, D_MODEL, IN_WIDTH), D_MODEL),
        "w_br_a": dense(ks[2], (DEPTH, A_OUT, D_MODEL), A_OUT),
        "w_br_b": dense(ks[3], (DEPTH, B_OUT, D_MODEL), B_OUT),
        "w_br_c": dense(ks[4], (DEPTH, C_OUT, D_MODEL), C_OUT),
        "w_out": dense(ks[5], (DEPTH, D_MODEL, D_MODEL), D_MODEL),
        "lam_q1": 0.1 * jax.random.normal(ks[6], (DEPTH, HEAD_DIM), f32),
        "lam_k1": 0.1 * jax.random.normal(ks[7], (DEPTH, HEAD_DIM), f32),
        "lam_q2": 0.1 * jax.random.normal(ks[8], (DEPTH, HEAD_DIM), f32),
        "lam_k2": 0.1 * jax.random.normal(ks[9], (DEPTH, HEAD_DIM), f32),
        "subln_g": gain(ks[10], (DEPTH, C_VDIM)),
        "norm_mix_pre": gain(ks[11], (DEPTH, D_MODEL)),
        "norm_mix_post": gain(ks[12], (DEPTH, D_MODEL)),
        "norm_ffn_pre": gain(ks[13], (DEPTH, D_MODEL)),
        "norm_ffn_post": gain(ks[14], (DEPTH, D_MODEL)),
        "w_ffn_up": dense(ks[15], (DEPTH, D_MODEL, 2 * D_FF), D_MODEL),
        "conv_w": dense(ks[16], (DEPTH, CONV_WIDTH, 2 * D_FF), CONV_WIDTH),
        "conv_b": 0.02 * jax.random.normal(ks[17], (DEPTH, 2 * D_FF), f32),
        "w_ffn_down": dense(ks[18], (DEPTH, D_FF, D_MODEL), D_FF),
    }


def reference(x, positions, w_in, w_br_a, w_br_b, w_br_c, w_out, lam_q1, lam_k1, lam_q2, lam_k2,
              subln_g, norm_mix_pre, norm_mix_post, norm_ffn_pre, norm_ffn_post,
              w_ffn_up, conv_w, conv_b, w_ffn_down):
    cos, sin = rope_tables(positions)
    for layer in range(DEPTH):
        lam_init = 0.8 - 0.6 * math.exp(-0.3 * layer)
        h = mixer_block(rms_norm(x, norm_mix_pre[layer]), cos, sin, w_in[layer], w_br_a[layer],
                        w_br_b[layer], w_br_c[layer], w_out[layer], lam_q1[layer], lam_k1[layer],
                        lam_q2[layer], lam_k2[layer], lam_init, subln_g[layer])
        x = x + rms_norm(h, norm_mix_post[layer])
        h = conv_ffn(rms_norm(x, norm_ffn_pre[layer]), w_ffn_up[layer], conv_w[layer],
                     conv_b[layer], w_ffn_down[layer])
        x = x + rms_norm(h, norm_ffn_post[layer])
    return x
```

```python
import math
import os as _os
from contextlib import ExitStack
import numpy as np
import concourse.bass as bass
import concourse.mybir as mybir
from concourse.bass_utils import run_bass_kernel_spmd

F32 = mybir.dt.float32
BF16 = mybir.dt.bfloat16
I32 = mybir.dt.int32
AF = mybir.ActivationFunctionType
ALU = mybir.AluOpType
AX = mybir.AxisListType

D = 1024
IN_W = 8264
DFF = 2816
NCORES = 8
DEPTH = 2
EPS = 1e-6
A_WB = (1, 4, 16)
A_PAIRS = ((128, 1), (512, 4), (2048, 16))
NSLOT = 17
NIT = 16
TOPK = 256
NMASK = 24
C_ID = 0
C_MASK = 128
C_NEG = C_MASK + NMASK * 128
C_POW = C_NEG + 128
C_INV = C_POW + NIT
NCST = C_INV + 8


def make_consts():
    c = np.zeros((128, NCST), np.float32)
    c[:, C_ID:C_ID + 128] = np.eye(128, dtype=np.float32)
    s = np.arange(128)[:, None]
    q = np.arange(128)[None, :]
    mi = 0
    for g, (w, d) in enumerate(A_PAIRS):
        for dl in range(A_WB[g] + 1):
            dist = 128 * dl + q - s
            m = (dist >= 0) & (dist <= w) & (dist % d == 0)
            c[:, C_MASK + mi * 128:C_MASK + (mi + 1) * 128] = m.astype(np.float32)
            mi += 1
    assert mi == NMASK
    c[:, C_NEG:C_NEG + 128] = np.where(q <= s, 0.0, -1e30).astype(np.float32)
    for k in range(NIT):
        c[:, C_POW + k] = 2.0 ** (-(k + 1))
    inv = 500000.0 ** (-np.arange(0, 16, 2, dtype=np.float32) / 16.0)
    c[:, C_INV:C_INV + 8] = inv[None, :].astype(np.float32)
    return c


def mask_index(g, dl):
    return sum(A_WB[i] + 1 for i in range(g)) + dl


class Buf:
    __slots__ = ("writers", "readers")

    def __init__(self):
        self.writers = []
        self.readers = []


def _compact(toks):
    best = {}
    for s, v in toks:
        if s.num not in best or best[s.num][1] < v:
            best[s.num] = (s, v)
    return list(best.values())


class Eng:
    def __init__(self, ctx, name, eng):
        self.ctx = ctx
        self.name = name
        self.eng = eng
        self.sem = None
        self.count = 0
        self.seen = {}
        self.own = set()

    def wait(self, tok):
        sem, val = tok
        if self.name == "pe" and sem.num in self.own:
            return
        if self.seen.get(sem.num, 0) >= val:
            return
        self.eng.wait_ge(sem, val)
        self.seen[sem.num] = val

    def last(self):
        return (self.sem, self.count) if self.sem is not None and self.count > 0 else None


class Ctx:
    SEM_ROLL = 32000

    def __init__(self, nc, semstack):
        self.nc = nc
        self.semstack = semstack
        self.stack = None
        self.nsem = 0
        self.pe = Eng(self, "pe", nc.tensor)
        self.act = Eng(self, "act", nc.scalar)
        self.dve = Eng(self, "dve", nc.vector)
        self.pool = Eng(self, "pool", nc.gpsimd)
        self.sp = Eng(self, "sp", nc.sync)
        self.engs = [self.pe, self.act, self.dve, self.pool, self.sp]
        self.dma_pool = {}
        self.old_sems = []
        self.nuniq = 0

    def alloc_sem(self, name):
        self.nsem += 1
        return self.semstack.enter_context(self.nc.semaphore(f"{name}_{self.nsem}"))

    def sb(self, name, shape, dtype):
        self.nuniq += 1
        return self.stack.enter_context(self.nc.sbuf_tensor(f"{name}_{self.nuniq}", list(shape), dtype))

    def ps(self, name, shape, dtype):
        self.nuniq += 1
        return self.stack.enter_context(self.nc.psum_tensor(f"{name}_{self.nuniq}", list(shape), dtype))

    def _pre(self, E, reads, writes, adds, upd):
        for b in reads:
            for t in b.writers:
                E.wait(t)
        for b in writes:
            for t in b.writers:
                E.wait(t)
            for t in b.readers:
                E.wait(t)
        for b in upd:
            for t in b.writers:
                E.wait(t)
            for t in b.readers:
                E.wait(t)
        for b in adds:
            for t in b.readers:
                E.wait(t)

    def _post(self, tok, reads, writes, adds, upd):
        for b in reads:
            b.readers.append(tok)
            if len(b.readers) > 16:
                b.readers = _compact(b.readers)
        for b in writes:
            b.writers = [tok]
            b.readers = []
        for b in upd:
            b.writers.append(tok)
            b.readers = []
            if len(b.writers) > 16:
                b.writers = _compact(b.writers)
        for b in adds:
            b.writers.append(tok)
            b.readers = []
            if len(b.writers) > 16:
                b.writers = _compact(b.writers)

    def op(self, E, fn, reads=(), writes=(), adds=(), upd=()):
        self._pre(E, reads, writes, adds, upd)
        if E.sem is None or E.count >= self.SEM_ROLL:
            if E.sem is not None:
                self.old_sems.append((E.sem, E.count))
            E.sem = self.alloc_sem(E.name)
            E.own.add(E.sem.num)
            E.count = 0
        ins = fn(E.eng)
        E.count += 1
        ins.then_inc(E.sem, 1)
        tok = (E.sem, E.count)
        self._post(tok, reads, writes, adds, upd)
        return tok

    def dma(self, E, out, in_, reads=(), writes=(), adds=(), upd=(), nsem=16, **kw):
        key = E.name
        if key not in self.dma_pool:
            self.dma_pool[key] = {"sems": [], "vals": [], "next": 0, "toks": []}
        P = self.dma_pool[key]
        self._pre(E, reads, writes, adds, upd)
        i = P["next"]
        if i >= len(P["sems"]):
            P["sems"].append(self.alloc_sem("dma" + key))
            P["vals"].append(0)
            P["toks"].append(None)
        else:
            E.wait(P["toks"][i])
        P["next"] = (i + 1) % nsem
        sem = P["sems"][i]
        P["vals"][i] += 16
        ins = E.eng.dma_start(out=out, in_=in_, **kw)
        ins.then_inc(sem, 16)
        tok = (sem, P["vals"][i])
        P["toks"][i] = tok
        self._post(tok, reads, writes, adds, upd)
        return tok

    def barrier(self):
        toks = []
        for E in self.engs:
            t = E.last()
            if t is not None:
                toks.append(t)
        for P in self.dma_pool.values():
            for t in P["toks"]:
                if t is not None:
                    toks.append(t)
        for E in self.engs:
            for t in toks:
                E.wait(t)


class Ring:
    def __init__(self, c, name, n, shape, dtype, psum=False):
        self.t = [(c.ps if psum else c.sb)(f"{name}{i}", shape, dtype) for i in range(n)]
        self.b = [Buf() for _ in range(n)]
        self.i = 0
        self.n = n

    def next(self):
        t, b = self.t[self.i], self.b[self.i]
        self.i = (self.i + 1) % self.n
        return t, b


def build(S, depth=DEPTH, dbg=False, phases="1AbBCMF"):
    NT = S // 128
    nc = bass.Bass("TRN2", target_bir_lowering=False)

    def din(name, shape, dt=F32):
        return nc.dram_tensor(name, list(shape), dt, kind="ExternalInput").ap()

    x_in = din("x", [S, D])
    pos_in = din("pos", [NT, 128], I32)
    w_in = din("w_in", [DEPTH, D, IN_W])
    w_br_a = din("w_br_a", [DEPTH, 256, D])
    w_br_b = din("w_br_b", [DEPTH, 256, D])
    w_br_c = din("w_br_c", [DEPTH, 512, D])
    w_out = din("w_out", [DEPTH, D, D])
    lam_in = din("lam", [DEPTH, 4, 64])
    subln = din("subln_g", [DEPTH, 128])
    norms = din("norms", [DEPTH, 32, 128])
    w_up = din("w_up", [DEPTH, D, 2 * DFF])
    conv_w = din("conv_w", [DEPTH, 132, 128])
    conv_b = din("conv_b", [DEPTH, 44, 128])
    w_down = din("w_down", [DEPTH, DFF, D])
    cst_in = din("cst", [128, NCST])
    y_out = nc.dram_tensor("y", [S, D], F32, kind="ExternalOutput").ap()

    def scr(name, shape, dt):
        return nc.dram_tensor(name, list(shape), dt).ap()

    QT = scr("QT", [58 * 64, S], BF16)
    VA_A = scr("VA_A", [S, 12 * 65], BF16)
    VA_B = scr("VA_B", [S, 4 * 65], BF16)
    VA_C = scr("VA_C", [S, 4 * 129], BF16)
    W8 = scr("W8", [S, 8], F32)
    GT = scr("GT", [3072, S], BF16)
    OT = scr("OT", [1024, S], BF16)
    MT = scr("MT", [NT, 128, NT, 128], BF16)
    X1 = scr("X1", [S, D], F32)
    X2 = scr("X2", [S, D], F32)
    QT3 = QT.rearrange("(n d) t -> d n t", d=64)

    with ExitStack() as semstack, ExitStack() as gstack:
        c = Ctx(nc, semstack)
        c.stack = gstack
        pe, act, dve, pool, sp = c.pe, c.act, c.dve, c.pool, c.sp
        pl = pool if _os.environ.get("K_POOL", "1") == "1" else dve

        cstf = c.sb("cstf", [128, NCST], F32)
        Bc = Buf()
        ident_b = c.sb("ident_b", [128, 128], BF16)
        maskA = c.sb("maskA", [128, NMASK, 128], BF16)
        cosT = c.sb("cosT", [128, NT, 8], F32)
        sinT = c.sb("sinT", [128, NT, 8], F32)
        pib = c.sb("pib", [128, 1], F32)
        Bg = Buf()
        ident_f = cstf[:, C_ID:C_ID + 128]
        negtri = cstf[:, C_NEG:C_NEG + 128]

        c.dma(sp, cstf[:], cst_in, writes=[Bc])
        c.op(dve, lambda e: e.tensor_copy(ident_b[:], cstf[:, C_ID:C_ID + 128]), reads=[Bc], adds=[Bg])
        c.op(dve, lambda e: e.tensor_copy(maskA[:].rearrange("p m q -> p (m q)"), cstf[:, C_MASK:C_MASK + NMASK * 128]),
             reads=[Bc], adds=[Bg])
        c.op(dve, lambda e: e.memset(pib[:], math.pi), adds=[Bg])

        def load_cols(dst, src_rows_ap, n, stack_tag):
            with ExitStack() as ls:
                old = c.stack
                c.stack = ls
                tmp = c.sb("lc_tmp", [128, 128], F32)
                pt = c.ps("lc_ps", [128, 128], F32)
                bt, bp = Buf(), Buf()
                c.dma(sp, tmp[0:n, :], src_rows_ap, writes=[bt])
                c.op(pe, lambda e: e.transpose(pt[:, 0:n], tmp[0:n, :], cstf[0:n, C_ID:C_ID + n]),
                     reads=[bt, Bc], writes=[bp])
                c.op(dve, lambda e: e.tensor_copy(dst, pt[:, 0:n]), reads=[bp], adds=[Bg])
                c.barrier()
                c.stack = old

        with ExitStack() as ls:
            c.stack = ls
            posi = c.sb("posi", [128, 128], I32)
            posf = c.sb("posf", [128, 128], F32)
            pt = c.ps("pos_ps", [128, 128], F32)
            posT = c.sb("posT", [128, NT], F32)
            ang = c.sb("ang", [128, NT, 8], F32)
            ang2 = c.sb("ang2", [128, NT, 8], F32)
            b1, b2, b3, b4, b5, b6 = (Buf() for _ in range(6))
            c.dma(sp, posi[0:NT, :], pos_in, writes=[b1])
            c.op(dve, lambda e: e.tensor_copy(posf[0:NT, :], posi[0:NT, :]), reads=[b1], writes=[b2])
            c.op(pe, lambda e: e.transpose(pt[:, 0:NT], posf[0:NT, :], cstf[0:NT, C_ID:C_ID + NT]),
                 reads=[b2, Bc], writes=[b3])
            c.op(dve, lambda e: e.tensor_copy(posT[:], pt[:, 0:NT]), reads=[b3], writes=[b4])
            for i in range(8):
                c.op(dve, lambda e: e.tensor_scalar(ang[:, :, i], posT[:], cstf[:, C_INV + i:C_INV + i + 1], None,
                                                    ALU.mult), reads=[b4, Bc], adds=[b5])
            TWO_PI = 2.0 * math.pi
            ki = c.sb("ki", [128, NT, 8], I32)
            kf = c.sb("kf", [128, NT, 8], F32)
            rr_ = c.sb("rr_", [128, NT, 8], F32)
            tt_ = c.sb("tt_", [128, NT, 8], F32)
            b7, b8, b9, b10 = (Buf() for _ in range(4))
            for (dst, shift) in ((sinT, 0.0), (cosT, math.pi / 2)):
                c.op(dve, lambda e: e.tensor_scalar(ang2[:], ang[:], shift, None, ALU.add), reads=[b5], writes=[b6])
                c.op(dve, lambda e: e.tensor_scalar(kf[:], ang2[:], 1.0 / TWO_PI, None, ALU.mult), reads=[b6], writes=[b8])
                c.op(dve, lambda e: e.tensor_copy(ki[:], kf[:]), reads=[b8], writes=[b7])
                c.op(dve, lambda e: e.tensor_copy(kf[:], ki[:]), reads=[b7], writes=[b8])
                c.op(dve, lambda e: e.scalar_tensor_tensor(rr_[:], kf[:], -TWO_PI, ang2[:], ALU.mult, ALU.add),
                     reads=[b8, b6], writes=[b9])
                c.op(dve, lambda e: e.tensor_scalar(tt_[:], rr_[:], math.pi, TWO_PI, ALU.is_gt, ALU.mult),
                     reads=[b9], writes=[b10])
                c.op(dve, lambda e: e.tensor_tensor(rr_[:], rr_[:], tt_[:], ALU.subtract), reads=[b10], upd=[b9])
                c.op(dve, lambda e: e.tensor_scalar(tt_[:], rr_[:], -math.pi, TWO_PI, ALU.is_lt, ALU.mult),
                     reads=[b9], upd=[b10])
                c.op(dve, lambda e: e.tensor_tensor(rr_[:], rr_[:], tt_[:], ALU.add), reads=[b10], upd=[b9])
                c.op(act, lambda e: e.activation(dst[:], rr_[:], AF.Sin), reads=[b9], adds=[Bg])
            c.barrier()
        c.stack = gstack

        gcols = c.sb("gcols", [128, DEPTH, 32], F32)
        cw = c.sb("cw", [128, DEPTH, 132], F32)
        cb = c.sb("cb", [128, DEPTH, 44], F32)
        lamt = c.sb("lamt", [128, DEPTH, 4], F32)
        for l in range(depth):
            load_cols(gcols[:, l, :], norms[l], 32, "g")
            load_cols(cw[:, l, 0:128], conv_w[l, 0:128, :], 128, "cw")
            load_cols(cw[:, l, 128:132], conv_w[l, 128:132, :], 4, "cw2")
            load_cols(cb[:, l, :], conv_b[l], 44, "cb")
        with ExitStack() as ls:
            c.stack = ls
            lt = c.sb("lt", [128, DEPTH, 4, 64], F32)
            pr = c.sb("pr", [128, DEPTH, 2, 64], F32)
            sm = c.sb("sm", [128, DEPTH, 2], F32)
            ex = c.sb("ex", [128, DEPTH, 2], F32)
            b1, b2, b3, b4 = (Buf() for _ in range(4))
            for l in range(DEPTH):
                c.dma(sp, lt[:, l, :, :].rearrange("p a b -> p (a b)"),
                      lam_in[l].rearrange("a b -> (a b)").partition_broadcast(128), adds=[b1])
            for l in range(DEPTH):
                for j in range(2):
                    c.op(dve, lambda e: e.tensor_tensor(pr[:, l, j, :], lt[:, l, 2 * j, :], lt[:, l, 2 * j + 1, :],
                                                        ALU.mult), reads=[b1], adds=[b2])
            c.op(dve, lambda e: e.tensor_reduce(sm[:].rearrange("p l j -> p (l j)"),
                                                pr[:].rearrange("p l j d -> p (l j) d"), AX.X, ALU.add),
                 reads=[b2], writes=[b3])
            c.op(act, lambda e: e.activation(ex[:], sm[:], AF.Exp), reads=[b3], writes=[b4])
            for l in range(DEPTH):
                lam_init = 0.8 - 0.6 * math.exp(-0.3 * l)
                c.op(dve, lambda e: e.tensor_tensor(lamt[:, l, 0:1], ex[:, l, 0:1], ex[:, l, 1:2], ALU.subtract),
                     reads=[b4], adds=[Bg])
                c.op(dve, lambda e: e.tensor_scalar(lamt[:, l, 0:1], lamt[:, l, 0:1], lam_init, None, ALU.add),
                     upd=[Bg])
                c.op(dve, lambda e: e.tensor_scalar(lamt[:, l, 1:2], lamt[:, l, 0:1], -1.0, None, ALU.mult),
                     upd=[Bg])
            c.barrier()
        c.stack = gstack
        c.barrier()

        def norm_transpose(st, xsrc_rows, xnT_dst, bdst, first_write):
            xt, bx = st["xr"].next()
            c.dma(sp, xt[:], xsrc_rows, writes=[bx])
            ss, bs = st["ss"].next()
            c.op(dve, lambda e: e.memset(ss[:], 0.0), writes=[bs])
            jk, bj = st["junk"].next()
            c.op(act, lambda e: e.activation(jk[:], xt[:], AF.Square, accum_out=ss[:, 0:1]),
                 reads=[bx], writes=[bj], upd=[bs])
            c.op(dve, lambda e: e.tensor_scalar(ss[:, 1:2], ss[:, 0:1], 1.0 / D, EPS, ALU.mult, ALU.add), upd=[bs])
            c.op(act, lambda e: e.activation(ss[:, 2:3], ss[:, 1:2], AF.Sqrt), upd=[bs])
            c.op(dve, lambda e: e.reciprocal(ss[:, 2:3], ss[:, 2:3]), upd=[bs])
            xb, bxb = st["xb"].next()
            c.op(dve, lambda e: e.tensor_scalar(xb[:], xt[:], ss[:, 2:3], None, ALU.mult),
                 reads=[bx, bs], writes=[bxb])
            pt, bpt = st["pT"].next()
            for kc in range(8):
                c.op(pe, lambda e: e.transpose(pt[:, kc, :], xb[:, kc * 128:(kc + 1) * 128], ident_b[:]),
                     reads=[bxb, Bg], **({"writes": [bpt]} if kc == 0 else {"adds": [bpt]}))
            c.op(act, lambda e: e.activation(xnT_dst, pt[:], AF.Copy), reads=[bpt], writes=[bdst])
            return xt, bx

        def load_w(st, dst_bf, bdst, src_ap, nk, ncols, gain_ap=None):
            cap = st["wst"].t[0].shape[1]
            for k0 in range(0, nk, cap):
                kn = min(cap, nk - k0)
                ws, bws = st["wst"].next()
                c.dma(sp, ws[:, 0:kn, 0:ncols], src_ap[:, k0:k0 + kn, :], writes=[bws])
                for kk in range(kn):
                    kc = k0 + kk
                    if gain_ap is not None:
                        c.op(pl, lambda e: e.tensor_scalar(dst_bf[:, kc, 0:ncols], ws[:, kk, 0:ncols],
                                                             gain_ap[:, kc:kc + 1], None, ALU.mult),
                             reads=[bws, Bg], **({"writes": [bdst]} if kc == 0 else {"adds": [bdst]}))
                    else:
                        c.op(pl, lambda e: e.tensor_copy(dst_bf[:, kc, 0:ncols], ws[:, kk, 0:ncols]),
                             reads=[bws], **({"writes": [bdst]} if kc == 0 else {"adds": [bdst]}))

        def phase1(l, xin):
            TCH = min(S, 2048)
            NCH = S // TCH
            NTT = TCH // 128
            with ExitStack() as ps_:
                c.stack = ps_
                st = {
                    "xr": Ring(c, "xr", 2, [128, D], F32),
                    "ss": Ring(c, "ss", 2, [128, 4], F32),
                    "junk": Ring(c, "junk", 1, [128, D], BF16),
                    "xb": Ring(c, "xb", 2, [128, D], BF16),
                    "pT": Ring(c, "pT", 2, [128, 8, 128], BF16, psum=True),
                    "wst": Ring(c, "wst", 2, [128, 8, 512], F32),
                }
                wbr = Ring(c, "wb", 2, [128, 8, 512], BF16)
                pM = Ring(c, "pM", 2, [128, 512], F32, psum=True)
                pQ = Ring(c, "pQ", 2, [128, 4, 128], BF16, psum=True)
                xnT = c.sb("xnT", [128, 8, TCH], BF16)
                bxn = [Buf() for _ in range(NTT)]
                qbr = Ring(c, "qb", 2, [128, 512], BF16)
                tmps = [Ring(c, f"rt{i}", 2, [128, 8, 8], F32) for i in range(4)]
                rpr = Ring(c, "rp", 2, [128, 8, 16], F32)
                cos8 = c.sb("cos8", [128, NT, 8, 8], F32)
                sin8 = c.sb("sin8", [128, NT, 8, 8], F32)
                b88 = Buf()
                for (d8, src8) in ((cos8, cosT), (sin8, sinT)):
                    for hh_ in range(8):
                        c.op(dve, lambda e: e.tensor_copy(d8[:, :, hh_, :], src8[:]), reads=[Bg], adds=[b88])
                qsr = Ring(c, "qs", 3, [128, 4, 128], BF16)
                vsa = Ring(c, "vsa", 3, [128, 4, 65], BF16)
                vsc = Ring(c, "vsc", 3, [128, 4, 129], BF16)
                gsr = Ring(c, "gs", 2, [128, 4, 512], BF16)
                w8r = Ring(c, "w8", 3, [128, 8], F32)
                for r in (vsa, vsc):
                    for t, b in zip(r.t, r.b):
                        c.op(dve, lambda e: e.memset(t[:], 1.0), writes=[b])
                w_l = w_in[l].rearrange("(kc p) n -> p kc n", p=128)
                gain = gcols[:, l, 0:8]

                tiles = []
                for g in range(3):
                    tiles.append(("qk", g * 768, 8, g * 8))
                    tiles.append(("v", g * 768 + 512, 256, VA_A, g * 4 * 65, 64))
                tiles.append(("qk", 2304, 8, 24))
                tiles.append(("v", 2816, 256, VA_B, 0, 64))
                tiles.append(("qk", 3072, 8, 32))
                tiles.append(("kw", 3584, 72))
                tiles.append(("qk", 3656, 8, 41))
                tiles.append(("qk", 4168, 8, 49))
                tiles.append(("v", 4680, 512, VA_C, 0, 128))
                for i in range(6):
                    tiles.append(("gate", 5192 + i * 512, 512, i * 512))

                def rope_tile(pm, bpm, qbt, bqb, nh, tglob):
                    pmv = pm[:, 0:nh * 64].rearrange("p (h d) -> p h d", d=64)
                    qbv = qbt[:, 0:nh * 64].rearrange("p (h d) -> p h d", d=64)
                    c.op(act, lambda e: e.activation(qbv[:, :, 16:64], pmv[:, :, 16:64], AF.Copy),
                         reads=[bpm], writes=[bqb])
                    if _os.environ.get("K_NOROPE"):
                        c.op(act, lambda e: e.activation(qbv[:, :, 0:16], pmv[:, :, 0:16], AF.Copy), reads=[bpm], adds=[bqb])
                        return
                    cosb = cos8[:, tglob, 0:nh, :]
                    sinb = sin8[:, tglob, 0:nh, :]
                    (t1, bt1), (t2, bt2), (t3, bt3), (t4, bt4) = [r.next() for r in tmps]
                    rp, brp = rpr.next()
                    c.op(act, lambda e: e.activation(rp[:, 0:nh, :], pmv[:, :, 0:16], AF.Copy), reads=[bpm], writes=[brp])
                    x1 = rp[:, 0:nh, 0:8]
                    x2 = rp[:, 0:nh, 8:16]
                    c.op(dve, lambda e: e.tensor_tensor(t1[:, 0:nh, :], x1, cosb, ALU.mult), reads=[brp, b88], writes=[bt1])
                    c.op(dve, lambda e: e.tensor_tensor(t2[:, 0:nh, :], x2, sinb, ALU.mult), reads=[brp, b88], writes=[bt2])
                    c.op(dve, lambda e: e.tensor_tensor(qbv[:, :, 0:8], t1[:, 0:nh, :], t2[:, 0:nh, :], ALU.subtract),
                         reads=[bt1, bt2], adds=[bqb])
                    c.op(dve, lambda e: e.tensor_tensor(t3[:, 0:nh, :], x2, cosb, ALU.mult), reads=[brp, b88], writes=[bt3])
                    c.op(dve, lambda e: e.tensor_tensor(t4[:, 0:nh, :], x1, sinb, ALU.mult), reads=[brp, b88], writes=[bt4])
                    c.op(dve, lambda e: e.tensor_tensor(qbv[:, :, 8:16], t3[:, 0:nh, :], t4[:, 0:nh, :], ALU.add),
                         reads=[bt3, bt4], adds=[bqb])

                for ch in range(NCH):
                    for tt in range(NTT):
                        t0 = ch * TCH + tt * 128
                        norm_transpose(st, xin[t0:t0 + 128, :], xnT[:, :, tt * 128:(tt + 1) * 128], bxn[tt], True)
                    import os as _os
                    _kinds = _os.environ.get("K_KINDS", "qk,v,kw,gate").split(",")
                    for tl in tiles:
                        kind, col0 = tl[0], tl[1]
                        if kind not in _kinds:
                            continue
                        ncols = 512 if kind in ("qk", "gate") else tl[2]
                        wb, bwb = wbr.next()
                        load_w(st, wb, bwb, w_l[:, :, col0:col0 + ncols], 8, ncols, gain)
                        if kind == "gate":
                            row0 = tl[3]
                            for tg in range(TCH // 512):
                                gs, bgs = gsr.next()
                                for cbk in range(4):
                                    pm, bpm = pM.next()
                                    for kc in range(8):
                                        c.op(pe, lambda e: e.matmul(pm[:], wb[:, kc, cbk * 128:(cbk + 1) * 128],
                                                                    xnT[:, kc, tg * 512:(tg + 1) * 512],
                                                                    start=(kc == 0), stop=(kc == 7)),
                                             reads=[bwb] + bxn[tg * 4:(tg + 1) * 4],
                                             **({"writes": [bpm]} if kc == 0 else {"adds": [bpm]}))
                                    c.op(act, lambda e: e.activation(gs[:, cbk, :], pm[:], AF.Sigmoid), reads=[bpm],
                                         **({"writes": [bgs]} if cbk == 0 else {"adds": [bgs]}))
                                tg0 = ch * TCH + tg * 512
                                c.dma(pool, GT[row0:row0 + 512, tg0:tg0 + 512].rearrange("(b p) t -> p b t", p=128),
                                      gs[:], reads=[bgs])
                            continue
                        for tt in range(NTT):
                            t0 = ch * TCH + tt * 128
                            tglob = t0 // 128
                            pm, bpm = pM.next()
                            for kc in range(8):
                                c.op(pe, lambda e: e.matmul(pm[:, 0:ncols], xnT[:, kc, tt * 128:(tt + 1) * 128],
                                                            wb[:, kc, 0:ncols], start=(kc == 0), stop=(kc == 7)),
                                     reads=[bwb, bxn[tt]], **({"writes": [bpm]} if kc == 0 else {"adds": [bpm]}))
                            if kind == "v":
                                dst, coff, hd = tl[3], tl[4], tl[5]
                                vs, bvs = (vsa if hd == 64 else vsc).next()
                                c.op(act, lambda e: e.activation(vs[:, :, 0:hd],
                                                                 pm[:, 0:ncols].rearrange("p (h d) -> p h d", d=hd),
                                                                 AF.Copy), reads=[bpm], writes=[bvs])
                                c.dma(pool, dst[t0:t0 + 128, coff:coff + 4 * (hd + 1)],
                                      vs[:].rearrange("p h d -> p (h d)"), reads=[bvs])
                                continue
                            nh = 8 if kind == "qk" else 1
                            qbt, bqb = qbr.next()
                            rope_tile(pm, bpm, qbt, bqb, nh, tglob)
                            pq, bpq = pQ.next()
                            qs, bqs = qsr.next()
                            if kind == "qk":
                                head0 = tl[3]
                                for blk in range(4):
                                    c.op(pe, lambda e: e.transpose(pq[:, blk, :], qbt[:, blk * 128:(blk + 1) * 128],
                                                                   ident_b[:]),
                                         reads=[bqb, Bg], **({"writes": [bpq]} if blk == 0 else {"adds": [bpq]}))
                                c.op(dve, lambda e: e.tensor_copy(qs[:], pq[:]), reads=[bpq], writes=[bqs])
                                c.dma(pool, QT[head0 * 64:head0 * 64 + 512, t0:t0 + 128].rearrange("(b p) t -> p b t", p=128),
                                      qs[:], reads=[bqs])
                            else:
                                c.op(pe, lambda e: e.transpose(pq[0:64, 0, :], qbt[:, 0:64], ident_b[:]),
                                     reads=[bqb, Bg], writes=[bpq])
                                c.op(dve, lambda e: e.tensor_copy(qs[0:64, 0, :], pq[0:64, 0, :]), reads=[bpq], writes=[bqs])
                                c.dma(pool, QT[40 * 64:41 * 64, t0:t0 + 128], qs[0:64, 0, :], reads=[bqs])
                                w8, bw8 = w8r.next()
                                c.op(act, lambda e: e.mul(w8[:], pm[:, 64:72], 8.0 ** -0.5), reads=[bpm], writes=[bw8])
                                c.dma(pool, W8[t0:t0 + 128, :], w8[:], reads=[bw8])
                c.barrier()
            c.stack = gstack

        def phaseA(l):
            with ExitStack() as ps_:
                c.stack = ps_
                kt = c.sb("a_kt", [64, 12, NSLOT * 128], BF16)
                va = c.sb("a_va", [128, NSLOT, 12 * 65], BF16)
                bk = [Buf() for _ in range(NSLOT)]
                bv = [Buf() for _ in range(NSLOT)]
                qtr = Ring(c, "a_qt", 2, [64, 12, 128], BF16)
                er = Ring(c, "a_e", 5, [128, 512], BF16)
                pr_ = Ring(c, "a_p", 5, [128, 512], BF16)
                psr = Ring(c, "a_ps", 4, [128, 512], F32, psum=True)
                accr = Ring(c, "a_acc", 2, [128, 4, 65], F32, psum=True)
                por = Ring(c, "a_po", 2, [128, 2, 128], BF16, psum=True)
                rcr = Ring(c, "a_rc", 2, [128, 4], F32)
                obr = Ring(c, "a_ob", 2, [128, 256], BF16)
                osr = Ring(c, "a_os", 2, [128, 2, 128], BF16)
                for qb in range(NT):
                    slot = qb % NSLOT
                    tok0 = qb * 128
                    qt, bq = qtr.next()
                    for g in range(3):
                        c.dma(sp, kt[:, g * 4:(g + 1) * 4, slot * 128:(slot + 1) * 128],
                              QT3[:, g * 8 + 4:g * 8 + 8, tok0:tok0 + 128],
                              **({"writes": [bk[slot]]} if g == 0 else {"adds": [bk[slot]]}))
                        c.dma(sp, qt[:, g * 4:(g + 1) * 4, :], QT3[:, g * 8:g * 8 + 4, tok0:tok0 + 128],
                              **({"writes": [bq]} if g == 0 else {"adds": [bq]}))
                    c.dma(sp, va[:, slot, :], VA_A[tok0:tok0 + 128, :], writes=[bv[slot]])
                    acc, bacc = accr.next()
                    c.op(dve, lambda e: e.memset(acc[:], 0.0), writes=[bacc])
                    pairs = [(g, kb) for g in range(3) for kb in range(max(0, qb - A_WB[g]), qb + 1)]

                    def a_qk(i):
                        g, kb = pairs[i]
                        ks = kb % NSLOT
                        ps, bps = psr.next()
                        for h in range(4):
                            c.op(pe, lambda e: e.matmul(ps[:, h * 128:(h + 1) * 128],
                                                        kt[:, g * 4 + h, ks * 128:(ks + 1) * 128], qt[:, g * 4 + h, :],
                                                        start=True, stop=True),
                                 reads=[bk[ks], bq], **({"writes": [bps]} if h == 0 else {"adds": [bps]}))
                        return ps, bps

                    def a_mid(i, ps, bps):
                        g, kb = pairs[i]
                        ee, be = er.next()
                        c.op(act, lambda e: e.activation(ee[:], ps[:], AF.Exp, scale=0.125), reads=[bps], writes=[be])
                        pp, bp = pr_.next()
                        mk = maskA[:, mask_index(g, qb - kb), :]
                        for h in range(4):
                            c.op(dve, lambda e: e.tensor_tensor(pp[:, h * 128:(h + 1) * 128], ee[:, h * 128:(h + 1) * 128],
                                                                mk, ALU.mult),
                                 reads=[be, Bg], **({"writes": [bp]} if h == 0 else {"adds": [bp]}))
                        return pp, bp

                    def a_pv(i, pp, bp):
                        g, kb = pairs[i]
                        ks = kb % NSLOT
                        for h in range(4):
                            c.op(pe, lambda e: e.matmul(acc[:, h, :], pp[:, h * 128:(h + 1) * 128],
                                                        va[:, ks, (g * 4 + h) * 65:(g * 4 + h + 1) * 65],
                                                        start=False, stop=False, skip_group_check=True),
                                 reads=[bp, bv[ks]], upd=[bacc])

                    LA = 3
                    pend = []
                    for i in range(len(pairs)):
                        ps, bps = a_qk(i)
                        pend.append((i,) + a_mid(i, ps, bps))
                        if len(pend) > LA:
                            a_pv(*pend.pop(0))
                    for it in pend:
                        a_pv(*it)
                    rc, brc = rcr.next()
                    c.op(dve, lambda e: e.reciprocal(rc[:], acc[:, :, 64]), reads=[bacc], writes=[brc])
                    ob, bob = obr.next()
                    for h in range(4):
                        c.op(dve, lambda e: e.tensor_scalar(ob[:, h * 64:(h + 1) * 64], acc[:, h, 0:64], rc[:, h:h + 1],
                                                            None, ALU.mult),
                             reads=[bacc, brc], **({"writes": [bob]} if h == 0 else {"adds": [bob]}))
                    po, bpo = por.next()
                    for j in range(2):
                        c.op(pe, lambda e: e.transpose(po[:, j, :], ob[:, j * 128:(j + 1) * 128], ident_b[:]),
                             reads=[bob, Bg], **({"writes": [bpo]} if j == 0 else {"adds": [bpo]}))
                    os_, bos = osr.next()
                    c.op(act, lambda e: e.activation(os_[:], po[:], AF.Copy), reads=[bpo], writes=[bos])
                    c.dma(pool, OT[0:256, tok0:tok0 + 128].rearrange("(b p) t -> p b t", p=128), os_[:], reads=[bos])
                c.barrier()
            c.stack = gstack

        def phaseB1(l):
            with ExitStack() as ps_:
                c.stack = ps_
                kx = c.sb("b_kx", [64, S], BF16)
                bkx = Buf()
                c.dma(sp, kx[:], QT3[:, 40, :], writes=[bkx])
                qxr = Ring(c, "b_qx", 2, [64, 8, 128], BF16)
                wr = Ring(c, "b_w", 2, [128, 8], F32)
                dgr = Ring(c, "b_dg", 2, [128, 8, 128], BF16)
                Ir = Ring(c, "b_I", 2, [128, S], F32)
                rr = Ring(c, "b_r", 4, [128, 512], BF16)
                psr = Ring(c, "b_ps", 4, [128, 512], F32, psum=True)
                pacc = Ring(c, "b_pa", 2, [128, 512], F32, psum=True)
                Mr = Ring(c, "b_M", 2, [128, S], BF16)
                scr_ = Ring(c, "b_sc", 2, [128, 8], F32)
                hsr = Ring(c, "b_hs", 2, [128, NIT], F32)
                ptr = Ring(c, "b_pt", 2, [128, 4, 128], BF16, psum=True)
                mtr = Ring(c, "b_mt", 1, [128, NT, 128], BF16)
                for qb in range(NT):
                    tok0 = qb * 128
                    nv = tok0 + 128
                    qx, bqx = qxr.next()
                    c.dma(sp, qx[:], QT3[:, 32:40, tok0:tok0 + 128], writes=[bqx])
                    w, bw = wr.next()
                    c.dma(sp, w[:], W8[tok0:tok0 + 128, :], writes=[bw])
                    dg, bdg = dgr.next()
                    for h in range(8):
                        c.op(dve, lambda e: e.tensor_scalar(dg[:, h, :], cstf[:, C_ID:C_ID + 128], w[:, h:h + 1], None, ALU.mult),
                             reads=[bw, Bc], **({"writes": [bdg]} if h == 0 else {"adds": [bdg]}))
                    I_, bI = Ir.next()
                    nst = (nv + 511) // 512
                    firstI = True
                    for s_t in range(nst):
                        s0 = s_t * 512
                        wd = min(512, nv - s0)
                        pa, bpa = pacc.next()

                        def lg(h):
                            ps, bps = psr.next()
                            c.op(pe, lambda e: e.matmul(ps[:, 0:wd], qx[:, h, :], kx[:, s0:s0 + wd], start=True, stop=True),
                                 reads=[bqx, bkx], writes=[bps])
                            r, br = rr.next()
                            c.op(act, lambda e: e.activation(r[:, 0:wd], ps[:, 0:wd], AF.Relu, scale=0.125),
                                 reads=[bps], writes=[br])
                            return r, br

                        def dgm(h, r, br):
                            c.op(pe, lambda e: e.matmul(pa[:, 0:wd], dg[:, h, :], r[:, 0:wd], start=(h == 0), stop=(h == 7)),
                                 reads=[bdg, br], **({"writes": [bpa]} if h == 0 else {"adds": [bpa]}))

                        pend = []
                        for h in range(8):
                            pend.append((h,) + lg(h))
                            if len(pend) > 2:
                                dgm(*pend.pop(0))
                        for it in pend:
                            dgm(*it)
                        c.op(act, lambda e: e.activation(I_[:, s0:s0 + wd], pa[:, 0:wd], AF.Copy), reads=[bpa],
                             **({"writes": [bI]} if firstI else {"adds": [bI]}))
                        firstI = False
                    M, bM = Mr.next()
                    sc, bsc = scr_.next()
                    hs, bhs = hsr.next()
                    if qb >= 2:
                        c.op(dve, lambda e: e.tensor_reduce(sc[:, 0:1], I_[:, 0:nv], AX.X, ALU.max,
                                                            apply_absolute_value=True), reads=[bI], writes=[bsc])
                    c.op(dve, lambda e: e.tensor_tensor(I_[:, tok0:nv], I_[:, tok0:nv], negtri, ALU.add),
                         reads=[Bc], upd=[bI])
                    if qb < 2:
                        c.op(dve, lambda e: e.tensor_scalar(M[:, 0:nv], I_[:, 0:nv], -1e29, None, ALU.is_ge),
                             reads=[bI], writes=[bM])
                    else:
                        c.op(dve, lambda e: e.tensor_scalar(sc[:, 0:1], sc[:, 0:1], 1.0, 1e-3, ALU.mult, ALU.add), upd=[bsc])
                        c.op(dve, lambda e: e.tensor_scalar(sc[:, 1:2], sc[:, 0:1], -1.0, None, ALU.mult), upd=[bsc])
                        c.op(dve, lambda e: e.tensor_scalar(sc[:, 4:5], sc[:, 0:1], 2.0, None, ALU.mult), upd=[bsc])
                        c.op(dve, lambda e: e.tensor_scalar(hs[:], cstf[:, C_POW:C_POW + NIT], sc[:, 4:5], None, ALU.mult),
                             reads=[bsc, Bc], writes=[bhs])
                        for k in range(NIT):
                            c.op(dve, lambda e: e.tensor_tensor(sc[:, 2:3], sc[:, 1:2], hs[:, k:k + 1], ALU.add),
                                 reads=[bhs], upd=[bsc])
                            c.op(dve, lambda e: e.tensor_scalar(M[:, 0:nv], I_[:, 0:nv], sc[:, 2:3], None, ALU.is_ge,
                                                                ALU.add, accum_out=sc[:, 3:4]),
                                 reads=[bI], writes=[bM], upd=[bsc])
                            c.op(dve, lambda e: e.scalar_tensor_tensor(sc[:, 4:5], sc[:, 3:4], TOPK - 0.5, hs[:, k:k + 1],
                                                                       ALU.is_ge, ALU.mult), reads=[bhs], upd=[bsc])
                            c.op(dve, lambda e: e.tensor_tensor(sc[:, 1:2], sc[:, 1:2], sc[:, 4:5], ALU.add), upd=[bsc])
                        c.op(dve, lambda e: e.tensor_scalar(M[:, 0:nv], I_[:, 0:nv], sc[:, 1:2], None, ALU.is_ge),
                             reads=[bI, bsc], writes=[bM])
                    mt, bmt = mtr.next()
                    nkb = qb + 1
                    for k0 in range(0, nkb, 4):
                        kn = min(4, nkb - k0)
                        pt, bpt = ptr.next()
                        for j in range(kn):
                            c.op(pe, lambda e: e.transpose(pt[:, j, :], M[:, (k0 + j) * 128:(k0 + j + 1) * 128], ident_b[:]),
                                 reads=[bM, Bg], **({"writes": [bpt]} if j == 0 else {"adds": [bpt]}))
                        c.op(act, lambda e: e.activation(mt[:, k0:k0 + kn, :], pt[:, 0:kn, :], AF.Copy), reads=[bpt],
                             **({"writes": [bmt]} if k0 == 0 else {"adds": [bmt]}))
                    c.dma(pool, MT[qb, :, 0:nkb, :], mt[:, 0:nkb, :], reads=[bmt])
                c.barrier()
            c.stack = gstack

        def phaseB2(l):
            with ExitStack() as ps_:
                c.stack = ps_
                kt = c.sb("b2_kt", [64, 4, S], BF16)
                va = c.sb("b2_va", [128, NT, 4 * 65], BF16)
                bkt, bva = Buf(), Buf()
                c.dma(sp, kt[:], QT3[:, 28:32, :], writes=[bkt])
                vbv = VA_B.rearrange("(n p) c -> p n c", p=128)
                for n0 in range(0, NT, 8):
                    c.dma(sp, va[:, n0:n0 + 8, :], vbv[:, n0:n0 + 8, :], **({"writes": [bva]} if n0 == 0 else {"adds": [bva]}))
                qtr = Ring(c, "b2_qt", 2, [64, 4, 128], BF16)
                mtr = Ring(c, "b2_mt", 2, [128, NT, 128], BF16)
                er = Ring(c, "b2_e", 5, [128, 512], BF16)
                pr_ = Ring(c, "b2_p", 5, [128, 512], BF16)
                psr = Ring(c, "b2_ps", 4, [128, 512], F32, psum=True)
                accr = Ring(c, "b2_acc", 2, [128, 4, 65], F32, psum=True)
                por = Ring(c, "b2_po", 2, [128, 2, 128], BF16, psum=True)
                rcr = Ring(c, "b2_rc", 2, [128, 4], F32)
                obr = Ring(c, "b2_ob", 2, [128, 256], BF16)
                osr = Ring(c, "b2_os", 2, [128, 2, 128], BF16)
                for qb in range(NT):
                    tok0 = qb * 128
                    nkb = qb + 1
                    qt, bq = qtr.next()
                    c.dma(sp, qt[:], QT3[:, 24:28, tok0:tok0 + 128], writes=[bq])
                    mt, bmt = mtr.next()
                    c.dma(sp, mt[:, 0:nkb, :], MT[qb, :, 0:nkb, :], writes=[bmt])
                    acc, bacc = accr.next()
                    c.op(dve, lambda e: e.memset(acc[:], 0.0), writes=[bacc])
                    def b_qk(kb):
                        ps, bps = psr.next()
                        for h in range(4):
                            c.op(pe, lambda e: e.matmul(ps[:, h * 128:(h + 1) * 128], kt[:, h, kb * 128:(kb + 1) * 128],
                                                        qt[:, h, :], start=True, stop=True),
                                 reads=[bkt, bq], **({"writes": [bps]} if h == 0 else {"adds": [bps]}))
                        return ps, bps

                    def b_mid(kb, ps, bps):
                        ee, be = er.next()
                        c.op(act, lambda e: e.activation(ee[:], ps[:], AF.Exp, scale=0.125), reads=[bps], writes=[be])
                        pp, bp = pr_.next()
                        mk = mt[:, kb, :]
                        for h in range(4):
                            c.op(dve, lambda e: e.tensor_tensor(pp[:, h * 128:(h + 1) * 128], ee[:, h * 128:(h + 1) * 128],
                                                                mk, ALU.mult),
                                 reads=[be, bmt], **({"writes": [bp]} if h == 0 else {"adds": [bp]}))
                        return pp, bp

                    def b_pv(kb, pp, bp):
                        for h in range(4):
                            c.op(pe, lambda e: e.matmul(acc[:, h, :], pp[:, h * 128:(h + 1) * 128],
                                                        va[:, kb, h * 65:(h + 1) * 65],
                                                        start=False, stop=False, skip_group_check=True),
                                 reads=[bp, bva], upd=[bacc])

                    LA = 3
                    pend = []
                    for kb in range(nkb):
                        ps, bps = b_qk(kb)
                        pend.append((kb,) + b_mid(kb, ps, bps))
                        if len(pend) > LA:
                            b_pv(*pend.pop(0))
                    for it in pend:
                        b_pv(*it)
                    rc, brc = rcr.next()
                    c.op(dve, lambda e: e.reciprocal(rc[:], acc[:, :, 64]), reads=[bacc], writes=[brc])
                    ob, bob = obr.next()
                    for h in range(4):
                        c.op(dve, lambda e: e.tensor_scalar(ob[:, h * 64:(h + 1) * 64], acc[:, h, 0:64], rc[:, h:h + 1],
                                                            None, ALU.mult),
                             reads=[bacc, brc], **({"writes": [bob]} if h == 0 else {"adds": [bob]}))
                    po, bpo = por.next()
                    for j in range(2):
                        c.op(pe, lambda e: e.transpose(po[:, j, :], ob[:, j * 128:(j + 1) * 128], ident_b[:]),
                             reads=[bob, Bg], **({"writes": [bpo]} if j == 0 else {"adds": [bpo]}))
                    os_, bos = osr.next()
                    c.op(act, lambda e: e.activation(os_[:], po[:], AF.Copy), reads=[bpo], writes=[bos])
                    c.dma(pool, OT[256:512, tok0:tok0 + 128].rearrange("(b p) t -> p b t", p=128), os_[:], reads=[bos])
                c.barrier()
            c.stack = gstack

        def phaseC(l):
            lam_init = 0.8 - 0.6 * math.exp(-0.3 * l)
            with ExitStack() as ps_:
                c.stack = ps_
                ktr = Ring(c, "c_kt", 2, [64, 2, S], BF16)
                var = Ring(c, "c_va", 2, [128, NT, 129], BF16)
                qtr = Ring(c, "c_qt", 2, [64, 2, 512], BF16)
                er = Ring(c, "c_e", 4, [128, 512], BF16)
                psr = Ring(c, "c_ps", 3, [128, 512], F32, psum=True)
                accs = [c.ps(f"c_acc{i}", [128, 3, 129], F32) for i in range(3)]
                bacc = Buf()
                por = Ring(c, "c_po", 2, [128, 128], BF16, psum=True)
                sg = c.sb("c_sg", [128, 128], F32)
                bsg = Buf()
                c.dma(sp, sg[:], subln[l].partition_broadcast(128), writes=[bsg])
                tri = maskA[:, 0, :]
                smr = Ring(c, "c_sm", 2, [128, 8], F32)
                o1r = Ring(c, "c_o1", 2, [128, 128], F32)
                o2r = Ring(c, "c_o2", 2, [128, 128], F32)
                jr = Ring(c, "c_j", 2, [128, 128], F32)
                obr = Ring(c, "c_ob", 2, [128, 128], BF16)
                osr = Ring(c, "c_os", 2, [128, 512], BF16)

                def accv(cc, j):
                    i = cc * 4 + j
                    return accs[i // 3][:, i % 3, :]

                for h in range(4):
                    kt, bkt = ktr.next()
                    va, bva = var.next()
                    c.dma(sp, kt[:], QT3[:, 49 + 2 * h:51 + 2 * h, :], writes=[bkt])
                    vcv = VA_C[:, h * 129:(h + 1) * 129].rearrange("(n p) c -> p n c", p=128)
                    for n0 in range(0, NT, 8):
                        c.dma(sp, va[:, n0:n0 + 8, :], vcv[:, n0:n0 + 8, :], **({"writes": [bva]} if n0 == 0 else {"adds": [bva]}))
                    for qt_i in range(S // 512):
                        q0 = qt_i * 512
                        qt, bq = qtr.next()
                        c.dma(sp, qt[:], QT3[:, 41 + 2 * h:43 + 2 * h, q0:q0 + 512], writes=[bq])
                        for a in accs:
                            c.op(dve, lambda e: e.memset(a[:], 0.0), **({"writes": [bacc]} if a is accs[0] else {"upd": [bacc]}))
                        nkb = (q0 + 512) // 128
                        steps = [(kb, cc) for kb in range(nkb) for cc in range(2)]

                        def c_qk(kb, cc):
                            ps, bps = psr.next()
                            c.op(pe, lambda e: e.matmul(ps[:], kt[:, cc, kb * 128:(kb + 1) * 128], qt[:, cc, :],
                                                        start=True, stop=True), reads=[bkt, bq], writes=[bps])
                            return ps, bps

                        def c_mid(kb, cc, ps, bps):
                            jk = kb - q0 // 128
                            ee, be = er.next()
                            c.op(act, lambda e: e.activation(ee[:], ps[:], AF.Exp, scale=0.125), reads=[bps], writes=[be])
                            if jk >= 0:
                                c.op(dve, lambda e: e.tensor_tensor(ee[:, jk * 128:(jk + 1) * 128],
                                                                    ee[:, jk * 128:(jk + 1) * 128], tri, ALU.mult),
                                     reads=[Bg], upd=[be])
                            return ee, be

                        def c_pv(kb, cc, ee, be):
                            jk = kb - q0 // 128
                            for j in range(max(jk, 0), 4):
                                c.op(pe, lambda e: e.matmul(accv(cc, j), ee[:, j * 128:(j + 1) * 128], va[:, kb, :],
                                                            start=False, stop=False, skip_group_check=True),
                                     reads=[be, bva], upd=[bacc])

                        LA = 2
                        pend = []
                        for (kb, cc) in steps:
                            ps, bps = c_qk(kb, cc)
                            pend.append((kb, cc) + c_mid(kb, cc, ps, bps))
                            if len(pend) > LA:
                                c_pv(*pend.pop(0))
                        for it in pend:
                            c_pv(*it)
                        os_, bos = osr.next()
                        for j in range(4):
                            sm, bsm = smr.next()
                            a1 = accv(0, j)
                            a2 = accv(1, j)
                            c.op(dve, lambda e: e.reciprocal(sm[:, 0:1], a1[:, 128:129]), reads=[bacc], writes=[bsm])
                            c.op(dve, lambda e: e.reciprocal(sm[:, 1:2], a2[:, 128:129]), reads=[bacc], upd=[bsm])
                            c.op(dve, lambda e: e.tensor_tensor(sm[:, 1:2], sm[:, 1:2], lamt[:, l, 1:2], ALU.mult),
                                 reads=[Bg], upd=[bsm])
                            o1, bo1 = o1r.next()
                            c.op(dve, lambda e: e.tensor_scalar(o1[:], a1[:, 0:128], sm[:, 0:1], None, ALU.mult),
                                 reads=[bacc, bsm], writes=[bo1])
                            o2, bo2 = o2r.next()
                            c.op(dve, lambda e: e.scalar_tensor_tensor(o2[:], a2[:, 0:128], sm[:, 1:2], o1[:],
                                                                       ALU.mult, ALU.add),
                                 reads=[bacc, bsm, bo1], writes=[bo2])
                            c.op(dve, lambda e: e.memset(sm[:, 2:3], 0.0), upd=[bsm])
                            jj, bjj = jr.next()
                            c.op(act, lambda e: e.activation(jj[:], o2[:], AF.Square, accum_out=sm[:, 2:3]),
                                 reads=[bo2], writes=[bjj], upd=[bsm])
                            c.op(dve, lambda e: e.tensor_scalar(sm[:, 3:4], sm[:, 2:3], 1.0 / 128, EPS, ALU.mult, ALU.add),
                                 upd=[bsm])
                            c.op(act, lambda e: e.activation(sm[:, 4:5], sm[:, 3:4], AF.Sqrt), upd=[bsm])
                            c.op(dve, lambda e: e.reciprocal(sm[:, 4:5], sm[:, 4:5]), upd=[bsm])
                            c.op(dve, lambda e: e.tensor_scalar(sm[:, 4:5], sm[:, 4:5], 1.0 - lam_init, None, ALU.mult),
                                 upd=[bsm])
                            ob, bob = obr.next()
                            c.op(dve, lambda e: e.scalar_tensor_tensor(ob[:], o2[:], sm[:, 4:5], sg[:], ALU.mult, ALU.mult),
                                 reads=[bo2, bsm, bsg], writes=[bob])
                            po, bpo = por.next()
                            c.op(pe, lambda e: e.transpose(po[:], ob[:], ident_b[:]), reads=[bob, Bg], writes=[bpo])
                            c.op(act, lambda e: e.activation(os_[:, j * 128:(j + 1) * 128], po[:], AF.Copy), reads=[bpo],
                                 **({"writes": [bos]} if j == 0 else {"adds": [bos]}))
                        c.dma(pool, OT[512 + h * 128:512 + (h + 1) * 128, q0:q0 + 512], os_[:], reads=[bos])
                c.barrier()
            c.stack = gstack

        def bcast_load(dst, src_vec_ap, bdst):
            c.dma(sp, dst, src_vec_ap.partition_broadcast(128), writes=[bdst])

        def phaseM(l, xin, xmid):
            with ExitStack() as ps_:
                c.stack = ps_
                st = {"wst": Ring(c, "m_wst", 2, [128, 4, 1024], F32)}
                wa = c.sb("m_wa", [128, 2, 1024], BF16)
                wbb = c.sb("m_wb", [128, 2, 1024], BF16)
                wc = c.sb("m_wc", [128, 4, 1024], BF16)
                wo = c.sb("m_wo", [128, 8, 1024], BF16)
                bwa, bwb_, bwc, bwo = Buf(), Buf(), Buf(), Buf()
                load_w(st, wa, bwa, w_br_a[l].rearrange("(kc p) n -> p kc n", p=128), 2, 1024)
                load_w(st, wbb, bwb_, w_br_b[l].rearrange("(kc p) n -> p kc n", p=128), 2, 1024)
                load_w(st, wc, bwc, w_br_c[l].rearrange("(kc p) n -> p kc n", p=128), 4, 1024)
                load_w(st, wo, bwo, w_out[l].rearrange("(kc p) n -> p kc n", p=128), 8, 1024)
                gp = c.sb("m_gp", [128, D], F32)
                bgp = Buf()
                bcast_load(gp[:], norms[l, 8:16, :].rearrange("a b -> (a b)"), bgp)
                otr = Ring(c, "m_ot", 2, [128, 8, 512], BF16)
                gtr = Ring(c, "m_gt", 1, [128, 24, 512], BF16)
                yT = c.sb("m_yT", [128, 8, 512], BF16)
                byT = Buf()
                pbr = Ring(c, "m_pb", 4, [128, 512], F32, psum=True)
                phr = Ring(c, "m_ph", 2, [128, 1024], F32, psum=True)
                t1r = Ring(c, "m_t1", 2, [128, 512], F32)
                t2r = Ring(c, "m_t2", 2, [128, 512], F32)
                xr = Ring(c, "m_x", 2, [128, D], F32)
                hr = Ring(c, "m_h", 2, [128, D], F32)
                jr = Ring(c, "m_j", 1, [128, D], BF16)
                ssr = Ring(c, "m_ss", 2, [128, 4], F32)
                OTv = OT.rearrange("(kc p) t -> p kc t", p=128)
                GTv = GT.rearrange("(kc p) t -> p kc t", p=128)
                for tg in range(S // 512):
                    t0 = tg * 512
                    ot, bot = otr.next()
                    c.dma(sp, ot[:], OTv[:, :, t0:t0 + 512], writes=[bot])
                    gt, bgt = gtr.next()
                    c.dma(sp, gt[:], GTv[:, :, t0:t0 + 512], writes=[bgt])
                    for n in range(8):
                        pa, bpa = pbr.next()
                        for kc in range(2):
                            c.op(pe, lambda e: e.matmul(pa[:], wa[:, kc, n * 128:(n + 1) * 128], ot[:, kc, :],
                                                        start=(kc == 0), stop=(kc == 1)),
                                 reads=[bwa, bot], **({"writes": [bpa]} if kc == 0 else {"adds": [bpa]}))
                        pb, bpb = pbr.next()
                        for kc in range(2):
                            c.op(pe, lambda e: e.matmul(pb[:], wbb[:, kc, n * 128:(n + 1) * 128], ot[:, 2 + kc, :],
                                                        start=(kc == 0), stop=(kc == 1)),
                                 reads=[bwb_, bot], **({"writes": [bpb]} if kc == 0 else {"adds": [bpb]}))
                        pc, bpc = pbr.next()
                        for kc in range(4):
                            c.op(pe, lambda e: e.matmul(pc[:], wc[:, kc, n * 128:(n + 1) * 128], ot[:, 4 + kc, :],
                                                        start=(kc == 0), stop=(kc == 3)),
                                 reads=[bwc, bot], **({"writes": [bpc]} if kc == 0 else {"adds": [bpc]}))
                        t1, bt1 = t1r.next()
                        t2, bt2 = t2r.next()
                        c.op(dve, lambda e: e.tensor_tensor(t1[:], pa[:], gt[:, n, :], ALU.mult), reads=[bpa, bgt], writes=[bt1])
                        c.op(dve, lambda e: e.tensor_tensor(t2[:], pb[:], gt[:, 8 + n, :], ALU.mult), reads=[bpb, bgt], writes=[bt2])
                        c.op(pl, lambda e: e.tensor_tensor(t1[:], t1[:], t2[:], ALU.add), reads=[bt2], upd=[bt1])
                        c.op(dve, lambda e: e.tensor_tensor(t2[:], pc[:], gt[:, 16 + n, :], ALU.mult), reads=[bpc, bgt], upd=[bt2])
                        c.op(pl, lambda e: e.tensor_tensor(yT[:, n, :], t1[:], t2[:], ALU.add), reads=[bt1, bt2],
                             **({"writes": [byT]} if n == 0 else {"adds": [byT]}))
                    for j in range(4):
                        tj = t0 + j * 128
                        ph, bph = phr.next()
                        for half in range(2):
                            for kc in range(8):
                                c.op(pe, lambda e: e.matmul(ph[:, half * 512:(half + 1) * 512], yT[:, kc, j * 128:(j + 1) * 128],
                                                            wo[:, kc, half * 512:(half + 1) * 512],
                                                            start=(kc == 0), stop=(kc == 7)),
                                     reads=[byT, bwo], **({"writes": [bph]} if (kc == 0 and half == 0) else {"adds": [bph]}))
                        xt, bx = xr.next()
                        c.dma(sp, xt[:], xin[tj:tj + 128, :], writes=[bx])
                        ss, bs = ssr.next()
                        c.op(dve, lambda e: e.memset(ss[:], 0.0), writes=[bs])
                        jk, bj = jr.next()
                        c.op(act, lambda e: e.activation(jk[:], ph[:], AF.Square, accum_out=ss[:, 0:1]),
                             reads=[bph], writes=[bj], upd=[bs])
                        c.op(dve, lambda e: e.tensor_scalar(ss[:, 1:2], ss[:, 0:1], 1.0 / D, EPS, ALU.mult, ALU.add), upd=[bs])
                        c.op(act, lambda e: e.activation(ss[:, 2:3], ss[:, 1:2], AF.Sqrt), upd=[bs])
                        c.op(dve, lambda e: e.reciprocal(ss[:, 2:3], ss[:, 2:3]), upd=[bs])
                        hh, bh = hr.next()
                        c.op(dve, lambda e: e.scalar_tensor_tensor(hh[:], ph[:], ss[:, 2:3], gp[:], ALU.mult, ALU.mult),
                             reads=[bph, bs, bgp], writes=[bh])
                        c.op(pl, lambda e: e.tensor_tensor(hh[:], hh[:], xt[:], ALU.add), reads=[bx], upd=[bh])
                        c.dma(pool, xmid[tj:tj + 128, :], hh[:], reads=[bh])
                c.barrier()
            c.stack = gstack

        def phaseF(l, xmid, xout):
            TF = 512
            GC = 0.7978845608028654
            with ExitStack() as ps_:
                c.stack = ps_
                st = {
                    "xr": Ring(c, "f_xr", 2, [128, D], F32),
                    "ss": Ring(c, "f_ss", 2, [128, 4], F32),
                    "junk": Ring(c, "f_junk", 1, [128, D], BF16),
                    "xb": Ring(c, "f_xb", 2, [128, D], BF16),
                    "pT": Ring(c, "f_pT", 2, [128, 8, 128], BF16, psum=True),
                    "wst": Ring(c, "f_wst", 2, [128, 8, 512], F32),
                }
                wd = c.sb("f_wd", [128, 22, 1024], BF16)
                bwd = Buf()
                wdv = w_down[l].rearrange("(kc p) n -> p kc n", p=128)
                firstw = True
                for k0 in range(0, 22, 8):
                    kn = min(8, 22 - k0)
                    for half in range(2):
                        ws, bws = st["wst"].next()
                        c.dma(sp, ws[:, 0:kn, :], wdv[:, k0:k0 + kn, half * 512:(half + 1) * 512], writes=[bws])
                        for kc in range(kn):
                            c.op(pl, lambda e: e.tensor_copy(wd[:, k0 + kc, half * 512:(half + 1) * 512], ws[:, kc, :]),
                                 reads=[bws], **({"writes": [bwd]} if firstw else {"adds": [bwd]}))
                            firstw = False
                gp = c.sb("f_gp", [128, D], F32)
                bgp = Buf()
                bcast_load(gp[:], norms[l, 24:32, :].rearrange("a b -> (a b)"), bgp)
                gain = gcols[:, l, 16:24]
                wur = Ring(c, "f_wu", 2, [128, 8, 256], BF16)
                xnT = c.sb("f_xnT", [128, 8, TF], BF16)
                bxn = [Buf() for _ in range(TF // 128)]
                halo = c.sb("f_halo", [128, 44, 2], F32)
                bhalo = [Buf() for _ in range(44)]
                c.op(dve, lambda e: e.memset(halo[:], 0.0), writes=bhalo)
                aT = c.sb("f_aT", [128, 22, TF], BF16)
                baT = Buf()
                pur = Ring(c, "f_pu", 4, [128, 512], F32, psum=True)
                phr = Ring(c, "f_ph", 1, [128, 1024], F32, psum=True)
                cgr = Ring(c, "f_cg", 1, [128, 512], F32)
                cur = Ring(c, "f_cu", 1, [128, 512], F32)
                t1r = Ring(c, "f_t1", 1, [128, 512], F32)
                t2r = Ring(c, "f_t2", 1, [128, 512], F32)
                hr = Ring(c, "f_h", 1, [128, D], F32)
                jr = Ring(c, "f_j", 1, [128, D], BF16)
                ssr = Ring(c, "f_ss2", 2, [128, 4], F32)
                wuv = w_up[l].rearrange("(kc p) n -> p kc n", p=128)

                def conv(pu, bpu, ch, dst, bdst):
                    w0 = cw[:, l, ch:ch + 1]
                    w1 = cw[:, l, 44 + ch:44 + ch + 1]
                    w2 = cw[:, l, 88 + ch:88 + ch + 1]
                    c.op(act, lambda e: e.activation(dst[:], pu[:], AF.Identity, bias=cb[:, l, ch:ch + 1], scale=w2),
                         reads=[bpu, Bg], writes=[bdst])
                    c.op(dve, lambda e: e.scalar_tensor_tensor(dst[:, 1:TF], pu[:, 0:TF - 1], w1, dst[:, 1:TF], ALU.mult, ALU.add),
                         reads=[bpu, Bg], upd=[bdst])
                    c.op(dve, lambda e: e.scalar_tensor_tensor(dst[:, 2:TF], pu[:, 0:TF - 2], w0, dst[:, 2:TF], ALU.mult, ALU.add),
                         reads=[bpu, Bg], upd=[bdst])
                    c.op(dve, lambda e: e.scalar_tensor_tensor(dst[:, 0:1], halo[:, ch, 1:2], w1, dst[:, 0:1], ALU.mult, ALU.add),
                         reads=[bhalo[ch], Bg], upd=[bdst])
                    c.op(dve, lambda e: e.scalar_tensor_tensor(dst[:, 0:2], halo[:, ch, 0:2], w0, dst[:, 0:2], ALU.mult, ALU.add),
                         reads=[bhalo[ch], Bg], upd=[bdst])
                    c.op(dve, lambda e: e.tensor_copy(halo[:, ch, :], pu[:, TF - 2:TF]), reads=[bpu], writes=[bhalo[ch]])

                for tg in range(S // TF):
                    t0 = tg * TF
                    for tt in range(TF // 128):
                        norm_transpose(st, xmid[t0 + tt * 128:t0 + (tt + 1) * 128, :],
                                       xnT[:, :, tt * 128:(tt + 1) * 128], bxn[tt], True)
                    for cp in range(22):
                        wu, bwu = wur.next()
                        ws, bws = st["wst"].next()
                        c.dma(sp, ws[:, :, 0:128], wuv[:, :, cp * 128:(cp + 1) * 128], writes=[bws])
                        c.dma(sp, ws[:, :, 128:256], wuv[:, :, DFF + cp * 128:DFF + (cp + 1) * 128], adds=[bws])
                        for kc in range(8):
                            c.op(pl, lambda e: e.tensor_scalar(wu[:, kc, :], ws[:, kc, 0:256], gain[:, kc:kc + 1], None, ALU.mult),
                                 reads=[bws, Bg], **({"writes": [bwu]} if kc == 0 else {"adds": [bwu]}))
                        pg, bpg = pur.next()
                        pu, bpu = pur.next()
                        for (pp_, bpp, off) in ((pg, bpg, 0), (pu, bpu, 128)):
                            for kc in range(8):
                                c.op(pe, lambda e: e.matmul(pp_[:], wu[:, kc, off:off + 128], xnT[:, kc, :],
                                                            start=(kc == 0), stop=(kc == 7)),
                                     reads=[bwu] + bxn, **({"writes": [bpp]} if kc == 0 else {"adds": [bpp]}))
                        cg, bcg = cgr.next()
                        cu, bcu = cur.next()
                        conv(pg, bpg, cp, cg, bcg)
                        conv(pu, bpu, 22 + cp, cu, bcu)
                        t1, bt1 = t1r.next()
                        t2, bt2 = t2r.next()
                        c.op(act, lambda e: e.activation(t1[:], cg[:], AF.Square), reads=[bcg], writes=[bt1])
                        c.op(dve, lambda e: e.tensor_scalar(t1[:], t1[:], 0.044715, 1.0, ALU.mult, ALU.add), upd=[bt1])
                        c.op(pl, lambda e: e.tensor_tensor(t1[:], t1[:], cg[:], ALU.mult), reads=[bcg], upd=[bt1])
                        c.op(act, lambda e: e.activation(t2[:], t1[:], AF.Sigmoid, scale=2.0 * GC), reads=[bt1], writes=[bt2])
                        c.op(pl, lambda e: e.tensor_tensor(t2[:], t2[:], cg[:], ALU.mult), reads=[bcg], upd=[bt2])
                        c.op(dve, lambda e: e.tensor_tensor(aT[:, cp, :], t2[:], cu[:], ALU.mult), reads=[bt2, bcu],
                             **({"writes": [baT]} if cp == 0 else {"adds": [baT]}))
                    for j in range(TF // 128):
                        tj = t0 + j * 128
                        ph, bph = phr.next()
                        for half in range(2):
                            for kc in range(22):
                                c.op(pe, lambda e: e.matmul(ph[:, half * 512:(half + 1) * 512], aT[:, kc, j * 128:(j + 1) * 128],
                                                            wd[:, kc, half * 512:(half + 1) * 512],
                                                            start=(kc == 0), stop=(kc == 21)),
                                     reads=[baT, bwd], **({"writes": [bph]} if (kc == 0 and half == 0) else {"adds": [bph]}))
                        ss, bs = ssr.next()
                        c.op(dve, lambda e: e.memset(ss[:], 0.0), writes=[bs])
                        jk, bj = jr.next()
                        c.op(act, lambda e: e.activation(jk[:], ph[:], AF.Square, accum_out=ss[:, 0:1]),
                             reads=[bph], writes=[bj], upd=[bs])
                        c.op(dve, lambda e: e.tensor_scalar(ss[:, 1:2], ss[:, 0:1], 1.0 / D, EPS, ALU.mult, ALU.add), upd=[bs])
                        c.op(act, lambda e: e.activation(ss[:, 2:3], ss[:, 1:2], AF.Sqrt), upd=[bs])
                        c.op(dve, lambda e: e.reciprocal(ss[:, 2:3], ss[:, 2:3]), upd=[bs])
                        hh, bh = hr.next()
                        c.op(dve, lambda e: e.scalar_tensor_tensor(hh[:], ph[:], ss[:, 2:3], gp[:], ALU.mult, ALU.mult),
                             reads=[bph, bs, bgp], writes=[bh])
                        xt, bx = st["xr"].next()
                        c.dma(sp, xt[:], xmid[tj:tj + 128, :], writes=[bx])
                        c.op(pl, lambda e: e.tensor_tensor(hh[:], hh[:], xt[:], ALU.add), reads=[bx], upd=[bh])
                        c.dma(pool, xout[tj:tj + 128, :], hh[:], reads=[bh])
                c.barrier()
            c.stack = gstack

        cur = x_in
        for l in range(depth):
            last = (l == depth - 1)
            if "1" in phases:
                phase1(l, cur)
            if "A" in phases:
                phaseA(l)
            if "b" in phases:
                phaseB1(l)
            if "B" in phases:
                phaseB2(l)
            if "C" in phases:
                phaseC(l)
            if "M" in phases:
                phaseM(l, cur, X1)
            if "F" in phases:
                phaseF(l, X1, y_out if last else X2)
            cur = X2
        c.barrier()
    return nc


def make_in_maps(inputs, S):
    cst = make_consts()
    f = lambda a: np.ascontiguousarray(np.asarray(a, dtype=np.float32))
    shared = {
        "w_in": f(inputs["w_in"]), "w_br_a": f(inputs["w_br_a"]), "w_br_b": f(inputs["w_br_b"]),
        "w_br_c": f(inputs["w_br_c"]), "w_out": f(inputs["w_out"]),
        "lam": np.ascontiguousarray(np.stack([f(inputs["lam_q1"]), f(inputs["lam_k1"]), f(inputs["lam_q2"]),
                                              f(inputs["lam_k2"])], axis=1)),
        "subln_g": f(inputs["subln_g"]),
        "norms": np.ascontiguousarray(np.stack([f(inputs["norm_mix_pre"]), f(inputs["norm_mix_post"]),
                                                f(inputs["norm_ffn_pre"]), f(inputs["norm_ffn_post"])],
                                               axis=1).reshape(DEPTH, 32, 128)),
        "w_up": f(inputs["w_ffn_up"]),
        "conv_w": np.ascontiguousarray(f(inputs["conv_w"]).reshape(DEPTH, 132, 128)),
        "conv_b": np.ascontiguousarray(f(inputs["conv_b"]).reshape(DEPTH, 44, 128)),
        "w_down": f(inputs["w_ffn_down"]),
        "cst": cst,
    }
    x = f(inputs["x"])
    pos = np.ascontiguousarray(np.asarray(inputs["positions"], dtype=np.int32))
    maps = []
    for b in range(x.shape[0]):
        m = dict(shared)
        m["x"] = np.ascontiguousarray(x[b])
        m["pos"] = np.ascontiguousarray(pos[b].reshape(S // 128, 128))
        maps.append(m)
    return maps


def kernel(**inputs):
    x = np.asarray(inputs["x"])
    B, S, _ = x.shape
    nc = build(S)
    maps = make_in_maps(inputs, S)
    res = run_bass_kernel_spmd(nc, maps, core_ids=list(range(B)))
    out = np.stack([np.asarray(r["y"], dtype=np.float32) for r in res.results], axis=0)
    return out
```

```python
import math
import os as _os
from contextlib import ExitStack
import numpy as np
import concourse.bass as bass
import concourse.mybir as mybir
from concourse.bass_utils import run_bass_kernel_spmd

F32 = mybir.dt.float32
BF16 = mybir.dt.bfloat16
I32 = mybir.dt.int32
AF = mybir.ActivationFunctionType
ALU = mybir.AluOpType
AX = mybir.AxisListType

D = 1024
IN_W = 8264
DFF = 2816
NCORES = 8
DEPTH = 2
EPS = 1e-6
A_WB = (1, 4, 16)
A_PAIRS = ((128, 1), (512, 4), (2048, 16))
NSLOT = 17
NIT = 16
TOPK = 256
NMASK = 24
C_ID = 0
C_MASK = 128
C_NEG = C_MASK + NMASK * 128
C_POW = C_NEG + 128
C_INV = C_POW + NIT
NCST = C_INV + 8


def make_consts():
    c = np.zeros((128, NCST), np.float32)
    c[:, C_ID:C_ID + 128] = np.eye(128, dtype=np.float32)
    s = np.arange(128)[:, None]
    q = np.arange(128)[None, :]
    mi = 0
    for g, (w, d) in enumerate(A_PAIRS):
        for dl in range(A_WB[g] + 1):
            dist = 128 * dl + q - s
            m = (dist >= 0) & (dist <= w) & (dist % d == 0)
            c[:, C_MASK + mi * 128:C_MASK + (mi + 1) * 128] = m.astype(np.float32)
            mi += 1
    assert mi == NMASK
    c[:, C_NEG:C_NEG + 128] = np.where(q <= s, 0.0, -1e30).astype(np.float32)
    for k in range(NIT):
        c[:, C_POW + k] = 2.0 ** (-(k + 1))
    inv = 500000.0 ** (-np.arange(0, 16, 2, dtype=np.float32) / 16.0)
    c[:, C_INV:C_INV + 8] = inv[None, :].astype(np.float32)
    return c


def mask_index(g, dl):
    return sum(A_WB[i] + 1 for i in range(g)) + dl


class Buf:
    __slots__ = ("writers", "readers")

    def __init__(self):
        self.writers = []
        self.readers = []


def _compact(toks):
    best = {}
    for s, v in toks:
        if s.num not in best or best[s.num][1] < v:
            best[s.num] = (s, v)
    return list(best.values())


class Eng:
    def __init__(self, ctx, name, eng):
        self.ctx = ctx
        self.name = name
        self.eng = eng
        self.sem = None
        self.count = 0
        self.seen = {}
        self.own = set()

    def wait(self, tok):
        sem, val = tok
        if self.name == "pe" and sem.num in self.own:
            return
        if self.seen.get(sem.num, 0) >= val:
            return
        self.eng.wait_ge(sem, val)
        self.seen[sem.num] = val

    def last(self):
        return (self.sem, self.count) if self.sem is not None and self.count > 0 else None


class Ctx:
    SEM_ROLL = 32000

    def __init__(self, nc, semstack):
        self.nc = nc
        self.semstack = semstack
        self.stack = None
        self.nsem = 0
        self.pe = Eng(self, "pe", nc.tensor)
        self.act = Eng(self, "act", nc.scalar)
        self.dve = Eng(self, "dve", nc.vector)
        self.pool = Eng(self, "pool", nc.gpsimd)
        self.sp = Eng(self, "sp", nc.sync)
        self.engs = [self.pe, self.act, self.dve, self.pool, self.sp]
        self.dma_pool = {}
        self.old_sems = []
        self.nuniq = 0

    def alloc_sem(self, name):
        self.nsem += 1
        return self.semstack.enter_context(self.nc.semaphore(f"{name}_{self.nsem}"))

    def sb(self, name, shape, dtype):
        self.nuniq += 1
        return self.stack.enter_context(self.nc.sbuf_tensor(f"{name}_{self.nuniq}", list(shape), dtype))

    def ps(self, name, shape, dtype):
        self.nuniq += 1
        return self.stack.enter_context(self.nc.psum_tensor(f"{name}_{self.nuniq}", list(shape), dtype))

    def _pre(self, E, reads, writes, adds, upd):
        for b in reads:
            for t in b.writers:
                E.wait(t)
        for b in writes:
            for t in b.writers:
                E.wait(t)
            for t in b.readers:
                E.wait(t)
        for b in upd:
            for t in b.writers:
                E.wait(t)
            for t in b.readers:
                E.wait(t)
        for b in adds:
            for t in b.readers:
                E.wait(t)

    def _post(self, tok, reads, writes, adds, upd):
        for b in reads:
            b.readers.append(tok)
            if len(b.readers) > 16:
                b.readers = _compact(b.readers)
        for b in writes:
            b.writers = [tok]
            b.readers = []
        for b in upd:
            b.writers.append(tok)
            b.readers = []
            if len(b.writers) > 16:
                b.writers = _compact(b.writers)
        for b in adds:
            b.writers.append(tok)
            b.readers = []
            if len(b.writers) > 16:
                b.writers = _compact(b.writers)

    def op(self, E, fn, reads=(), writes=(), adds=(), upd=()):
        self._pre(E, reads, writes, adds, upd)
        if E.sem is None or E.count >= self.SEM_ROLL:
            if E.sem is not None:
                self.old_sems.append((E.sem, E.count))
            E.sem = self.alloc_sem(E.name)
            E.own.add(E.sem.num)
            E.count = 0
        ins = fn(E.eng)
        E.count += 1
        ins.then_inc(E.sem, 1)
        tok = (E.sem, E.count)
        self._post(tok, reads, writes, adds, upd)
        return tok

    def dma(self, E, out, in_, reads=(), writes=(), adds=(), upd=(), nsem=16, **kw):
        key = E.name
        if key not in self.dma_pool:
            self.dma_pool[key] = {"sems": [], "vals": [], "next": 0, "toks": []}
        P = self.dma_pool[key]
        self._pre(E, reads, writes, adds, upd)
        i = P["next"]
        if i >= len(P["sems"]):
            P["sems"].append(self.alloc_sem("dma" + key))
            P["vals"].append(0)
            P["toks"].append(None)
        else:
            E.wait(P["toks"][i])
        P["next"] = (i + 1) % nsem
        sem = P["sems"][i]
        P["vals"][i] += 16
        ins = E.eng.dma_start(out=out, in_=in_, **kw)
        ins.then_inc(sem, 16)
        tok = (sem, P["vals"][i])
        P["toks"][i] = tok
        self._post(tok, reads, writes, adds, upd)
        return tok

    def barrier(self):
        toks = []
        for E in self.engs:
            t = E.last()
            if t is not None:
                toks.append(t)
        for P in self.dma_pool.values():
            for t in P["toks"]:
                if t is not None:
                    toks.append(t)
        for E in self.engs:
            for t in toks:
                E.wait(t)


class Ring:
    def __init__(self, c, name, n, shape, dtype, psum=False):
        self.t = [(c.ps if psum else c.sb)(f"{name}{i}", shape, dtype) for i in range(n)]
        self.b = [Buf() for _ in range(n)]
        self.i = 0
        self.n = n

    def next(self):
        t, b = self.t[self.i], self.b[self.i]
        self.i = (self.i + 1) % self.n
        return t, b


def build(S, depth=DEPTH, dbg=False, phases="1AbBCMF"):
    NT = S // 128
    nc = bass.Bass("TRN2", target_bir_lowering=False)

    def din(name, shape, dt=F32):
        return nc.dram_tensor(name, list(shape), dt, kind="ExternalInput").ap()

    x_in = din("x", [S, D])
    pos_in = din("pos", [NT, 128], I32)
    w_in = din("w_in", [DEPTH, D, IN_W])
    w_br_a = din("w_br_a", [DEPTH, 256, D])
    w_br_b = din("w_br_b", [DEPTH, 256, D])
    w_br_c = din("w_br_c", [DEPTH, 512, D])
    w_out = din("w_out", [DEPTH, D, D])
    lam_in = din("lam", [DEPTH, 4, 64])
    subln = din("subln_g", [DEPTH, 128])
    norms = din("norms", [DEPTH, 32, 128])
    w_up = din("w_up", [DEPTH, D, 2 * DFF])
    conv_w = din("conv_w", [DEPTH, 132, 128])
    conv_b = din("conv_b", [DEPTH, 44, 128])
    w_down = din("w_down", [DEPTH, DFF, D])
    cst_in = din("cst", [128, NCST])
    y_out = nc.dram_tensor("y", [S, D], F32, kind="ExternalOutput").ap()

    def scr(name, shape, dt):
        return nc.dram_tensor(name, list(shape), dt).ap()

    QT = scr("QT", [58 * 64, S], BF16)
    VA_A = scr("VA_A", [S, 12 * 65], BF16)
    VA_B = scr("VA_B", [S, 4 * 65], BF16)
    VA_C = scr("VA_C", [S, 4 * 129], BF16)
    W8 = scr("W8", [S, 8], F32)
    GT = scr("GT", [3072, S], BF16)
    OT = scr("OT", [1024, S], BF16)
    MT = scr("MT", [NT, 128, NT, 128], BF16)
    X1 = scr("X1", [S, D], F32)
    X2 = scr("X2", [S, D], F32)
    QT3 = QT.rearrange("(n d) t -> d n t", d=64)

    with ExitStack() as semstack, ExitStack() as gstack:
        c = Ctx(nc, semstack)
        c.stack = gstack
        pe, act, dve, pool, sp = c.pe, c.act, c.dve, c.pool, c.sp
        pl = pool if _os.environ.get("K_POOL", "0") == "1" else dve

        cstf = c.sb("cstf", [128, NCST], F32)
        Bc = Buf()
        ident_b = c.sb("ident_b", [128, 128], BF16)
        maskA = c.sb("maskA", [128, NMASK, 128], BF16)
        cosT = c.sb("cosT", [128, NT, 8], F32)
        sinT = c.sb("sinT", [128, NT, 8], F32)
        pib = c.sb("pib", [128, 1], F32)
        Bg = Buf()
        ident_f = cstf[:, C_ID:C_ID + 128]
        negtri = cstf[:, C_NEG:C_NEG + 128]

        c.dma(sp, cstf[:], cst_in, writes=[Bc])
        c.op(dve, lambda e: e.tensor_copy(ident_b[:], cstf[:, C_ID:C_ID + 128]), reads=[Bc], adds=[Bg])
        c.op(dve, lambda e: e.tensor_copy(maskA[:].rearrange("p m q -> p (m q)"), cstf[:, C_MASK:C_MASK + NMASK * 128]),
             reads=[Bc], adds=[Bg])
        c.op(dve, lambda e: e.memset(pib[:], math.pi), adds=[Bg])

        def load_cols(dst, src_rows_ap, n, stack_tag):
            with ExitStack() as ls:
                old = c.stack
                c.stack = ls
                tmp = c.sb("lc_tmp", [128, 128], F32)
                pt = c.ps("lc_ps", [128, 128], F32)
                bt, bp = Buf(), Buf()
                c.dma(sp, tmp[0:n, :], src_rows_ap, writes=[bt])
                c.op(pe, lambda e: e.transpose(pt[:, 0:n], tmp[0:n, :], cstf[0:n, C_ID:C_ID + n]),
                     reads=[bt, Bc], writes=[bp])
                c.op(dve, lambda e: e.tensor_copy(dst, pt[:, 0:n]), reads=[bp], adds=[Bg])
                c.barrier()
                c.stack = old

        with ExitStack() as ls:
            c.stack = ls
            posi = c.sb("posi", [128, 128], I32)
            posf = c.sb("posf", [128, 128], F32)
            pt = c.ps("pos_ps", [128, 128], F32)
            posT = c.sb("posT", [128, NT], F32)
            ang = c.sb("ang", [128, NT, 8], F32)
            ang2 = c.sb("ang2", [128, NT, 8], F32)
            b1, b2, b3, b4, b5, b6 = (Buf() for _ in range(6))
            c.dma(sp, posi[0:NT, :], pos_in, writes=[b1])
            c.op(dve, lambda e: e.tensor_copy(posf[0:NT, :], posi[0:NT, :]), reads=[b1], writes=[b2])
            c.op(pe, lambda e: e.transpose(pt[:, 0:NT], posf[0:NT, :], cstf[0:NT, C_ID:C_ID + NT]),
                 reads=[b2, Bc], writes=[b3])
            c.op(dve, lambda e: e.tensor_copy(posT[:], pt[:, 0:NT]), reads=[b3], writes=[b4])
            for i in range(8):
                c.op(dve, lambda e: e.tensor_scalar(ang[:, :, i], posT[:], cstf[:, C_INV + i:C_INV + i + 1], None,
                                                    ALU.mult), reads=[b4, Bc], adds=[b5])
            TWO_PI = 2.0 * math.pi
            ki = c.sb("ki", [128, NT, 8], I32)
            kf = c.sb("kf", [128, NT, 8], F32)
            rr_ = c.sb("rr_", [128, NT, 8], F32)
            tt_ = c.sb("tt_", [128, NT, 8], F32)
            b7, b8, b9, b10 = (Buf() for _ in range(4))
            for (dst, shift) in ((sinT, 0.0), (cosT, math.pi / 2)):
                c.op(dve, lambda e: e.tensor_scalar(ang2[:], ang[:], shift, None, ALU.add), reads=[b5], writes=[b6])
                c.op(dve, lambda e: e.tensor_scalar(kf[:], ang2[:], 1.0 / TWO_PI, None, ALU.mult), reads=[b6], writes=[b8])
                c.op(dve, lambda e: e.tensor_copy(ki[:], kf[:]), reads=[b8], writes=[b7])
                c.op(dve, lambda e: e.tensor_copy(kf[:], ki[:]), reads=[b7], writes=[b8])
                c.op(dve, lambda e: e.scalar_tensor_tensor(rr_[:], kf[:], -TWO_PI, ang2[:], ALU.mult, ALU.add),
                     reads=[b8, b6], writes=[b9])
                c.op(dve, lambda e: e.tensor_scalar(tt_[:], rr_[:], math.pi, TWO_PI, ALU.is_gt, ALU.mult),
                     reads=[b9], writes=[b10])
                c.op(dve, lambda e: e.tensor_tensor(rr_[:], rr_[:], tt_[:], ALU.subtract), reads=[b10], upd=[b9])
                c.op(dve, lambda e: e.tensor_scalar(tt_[:], rr_[:], -math.pi, TWO_PI, ALU.is_lt, ALU.mult),
                     reads=[b9], upd=[b10])
                c.op(dve, lambda e: e.tensor_tensor(rr_[:], rr_[:], tt_[:], ALU.add), reads=[b10], upd=[b9])
                c.op(act, lambda e: e.activation(dst[:], rr_[:], AF.Sin), reads=[b9], adds=[Bg])
            c.barrier()
        c.stack = gstack

        gcols = c.sb("gcols", [128, DEPTH, 32], F32)
        cw = c.sb("cw", [128, DEPTH, 132], F32)
        cb = c.sb("cb", [128, DEPTH, 44], F32)
        lamt = c.sb("lamt", [128, DEPTH, 4], F32)
        for l in range(depth):
            load_cols(gcols[:, l, :], norms[l], 32, "g")
            load_cols(cw[:, l, 0:128], conv_w[l, 0:128, :], 128, "cw")
            load_cols(cw[:, l, 128:132], conv_w[l, 128:132, :], 4, "cw2")
            load_cols(cb[:, l, :], conv_b[l], 44, "cb")
        with ExitStack() as ls:
            c.stack = ls
            lt = c.sb("lt", [128, DEPTH, 4, 64], F32)
            pr = c.sb("pr", [128, DEPTH, 2, 64], F32)
            sm = c.sb("sm", [128, DEPTH, 2], F32)
            ex = c.sb("ex", [128, DEPTH, 2], F32)
            b1, b2, b3, b4 = (Buf() for _ in range(4))
            for l in range(DEPTH):
                c.dma(sp, lt[:, l, :, :].rearrange("p a b -> p (a b)"),
                      lam_in[l].rearrange("a b -> (a b)").partition_broadcast(128), adds=[b1])
            for l in range(DEPTH):
                for j in range(2):
                    c.op(dve, lambda e: e.tensor_tensor(pr[:, l, j, :], lt[:, l, 2 * j, :], lt[:, l, 2 * j + 1, :],
                                                        ALU.mult), reads=[b1], adds=[b2])
            c.op(dve, lambda e: e.tensor_reduce(sm[:].rearrange("p l j -> p (l j)"),
                                                pr[:].rearrange("p l j d -> p (l j) d"), AX.X, ALU.add),
                 reads=[b2], writes=[b3])
            c.op(act, lambda e: e.activation(ex[:], sm[:], AF.Exp), reads=[b3], writes=[b4])
            for l in range(DEPTH):
                lam_init = 0.8 - 0.6 * math.exp(-0.3 * l)
                c.op(dve, lambda e: e.tensor_tensor(lamt[:, l, 0:1], ex[:, l, 0:1], ex[:, l, 1:2], ALU.subtract),
                     reads=[b4], adds=[Bg])
                c.op(dve, lambda e: e.tensor_scalar(lamt[:, l, 0:1], lamt[:, l, 0:1], lam_init, None, ALU.add),
                     upd=[Bg])
                c.op(dve, lambda e: e.tensor_scalar(lamt[:, l, 1:2], lamt[:, l, 0:1], -1.0, None, ALU.mult),
                     upd=[Bg])
            c.barrier()
        c.stack = gstack
        c.barrier()

        def norm_transpose(st, xsrc_rows, xnT_dst, bdst, first_write):
            xt, bx = st["xr"].next()
            c.dma(sp, xt[:], xsrc_rows, writes=[bx])
            ss, bs = st["ss"].next()
            c.op(dve, lambda e: e.memset(ss[:], 0.0), writes=[bs])
            jk, bj = st["junk"].next()
            c.op(act, lambda e: e.activation(jk[:], xt[:], AF.Square, accum_out=ss[:, 0:1]),
                 reads=[bx], writes=[bj], upd=[bs])
            c.op(dve, lambda e: e.tensor_scalar(ss[:, 1:2], ss[:, 0:1], 1.0 / D, EPS, ALU.mult, ALU.add), upd=[bs])
            c.op(act, lambda e: e.activation(ss[:, 2:3], ss[:, 1:2], AF.Sqrt), upd=[bs])
            c.op(dve, lambda e: e.reciprocal(ss[:, 2:3], ss[:, 2:3]), upd=[bs])
            xb, bxb = st["xb"].next()
            c.op(dve, lambda e: e.tensor_scalar(xb[:], xt[:], ss[:, 2:3], None, ALU.mult),
                 reads=[bx, bs], writes=[bxb])
            pt, bpt = st["pT"].next()
            for kc in range(8):
                c.op(pe, lambda e: e.transpose(pt[:, kc, :], xb[:, kc * 128:(kc + 1) * 128], ident_b[:]),
                     reads=[bxb, Bg], **({"writes": [bpt]} if kc == 0 else {"adds": [bpt]}))
            c.op(act, lambda e: e.activation(xnT_dst, pt[:], AF.Copy), reads=[bpt], writes=[bdst])
            return xt, bx

        def load_w(st, dst_bf, bdst, src_ap, nk, ncols, gain_ap=None):
            cap = st["wst"].t[0].shape[1]
            for k0 in range(0, nk, cap):
                kn = min(cap, nk - k0)
                ws, bws = st["wst"].next()
                c.dma(sp, ws[:, 0:kn, 0:ncols], src_ap[:, k0:k0 + kn, :], writes=[bws])
                for kk in range(kn):
                    kc = k0 + kk
                    if gain_ap is not None:
                        c.op(act, lambda e: e.activation(dst_bf[:, kc, 0:ncols], ws[:, kk, 0:ncols], AF.Identity,
                                                         scale=gain_ap[:, kc:kc + 1]),
                             reads=[bws, Bg], **({"writes": [bdst]} if kc == 0 else {"adds": [bdst]}))
                    else:
                        c.op(act, lambda e: e.activation(dst_bf[:, kc, 0:ncols], ws[:, kk, 0:ncols], AF.Copy),
                             reads=[bws], **({"writes": [bdst]} if kc == 0 else {"adds": [bdst]}))

        def phase1(l, xin):
            TCH = min(S, 2048)
            NCH = S // TCH
            NTT = TCH // 128
            with ExitStack() as ps_:
                c.stack = ps_
                st = {
                    "xr": Ring(c, "xr", 2, [128, D], F32),
                    "ss": Ring(c, "ss", 2, [128, 4], F32),
                    "junk": Ring(c, "junk", 1, [128, D], BF16),
                    "xb": Ring(c, "xb", 2, [128, D], BF16),
                    "pT": Ring(c, "pT", 2, [128, 8, 128], BF16, psum=True),
                    "wst": Ring(c, "wst", 2, [128, 8, 512], F32),
                }
                wbr = Ring(c, "wb", 2, [128, 8, 512], BF16)
                pM = Ring(c, "pM", 2, [128, 512], F32, psum=True)
                pQ = Ring(c, "pQ", 2, [128, 4, 128], BF16, psum=True)
                xnT = c.sb("xnT", [128, 8, TCH], BF16)
                bxn = [Buf() for _ in range(NTT)]
                qbr = Ring(c, "qb", 2, [128, 512], BF16)
                tmps = [Ring(c, f"rt{i}", 2, [128, 8, 8], F32) for i in range(4)]
                rpr = Ring(c, "rp", 2, [128, 8, 16], F32)
                cos8 = c.sb("cos8", [128, NT, 8, 8], F32)
                sin8 = c.sb("sin8", [128, NT, 8, 8], F32)
                b88 = Buf()
                for (d8, src8) in ((cos8, cosT), (sin8, sinT)):
                    for hh_ in range(8):
                        c.op(dve, lambda e: e.tensor_copy(d8[:, :, hh_, :], src8[:]), reads=[Bg], adds=[b88])
                qsr = Ring(c, "qs", 3, [128, 4, 128], BF16)
                vsa = Ring(c, "vsa", 3, [128, 4, 65], BF16)
                vsc = Ring(c, "vsc", 3, [128, 4, 129], BF16)
                gsr = Ring(c, "gs", 2, [128, 4, 512], BF16)
                w8r = Ring(c, "w8", 3, [128, 8], F32)
                for r in (vsa, vsc):
                    for t, b in zip(r.t, r.b):
                        c.op(dve, lambda e: e.memset(t[:], 1.0), writes=[b])
                w_l = w_in[l].rearrange("(kc p) n -> p kc n", p=128)
                gain = gcols[:, l, 0:8]

                tiles = []
                for g in range(3):
                    tiles.append(("qk", g * 768, 8, g * 8))
                    tiles.append(("v", g * 768 + 512, 256, VA_A, g * 4 * 65, 64))
                tiles.append(("qk", 2304, 8, 24))
                tiles.append(("v", 2816, 256, VA_B, 0, 64))
                tiles.append(("qk", 3072, 8, 32))
                tiles.append(("kw", 3584, 72))
                tiles.append(("qk", 3656, 8, 41))
                tiles.append(("qk", 4168, 8, 49))
                tiles.append(("v", 4680, 512, VA_C, 0, 128))
                for i in range(6):
                    tiles.append(("gate", 5192 + i * 512, 512, i * 512))

                def rope_tile(pm, bpm, qbt, bqb, nh, tglob):
                    pmv = pm[:, 0:nh * 64].rearrange("p (h d) -> p h d", d=64)
                    qbv = qbt[:, 0:nh * 64].rearrange("p (h d) -> p h d", d=64)
                    c.op(act, lambda e: e.activation(qbv[:, :, 16:64], pmv[:, :, 16:64], AF.Copy),
                         reads=[bpm], writes=[bqb])
                    if _os.environ.get("K_NOROPE"):
                        c.op(act, lambda e: e.activation(qbv[:, :, 0:16], pmv[:, :, 0:16], AF.Copy), reads=[bpm], adds=[bqb])
                        return
                    cosb = cos8[:, tglob, 0:nh, :]
                    sinb = sin8[:, tglob, 0:nh, :]
                    (t1, bt1), (t2, bt2), (t3, bt3), (t4, bt4) = [r.next() for r in tmps]
                    rp, brp = rpr.next()
                    c.op(act, lambda e: e.activation(rp[:, 0:nh, :], pmv[:, :, 0:16], AF.Copy), reads=[bpm], writes=[brp])
                    x1 = rp[:, 0:nh, 0:8]
                    x2 = rp[:, 0:nh, 8:16]
                    c.op(dve, lambda e: e.tensor_tensor(t1[:, 0:nh, :], x1, cosb, ALU.mult), reads=[brp, b88], writes=[bt1])
                    c.op(dve, lambda e: e.tensor_tensor(t2[:, 0:nh, :], x2, sinb, ALU.mult), reads=[brp, b88], writes=[bt2])
                    c.op(dve, lambda e: e.tensor_tensor(qbv[:, :, 0:8], t1[:, 0:nh, :], t2[:, 0:nh, :], ALU.subtract),
                         reads=[bt1, bt2], adds=[bqb])
                    c.op(dve, lambda e: e.tensor_tensor(t3[:, 0:nh, :], x2, cosb, ALU.mult), reads=[brp, b88], writes=[bt3])
                    c.op(dve, lambda e: e.tensor_tensor(t4[:, 0:nh, :], x1, sinb, ALU.mult), reads=[brp, b88], writes=[bt4])
                    c.op(dve, lambda e: e.tensor_tensor(qbv[:, :, 8:16], t3[:, 0:nh, :], t4[:, 0:nh, :], ALU.add),
                         reads=[bt3, bt4], adds=[bqb])

                for ch in range(NCH):
                    for tt in range(NTT):
                        t0 = ch * TCH + tt * 128
                        norm_transpose(st, xin[t0:t0 + 128, :], xnT[:, :, tt * 128:(tt + 1) * 128], bxn[tt], True)
                    import os as _os
                    _kinds = _os.environ.get("K_KINDS", "qk,v,kw,gate").split(",")
                    for tl in tiles:
                        kind, col0 = tl[0], tl[1]
                        if kind not in _kinds:
                            continue
                        ncols = 512 if kind in ("qk", "gate") else tl[2]
                        wb, bwb = wbr.next()
                        load_w(st, wb, bwb, w_l[:, :, col0:col0 + ncols], 8, ncols, gain)
                        if kind == "gate":
                            row0 = tl[3]
                            for tg in range(TCH // 512):
                                gs, bgs = gsr.next()
                                for cbk in range(4):
                                    pm, bpm = pM.next()
                                    for kc in range(8):
                                        c.op(pe, lambda e: e.matmul(pm[:], wb[:, kc, cbk * 128:(cbk + 1) * 128],
                                                                    xnT[:, kc, tg * 512:(tg + 1) * 512],
                                                                    start=(kc == 0), stop=(kc == 7)),
                                             reads=[bwb] + bxn[tg * 4:(tg + 1) * 4],
                                             **({"writes": [bpm]} if kc == 0 else {"adds": [bpm]}))
                                    c.op(act, lambda e: e.activation(gs[:, cbk, :], pm[:], AF.Sigmoid), reads=[bpm],
                                         **({"writes": [bgs]} if cbk == 0 else {"adds": [bgs]}))
                                tg0 = ch * TCH + tg * 512
                                c.dma(pool, GT[row0:row0 + 512, tg0:tg0 + 512].rearrange("(b p) t -> p b t", p=128),
                                      gs[:], reads=[bgs])
                            continue
                        for tt in range(NTT):
                            t0 = ch * TCH + tt * 128
                            tglob = t0 // 128
                            pm, bpm = pM.next()
                            for kc in range(8):
                                c.op(pe, lambda e: e.matmul(pm[:, 0:ncols], xnT[:, kc, tt * 128:(tt + 1) * 128],
                                                            wb[:, kc, 0:ncols], start=(kc == 0), stop=(kc == 7)),
                                     reads=[bwb, bxn[tt]], **({"writes": [bpm]} if kc == 0 else {"adds": [bpm]}))
                            if kind == "v":
                                dst, coff, hd = tl[3], tl[4], tl[5]
                                vs, bvs = (vsa if hd == 64 else vsc).next()
                                c.op(act, lambda e: e.activation(vs[:, :, 0:hd],
                                                                 pm[:, 0:ncols].rearrange("p (h d) -> p h d", d=hd),
                                                                 AF.Copy), reads=[bpm], writes=[bvs])
                                c.dma(pool, dst[t0:t0 + 128, coff:coff + 4 * (hd + 1)],
                                      vs[:].rearrange("p h d -> p (h d)"), reads=[bvs])
                                continue
                            nh = 8 if kind == "qk" else 1
                            qbt, bqb = qbr.next()
                            rope_tile(pm, bpm, qbt, bqb, nh, tglob)
                            pq, bpq = pQ.next()
                            qs, bqs = qsr.next()
                            if kind == "qk":
                                head0 = tl[3]
                                for blk in range(4):
                                    c.op(pe, lambda e: e.transpose(pq[:, blk, :], qbt[:, blk * 128:(blk + 1) * 128],
                                                                   ident_b[:]),
                                         reads=[bqb, Bg], **({"writes": [bpq]} if blk == 0 else {"adds": [bpq]}))
                                c.op(dve, lambda e: e.tensor_copy(qs[:], pq[:]), reads=[bpq], writes=[bqs])
                                c.dma(pool, QT[head0 * 64:head0 * 64 + 512, t0:t0 + 128].rearrange("(b p) t -> p b t", p=128),
                                      qs[:], reads=[bqs])
                            else:
                                c.op(pe, lambda e: e.transpose(pq[0:64, 0, :], qbt[:, 0:64], ident_b[:]),
                                     reads=[bqb, Bg], writes=[bpq])
                                c.op(dve, lambda e: e.tensor_copy(qs[0:64, 0, :], pq[0:64, 0, :]), reads=[bpq], writes=[bqs])
                                c.dma(pool, QT[40 * 64:41 * 64, t0:t0 + 128], qs[0:64, 0, :], reads=[bqs])
                                w8, bw8 = w8r.next()
                                c.op(act, lambda e: e.mul(w8[:], pm[:, 64:72], 8.0 ** -0.5), reads=[bpm], writes=[bw8])
                                c.dma(pool, W8[t0:t0 + 128, :], w8[:], reads=[bw8])
                c.barrier()
            c.stack = gstack

        def phaseA(l):
            with ExitStack() as ps_:
                c.stack = ps_
                kt = c.sb("a_kt", [64, 12, NSLOT * 128], BF16)
                va = c.sb("a_va", [128, NSLOT, 12 * 65], BF16)
                bk = [Buf() for _ in range(NSLOT)]
                bv = [Buf() for _ in range(NSLOT)]
                qtr = Ring(c, "a_qt", 2, [64, 12, 128], BF16)
                er = Ring(c, "a_e", 5, [128, 512], BF16)
                pr_ = Ring(c, "a_p", 5, [128, 512], BF16)
                psr = Ring(c, "a_ps", 4, [128, 512], F32, psum=True)
                accr = Ring(c, "a_acc", 2, [128, 4, 65], F32, psum=True)
                por = Ring(c, "a_po", 2, [128, 2, 128], BF16, psum=True)
                rcr = Ring(c, "a_rc", 2, [128, 4], F32)
                obr = Ring(c, "a_ob", 2, [128, 256], BF16)
                osr = Ring(c, "a_os", 2, [128, 2, 128], BF16)
                for qb in range(NT):
                    slot = qb % NSLOT
                    tok0 = qb * 128
                    qt, bq = qtr.next()
                    for g in range(3):
                        c.dma(sp, kt[:, g * 4:(g + 1) * 4, slot * 128:(slot + 1) * 128],
                              QT3[:, g * 8 + 4:g * 8 + 8, tok0:tok0 + 128],
                              **({"writes": [bk[slot]]} if g == 0 else {"adds": [bk[slot]]}))
                        c.dma(sp, qt[:, g * 4:(g + 1) * 4, :], QT3[:, g * 8:g * 8 + 4, tok0:tok0 + 128],
                              **({"writes": [bq]} if g == 0 else {"adds": [bq]}))
                    c.dma(sp, va[:, slot, :], VA_A[tok0:tok0 + 128, :], writes=[bv[slot]])
                    acc, bacc = accr.next()
                    c.op(dve, lambda e: e.memset(acc[:], 0.0), writes=[bacc])
                    pairs = [(g, kb) for g in range(3) for kb in range(max(0, qb - A_WB[g]), qb + 1)]

                    def a_qk(i):
                        g, kb = pairs[i]
                        ks = kb % NSLOT
                        ps, bps = psr.next()
                        for h in range(4):
                            c.op(pe, lambda e: e.matmul(ps[:, h * 128:(h + 1) * 128],
                                                        kt[:, g * 4 + h, ks * 128:(ks + 1) * 128], qt[:, g * 4 + h, :],
                                                        start=True, stop=True),
                                 reads=[bk[ks], bq], **({"writes": [bps]} if h == 0 else {"adds": [bps]}))
                        return ps, bps

                    def a_mid(i, ps, bps):
                        g, kb = pairs[i]
                        ee, be = er.next()
                        c.op(act, lambda e: e.activation(ee[:], ps[:], AF.Exp, scale=0.125), reads=[bps], writes=[be])
                        pp, bp = pr_.next()
                        mk = maskA[:, mask_index(g, qb - kb), :]
                        for h in range(4):
                            c.op(dve, lambda e: e.tensor_tensor(pp[:, h * 128:(h + 1) * 128], ee[:, h * 128:(h + 1) * 128],
                                                                mk, ALU.mult),
                                 reads=[be, Bg], **({"writes": [bp]} if h == 0 else {"adds": [bp]}))
                        return pp, bp

                    def a_pv(i, pp, bp):
                        g, kb = pairs[i]
                        ks = kb % NSLOT
                        for h in range(4):
                            c.op(pe, lambda e: e.matmul(acc[:, h, :], pp[:, h * 128:(h + 1) * 128],
                                                        va[:, ks, (g * 4 + h) * 65:(g * 4 + h + 1) * 65],
                                                        start=False, stop=False, skip_group_check=True),
                                 reads=[bp, bv[ks]], upd=[bacc])

                    LA = 3
                    pend = []
                    for i in range(len(pairs)):
                        ps, bps = a_qk(i)
                        pend.append((i,) + a_mid(i, ps, bps))
                        if len(pend) > LA:
                            a_pv(*pend.pop(0))
                    for it in pend:
                        a_pv(*it)
                    rc, brc = rcr.next()
                    c.op(dve, lambda e: e.reciprocal(rc[:], acc[:, :, 64]), reads=[bacc], writes=[brc])
                    ob, bob = obr.next()
                    for h in range(4):
                        c.op(dve, lambda e: e.tensor_scalar(ob[:, h * 64:(h + 1) * 64], acc[:, h, 0:64], rc[:, h:h + 1],
                                                            None, ALU.mult),
                             reads=[bacc, brc], **({"writes": [bob]} if h == 0 else {"adds": [bob]}))
                    po, bpo = por.next()
                    for j in range(2):
                        c.op(pe, lambda e: e.transpose(po[:, j, :], ob[:, j * 128:(j + 1) * 128], ident_b[:]),
                             reads=[bob, Bg], **({"writes": [bpo]} if j == 0 else {"adds": [bpo]}))
                    os_, bos = osr.next()
                    c.op(act, lambda e: e.activation(os_[:], po[:], AF.Copy), reads=[bpo], writes=[bos])
                    c.dma(pool, OT[0:256, tok0:tok0 + 128].rearrange("(b p) t -> p b t", p=128), os_[:], reads=[bos])
                c.barrier()
            c.stack = gstack

        def phaseB1(l):
            with ExitStack() as ps_:
                c.stack = ps_
                kx = c.sb("b_kx", [64, S], BF16)
                bkx = Buf()
                c.dma(sp, kx[:], QT3[:, 40, :], writes=[bkx])
                qxr = Ring(c, "b_qx", 2, [64, 8, 128], BF16)
                wr = Ring(c, "b_w", 2, [128, 8], F32)
                dgr = Ring(c, "b_dg", 2, [128, 8, 128], BF16)
                Ir = Ring(c, "b_I", 2, [128, S], F32)
                rr = Ring(c, "b_r", 4, [128, 512], BF16)
                psr = Ring(c, "b_ps", 4, [128, 512], F32, psum=True)
                pacc = Ring(c, "b_pa", 2, [128, 512], F32, psum=True)
                Mr = Ring(c, "b_M", 2, [128, S], BF16)
                scr_ = Ring(c, "b_sc", 2, [128, 8], F32)
                hsr = Ring(c, "b_hs", 2, [128, NIT], F32)
                ptr = Ring(c, "b_pt", 2, [128, 4, 128], BF16, psum=True)
                mtr = Ring(c, "b_mt", 1, [128, NT, 128], BF16)
                def b1_finish(qb, M, bM):
                        mt, bmt = mtr.next()
                        nkb = qb + 1
                        for k0 in range(0, nkb, 4):
                            kn = min(4, nkb - k0)
                            pt, bpt = ptr.next()
                            for j in range(kn):
                                c.op(pe, lambda e: e.transpose(pt[:, j, :], M[:, (k0 + j) * 128:(k0 + j + 1) * 128], ident_b[:]),
                                     reads=[bM, Bg], **({"writes": [bpt]} if j == 0 else {"adds": [bpt]}))
                            c.op(act, lambda e: e.activation(mt[:, k0:k0 + kn, :], pt[:, 0:kn, :], AF.Copy), reads=[bpt],
                                 **({"writes": [bmt]} if k0 == 0 else {"adds": [bmt]}))
                        c.dma(pool, MT[qb, :, 0:nkb, :], mt[:, 0:nkb, :], reads=[bmt])

                prev_fin = None
                for qb in range(NT):
                    tok0 = qb * 128
                    nv = tok0 + 128
                    qx, bqx = qxr.next()
                    c.dma(sp, qx[:], QT3[:, 32:40, tok0:tok0 + 128], writes=[bqx])
                    w, bw = wr.next()
                    c.dma(sp, w[:], W8[tok0:tok0 + 128, :], writes=[bw])
                    dg, bdg = dgr.next()
                    for h in range(8):
                        c.op(dve, lambda e: e.tensor_scalar(dg[:, h, :], cstf[:, C_ID:C_ID + 128], w[:, h:h + 1], None, ALU.mult),
                             reads=[bw, Bc], **({"writes": [bdg]} if h == 0 else {"adds": [bdg]}))
                    I_, bI = Ir.next()
                    nst = (nv + 511) // 512
                    firstI = True
                    for s_t in range(nst):
                        s0 = s_t * 512
                        wd = min(512, nv - s0)
                        pa, bpa = pacc.next()

                        def lg(h):
                            ps, bps = psr.next()
                            c.op(pe, lambda e: e.matmul(ps[:, 0:wd], qx[:, h, :], kx[:, s0:s0 + wd], start=True, stop=True),
                                 reads=[bqx, bkx], writes=[bps])
                            r, br = rr.next()
                            c.op(act, lambda e: e.activation(r[:, 0:wd], ps[:, 0:wd], AF.Relu, scale=0.125),
                                 reads=[bps], writes=[br])
                            return r, br

                        def dgm(h, r, br):
                            c.op(pe, lambda e: e.matmul(pa[:, 0:wd], dg[:, h, :], r[:, 0:wd], start=(h == 0), stop=(h == 7)),
                                 reads=[bdg, br], **({"writes": [bpa]} if h == 0 else {"adds": [bpa]}))

                        pend = []
                        for h in range(8):
                            pend.append((h,) + lg(h))
                            if len(pend) > 2:
                                dgm(*pend.pop(0))
                        for it in pend:
                            dgm(*it)
                        c.op(act, lambda e: e.activation(I_[:, s0:s0 + wd], pa[:, 0:wd], AF.Copy), reads=[bpa],
                             **({"writes": [bI]} if firstI else {"adds": [bI]}))
                        firstI = False
                    M, bM = Mr.next()
                    sc, bsc = scr_.next()
                    hs, bhs = hsr.next()
                    if qb >= 2:
                        c.op(dve, lambda e: e.tensor_reduce(sc[:, 0:1], I_[:, 0:nv], AX.X, ALU.max,
                                                            apply_absolute_value=True), reads=[bI], writes=[bsc])
                    c.op(dve, lambda e: e.tensor_tensor(I_[:, tok0:nv], I_[:, tok0:nv], negtri, ALU.add),
                         reads=[Bc], upd=[bI])
                    if qb < 2:
                        c.op(dve, lambda e: e.tensor_scalar(M[:, 0:nv], I_[:, 0:nv], -1e29, None, ALU.is_ge),
                             reads=[bI], writes=[bM])
                    else:
                        c.op(dve, lambda e: e.tensor_scalar(sc[:, 0:1], sc[:, 0:1], 1.0, 1e-3, ALU.mult, ALU.add), upd=[bsc])
                        c.op(dve, lambda e: e.tensor_scalar(sc[:, 1:2], sc[:, 0:1], -1.0, None, ALU.mult), upd=[bsc])
                        c.op(dve, lambda e: e.tensor_scalar(sc[:, 4:5], sc[:, 0:1], 2.0, None, ALU.mult), upd=[bsc])
                        c.op(dve, lambda e: e.tensor_scalar(hs[:], cstf[:, C_POW:C_POW + NIT], sc[:, 4:5], None, ALU.mult),
                             reads=[bsc, Bc], writes=[bhs])
                        for k in range(NIT):
                            c.op(dve, lambda e: e.tensor_tensor(sc[:, 2:3], sc[:, 1:2], hs[:, k:k + 1], ALU.add),
                                 reads=[bhs], upd=[bsc])
                            c.op(dve, lambda e: e.tensor_scalar(M[:, 0:nv], I_[:, 0:nv], sc[:, 2:3], None, ALU.is_ge,
                                                                ALU.add, accum_out=sc[:, 3:4]),
                                 reads=[bI], writes=[bM], upd=[bsc])
                            c.op(dve, lambda e: e.scalar_tensor_tensor(sc[:, 4:5], sc[:, 3:4], TOPK - 0.5, hs[:, k:k + 1],
                                                                       ALU.is_ge, ALU.mult), reads=[bhs], upd=[bsc])
                            c.op(dve, lambda e: e.tensor_tensor(sc[:, 1:2], sc[:, 1:2], sc[:, 4:5], ALU.add), upd=[bsc])
                        c.op(dve, lambda e: e.tensor_scalar(M[:, 0:nv], I_[:, 0:nv], sc[:, 1:2], None, ALU.is_ge),
                             reads=[bI, bsc], writes=[bM])
                    if prev_fin is not None:
                        b1_finish(*prev_fin)
                    prev_fin = (qb, M, bM)
                b1_finish(*prev_fin)
                c.barrier()
            c.stack = gstack

        def phaseB2(l):
            with ExitStack() as ps_:
                c.stack = ps_
                kt = c.sb("b2_kt", [64, 4, S], BF16)
                va = c.sb("b2_va", [128, NT, 4 * 65], BF16)
                bkt, bva = Buf(), Buf()
                c.dma(sp, kt[:], QT3[:, 28:32, :], writes=[bkt])
                vbv = VA_B.rearrange("(n p) c -> p n c", p=128)
                for n0 in range(0, NT, 8):
                    c.dma(sp, va[:, n0:n0 + 8, :], vbv[:, n0:n0 + 8, :], **({"writes": [bva]} if n0 == 0 else {"adds": [bva]}))
                qtr = Ring(c, "b2_qt", 2, [64, 4, 128], BF16)
                mtr = Ring(c, "b2_mt", 2, [128, NT, 128], BF16)
                er = Ring(c, "b2_e", 5, [128, 512], BF16)
                pr_ = Ring(c, "b2_p", 5, [128, 512], BF16)
                psr = Ring(c, "b2_ps", 4, [128, 512], F32, psum=True)
                accr = Ring(c, "b2_acc", 2, [128, 4, 65], F32, psum=True)
                por = Ring(c, "b2_po", 2, [128, 2, 128], BF16, psum=True)
                rcr = Ring(c, "b2_rc", 2, [128, 4], F32)
                obr = Ring(c, "b2_ob", 2, [128, 256], BF16)
                osr = Ring(c, "b2_os", 2, [128, 2, 128], BF16)
                for qb in range(NT):
                    tok0 = qb * 128
                    nkb = qb + 1
                    qt, bq = qtr.next()
                    c.dma(sp, qt[:], QT3[:, 24:28, tok0:tok0 + 128], writes=[bq])
                    mt, bmt = mtr.next()
                    c.dma(sp, mt[:, 0:nkb, :], MT[qb, :, 0:nkb, :], writes=[bmt])
                    acc, bacc = accr.next()
                    c.op(dve, lambda e: e.memset(acc[:], 0.0), writes=[bacc])
                    def b_qk(kb):
                        ps, bps = psr.next()
                        for h in range(4):
                            c.op(pe, lambda e: e.matmul(ps[:, h * 128:(h + 1) * 128], kt[:, h, kb * 128:(kb + 1) * 128],
                                                        qt[:, h, :], start=True, stop=True),
                                 reads=[bkt, bq], **({"writes": [bps]} if h == 0 else {"adds": [bps]}))
                        return ps, bps

                    def b_mid(kb, ps, bps):
                        ee, be = er.next()
                        c.op(act, lambda e: e.activation(ee[:], ps[:], AF.Exp, scale=0.125), reads=[bps], writes=[be])
                        pp, bp = pr_.next()
                        mk = mt[:, kb, :]
                        for h in range(4):
                            c.op(dve, lambda e: e.tensor_tensor(pp[:, h * 128:(h + 1) * 128], ee[:, h * 128:(h + 1) * 128],
                                                                mk, ALU.mult),
                                 reads=[be, bmt], **({"writes": [bp]} if h == 0 else {"adds": [bp]}))
                        return pp, bp

                    def b_pv(kb, pp, bp):
                        for h in range(4):
                            c.op(pe, lambda e: e.matmul(acc[:, h, :], pp[:, h * 128:(h + 1) * 128],
                                                        va[:, kb, h * 65:(h + 1) * 65],
                                                        start=False, stop=False, skip_group_check=True),
                                 reads=[bp, bva], upd=[bacc])

                    LA = 3
                    pend = []
                    for kb in range(nkb):
                        ps, bps = b_qk(kb)
                        pend.append((kb,) + b_mid(kb, ps, bps))
                        if len(pend) > LA:
                            b_pv(*pend.pop(0))
                    for it in pend:
                        b_pv(*it)
                    rc, brc = rcr.next()
                    c.op(dve, lambda e: e.reciprocal(rc[:], acc[:, :, 64]), reads=[bacc], writes=[brc])
                    ob, bob = obr.next()
                    for h in range(4):
                        c.op(dve, lambda e: e.tensor_scalar(ob[:, h * 64:(h + 1) * 64], acc[:, h, 0:64], rc[:, h:h + 1],
                                                            None, ALU.mult),
                             reads=[bacc, brc], **({"writes": [bob]} if h == 0 else {"adds": [bob]}))
                    po, bpo = por.next()
                    for j in range(2):
                        c.op(pe, lambda e: e.transpose(po[:, j, :], ob[:, j * 128:(j + 1) * 128], ident_b[:]),
                             reads=[bob, Bg], **({"writes": [bpo]} if j == 0 else {"adds": [bpo]}))
                    os_, bos = osr.next()
                    c.op(act, lambda e: e.activation(os_[:], po[:], AF.Copy), reads=[bpo], writes=[bos])
                    c.dma(pool, OT[256:512, tok0:tok0 + 128].rearrange("(b p) t -> p b t", p=128), os_[:], reads=[bos])
                c.barrier()
            c.stack = gstack

        def phaseC(l):
            lam_init = 0.8 - 0.6 * math.exp(-0.3 * l)
            with ExitStack() as ps_:
                c.stack = ps_
                ktr = Ring(c, "c_kt", 2, [64, 2, S], BF16)
                var = Ring(c, "c_va", 2, [128, NT, 129], BF16)
                qtr = Ring(c, "c_qt", 2, [64, 2, 512], BF16)
                er = Ring(c, "c_e", 4, [128, 512], BF16)
                psr = Ring(c, "c_ps", 3, [128, 512], F32, psum=True)
                accs = [c.ps(f"c_acc{i}", [128, 3, 129], F32) for i in range(3)]
                bacc = Buf()
                por = Ring(c, "c_po", 2, [128, 128], BF16, psum=True)
                sg = c.sb("c_sg", [128, 128], F32)
                bsg = Buf()
                c.dma(sp, sg[:], subln[l].partition_broadcast(128), writes=[bsg])
                tri = maskA[:, 0, :]
                smr = Ring(c, "c_sm", 2, [128, 8], F32)
                o1r = Ring(c, "c_o1", 2, [128, 128], F32)
                o2r = Ring(c, "c_o2", 2, [128, 128], F32)
                jr = Ring(c, "c_j", 2, [128, 128], F32)
                obr = Ring(c, "c_ob", 2, [128, 128], BF16)
                osr = Ring(c, "c_os", 2, [128, 512], BF16)

                def accv(cc, j):
                    i = cc * 4 + j
                    return accs[i // 3][:, i % 3, :]

                for h in range(4):
                    kt, bkt = ktr.next()
                    va, bva = var.next()
                    c.dma(sp, kt[:], QT3[:, 49 + 2 * h:51 + 2 * h, :], writes=[bkt])
                    vcv = VA_C[:, h * 129:(h + 1) * 129].rearrange("(n p) c -> p n c", p=128)
                    for n0 in range(0, NT, 8):
                        c.dma(sp, va[:, n0:n0 + 8, :], vcv[:, n0:n0 + 8, :], **({"writes": [bva]} if n0 == 0 else {"adds": [bva]}))
                    for qt_i in range(S // 512):
                        q0 = qt_i * 512
                        qt, bq = qtr.next()
                        c.dma(sp, qt[:], QT3[:, 41 + 2 * h:43 + 2 * h, q0:q0 + 512], writes=[bq])
                        for a in accs:
                            c.op(dve, lambda e: e.memset(a[:], 0.0), **({"writes": [bacc]} if a is accs[0] else {"upd": [bacc]}))
                        nkb = (q0 + 512) // 128
                        steps = [(kb, cc) for kb in range(nkb) for cc in range(2)]

                        def c_qk(kb, cc):
                            ps, bps = psr.next()
                            c.op(pe, lambda e: e.matmul(ps[:], kt[:, cc, kb * 128:(kb + 1) * 128], qt[:, cc, :],
                                                        start=True, stop=True), reads=[bkt, bq], writes=[bps])
                            return ps, bps

                        def c_mid(kb, cc, ps, bps):
                            jk = kb - q0 // 128
                            ee, be = er.next()
                            c.op(act, lambda e: e.activation(ee[:], ps[:], AF.Exp, scale=0.125), reads=[bps], writes=[be])
                            if jk >= 0:
                                c.op(dve, lambda e: e.tensor_tensor(ee[:, jk * 128:(jk + 1) * 128],
                                                                    ee[:, jk * 128:(jk + 1) * 128], tri, ALU.mult),
                                     reads=[Bg], upd=[be])
                            return ee, be

                        def c_pv(kb, cc, ee, be):
                            jk = kb - q0 // 128
                            for j in range(max(jk, 0), 4):
                                c.op(pe, lambda e: e.matmul(accv(cc, j), ee[:, j * 128:(j + 1) * 128], va[:, kb, :],
                                                            start=False, stop=False, skip_group_check=True),
                                     reads=[be, bva], upd=[bacc])

                        LA = 2
                        pend = []
                        for (kb, cc) in steps:
                            ps, bps = c_qk(kb, cc)
                            pend.append((kb, cc) + c_mid(kb, cc, ps, bps))
                            if len(pend) > LA:
                                c_pv(*pend.pop(0))
                        for it in pend:
                            c_pv(*it)
                        os_, bos = osr.next()
                        for j in range(4):
                            sm, bsm = smr.next()
                            a1 = accv(0, j)
                            a2 = accv(1, j)
                            c.op(dve, lambda e: e.reciprocal(sm[:, 0:1], a1[:, 128:129]), reads=[bacc], writes=[bsm])
                            c.op(dve, lambda e: e.reciprocal(sm[:, 1:2], a2[:, 128:129]), reads=[bacc], upd=[bsm])
                            c.op(dve, lambda e: e.tensor_tensor(sm[:, 1:2], sm[:, 1:2], lamt[:, l, 1:2], ALU.mult),
                                 reads=[Bg], upd=[bsm])
                            o1, bo1 = o1r.next()
                            c.op(dve, lambda e: e.tensor_scalar(o1[:], a1[:, 0:128], sm[:, 0:1], None, ALU.mult),
                                 reads=[bacc, bsm], writes=[bo1])
                            o2, bo2 = o2r.next()
                            c.op(dve, lambda e: e.scalar_tensor_tensor(o2[:], a2[:, 0:128], sm[:, 1:2], o1[:],
                                                                       ALU.mult, ALU.add),
                                 reads=[bacc, bsm, bo1], writes=[bo2])
                            c.op(dve, lambda e: e.memset(sm[:, 2:3], 0.0), upd=[bsm])
                            jj, bjj = jr.next()
                            c.op(act, lambda e: e.activation(jj[:], o2[:], AF.Square, accum_out=sm[:, 2:3]),
                                 reads=[bo2], writes=[bjj], upd=[bsm])
                            c.op(dve, lambda e: e.tensor_scalar(sm[:, 3:4], sm[:, 2:3], 1.0 / 128, EPS, ALU.mult, ALU.add),
                                 upd=[bsm])
                            c.op(act, lambda e: e.activation(sm[:, 4:5], sm[:, 3:4], AF.Sqrt), upd=[bsm])
                            c.op(dve, lambda e: e.reciprocal(sm[:, 4:5], sm[:, 4:5]), upd=[bsm])
                            c.op(dve, lambda e: e.tensor_scalar(sm[:, 4:5], sm[:, 4:5], 1.0 - lam_init, None, ALU.mult),
                                 upd=[bsm])
                            ob, bob = obr.next()
                            c.op(dve, lambda e: e.scalar_tensor_tensor(ob[:], o2[:], sm[:, 4:5], sg[:], ALU.mult, ALU.mult),
                                 reads=[bo2, bsm, bsg], writes=[bob])
                            po, bpo = por.next()
                            c.op(pe, lambda e: e.transpose(po[:], ob[:], ident_b[:]), reads=[bob, Bg], writes=[bpo])
                            c.op(act, lambda e: e.activation(os_[:, j * 128:(j + 1) * 128], po[:], AF.Copy), reads=[bpo],
                                 **({"writes": [bos]} if j == 0 else {"adds": [bos]}))
                        c.dma(pool, OT[512 + h * 128:512 + (h + 1) * 128, q0:q0 + 512], os_[:], reads=[bos])
                c.barrier()
            c.stack = gstack

        def bcast_load(dst, src_vec_ap, bdst):
            c.dma(sp, dst, src_vec_ap.partition_broadcast(128), writes=[bdst])

        def phaseM(l, xin, xmid):
            with ExitStack() as ps_:
                c.stack = ps_
                st = {"wst": Ring(c, "m_wst", 2, [128, 4, 1024], F32)}
                wa = c.sb("m_wa", [128, 2, 1024], BF16)
                wbb = c.sb("m_wb", [128, 2, 1024], BF16)
                wc = c.sb("m_wc", [128, 4, 1024], BF16)
                wo = c.sb("m_wo", [128, 8, 1024], BF16)
                bwa, bwb_, bwc, bwo = Buf(), Buf(), Buf(), Buf()
                load_w(st, wa, bwa, w_br_a[l].rearrange("(kc p) n -> p kc n", p=128), 2, 1024)
                load_w(st, wbb, bwb_, w_br_b[l].rearrange("(kc p) n -> p kc n", p=128), 2, 1024)
                load_w(st, wc, bwc, w_br_c[l].rearrange("(kc p) n -> p kc n", p=128), 4, 1024)
                load_w(st, wo, bwo, w_out[l].rearrange("(kc p) n -> p kc n", p=128), 8, 1024)
                gp = c.sb("m_gp", [128, D], F32)
                bgp = Buf()
                bcast_load(gp[:], norms[l, 8:16, :].rearrange("a b -> (a b)"), bgp)
                otr = Ring(c, "m_ot", 2, [128, 8, 512], BF16)
                gtr = Ring(c, "m_gt", 1, [128, 24, 512], BF16)
                yT = c.sb("m_yT", [128, 8, 512], BF16)
                byT = Buf()
                pbr = Ring(c, "m_pb", 4, [128, 512], F32, psum=True)
                phr = Ring(c, "m_ph", 2, [128, 1024], F32, psum=True)
                t1r = Ring(c, "m_t1", 2, [128, 512], F32)
                t2r = Ring(c, "m_t2", 2, [128, 512], F32)
                xr = Ring(c, "m_x", 2, [128, D], F32)
                hr = Ring(c, "m_h", 2, [128, D], F32)
                jr = Ring(c, "m_j", 1, [128, D], BF16)
                ssr = Ring(c, "m_ss", 2, [128, 4], F32)
                OTv = OT.rearrange("(kc p) t -> p kc t", p=128)
                GTv = GT.rearrange("(kc p) t -> p kc t", p=128)
                for tg in range(S // 512):
                    t0 = tg * 512
                    ot, bot = otr.next()
                    c.dma(sp, ot[:], OTv[:, :, t0:t0 + 512], writes=[bot])
                    gt, bgt = gtr.next()
                    c.dma(sp, gt[:], GTv[:, :, t0:t0 + 512], writes=[bgt])
                    for n in range(8):
                        pa, bpa = pbr.next()
                        for kc in range(2):
                            c.op(pe, lambda e: e.matmul(pa[:], wa[:, kc, n * 128:(n + 1) * 128], ot[:, kc, :],
                                                        start=(kc == 0), stop=(kc == 1)),
                                 reads=[bwa, bot], **({"writes": [bpa]} if kc == 0 else {"adds": [bpa]}))
                        pb, bpb = pbr.next()
                        for kc in range(2):
                            c.op(pe, lambda e: e.matmul(pb[:], wbb[:, kc, n * 128:(n + 1) * 128], ot[:, 2 + kc, :],
                                                        start=(kc == 0), stop=(kc == 1)),
                                 reads=[bwb_, bot], **({"writes": [bpb]} if kc == 0 else {"adds": [bpb]}))
                        pc, bpc = pbr.next()
                        for kc in range(4):
                            c.op(pe, lambda e: e.matmul(pc[:], wc[:, kc, n * 128:(n + 1) * 128], ot[:, 4 + kc, :],
                                                        start=(kc == 0), stop=(kc == 3)),
                                 reads=[bwc, bot], **({"writes": [bpc]} if kc == 0 else {"adds": [bpc]}))
                        t1, bt1 = t1r.next()
                        t2, bt2 = t2r.next()
                        c.op(dve, lambda e: e.tensor_tensor(t1[:], pa[:], gt[:, n, :], ALU.mult), reads=[bpa, bgt], writes=[bt1])
                        c.op(dve, lambda e: e.tensor_tensor(t2[:], pb[:], gt[:, 8 + n, :], ALU.mult), reads=[bpb, bgt], writes=[bt2])
                        c.op(pl, lambda e: e.tensor_tensor(t1[:], t1[:], t2[:], ALU.add), reads=[bt2], upd=[bt1])
                        c.op(dve, lambda e: e.tensor_tensor(t2[:], pc[:], gt[:, 16 + n, :], ALU.mult), reads=[bpc, bgt], upd=[bt2])
                        c.op(pl, lambda e: e.tensor_tensor(yT[:, n, :], t1[:], t2[:], ALU.add), reads=[bt1, bt2],
                             **({"writes": [byT]} if n == 0 else {"adds": [byT]}))
                    for j in range(4):
                        tj = t0 + j * 128
                        ph, bph = phr.next()
                        for half in range(2):
                            for kc in range(8):
                                c.op(pe, lambda e: e.matmul(ph[:, half * 512:(half + 1) * 512], yT[:, kc, j * 128:(j + 1) * 128],
                                                            wo[:, kc, half * 512:(half + 1) * 512],
                                                            start=(kc == 0), stop=(kc == 7)),
                                     reads=[byT, bwo], **({"writes": [bph]} if (kc == 0 and half == 0) else {"adds": [bph]}))
                        xt, bx = xr.next()
                        c.dma(sp, xt[:], xin[tj:tj + 128, :], writes=[bx])
                        ss, bs = ssr.next()
                        c.op(dve, lambda e: e.memset(ss[:], 0.0), writes=[bs])
                        jk, bj = jr.next()
                        c.op(act, lambda e: e.activation(jk[:], ph[:], AF.Square, accum_out=ss[:, 0:1]),
                             reads=[bph], writes=[bj], upd=[bs])
                        c.op(dve, lambda e: e.tensor_scalar(ss[:, 1:2], ss[:, 0:1], 1.0 / D, EPS, ALU.mult, ALU.add), upd=[bs])
                        c.op(act, lambda e: e.activation(ss[:, 2:3], ss[:, 1:2], AF.Sqrt), upd=[bs])
                        c.op(dve, lambda e: e.reciprocal(ss[:, 2:3], ss[:, 2:3]), upd=[bs])
                        hh, bh = hr.next()
                        c.op(dve, lambda e: e.scalar_tensor_tensor(hh[:], ph[:], ss[:, 2:3], gp[:], ALU.mult, ALU.mult),
                             reads=[bph, bs, bgp], writes=[bh])
                        c.op(pl, lambda e: e.tensor_tensor(hh[:], hh[:], xt[:], ALU.add), reads=[bx], upd=[bh])
                        c.dma(pool, xmid[tj:tj + 128, :], hh[:], reads=[bh])
                c.barrier()
            c.stack = gstack

        def phaseF(l, xmid, xout):
            TF = 512
            GC = 0.7978845608028654
            with ExitStack() as ps_:
                c.stack = ps_
                st = {
                    "xr": Ring(c, "f_xr", 2, [128, D], F32),
                    "ss": Ring(c, "f_ss", 2, [128, 4], F32),
                    "junk": Ring(c, "f_junk", 1, [128, D], BF16),
                    "xb": Ring(c, "f_xb", 2, [128, D], BF16),
                    "pT": Ring(c, "f_pT", 2, [128, 8, 128], BF16, psum=True),
                    "wst": Ring(c, "f_wst", 2, [128, 8, 512], F32),
                }
                wd = c.sb("f_wd", [128, 22, 1024], BF16)
                bwd = Buf()
                wdv = w_down[l].rearrange("(kc p) n -> p kc n", p=128)
                firstw = True
                for k0 in range(0, 22, 8):
                    kn = min(8, 22 - k0)
                    for half in range(2):
                        ws, bws = st["wst"].next()
                        c.dma(sp, ws[:, 0:kn, :], wdv[:, k0:k0 + kn, half * 512:(half + 1) * 512], writes=[bws])
                        for kc in range(kn):
                            c.op(act, lambda e: e.activation(wd[:, k0 + kc, half * 512:(half + 1) * 512], ws[:, kc, :], AF.Copy),
                                 reads=[bws], **({"writes": [bwd]} if firstw else {"adds": [bwd]}))
                            firstw = False
                gp = c.sb("f_gp", [128, D], F32)
                bgp = Buf()
                bcast_load(gp[:], norms[l, 24:32, :].rearrange("a b -> (a b)"), bgp)
                gain = gcols[:, l, 16:24]
                wur = Ring(c, "f_wu", 2, [128, 8, 256], BF16)
                xnT = c.sb("f_xnT", [128, 8, TF], BF16)
                bxn = [Buf() for _ in range(TF // 128)]
                halo = c.sb("f_halo", [128, 44, 2], F32)
                bhalo = [Buf() for _ in range(44)]
                c.op(dve, lambda e: e.memset(halo[:], 0.0), writes=bhalo)
                aT = c.sb("f_aT", [128, 22, TF], BF16)
                baT = Buf()
                pur = Ring(c, "f_pu", 4, [128, 512], F32, psum=True)
                phr = Ring(c, "f_ph", 1, [128, 1024], F32, psum=True)
                cgr = Ring(c, "f_cg", 1, [128, 512], F32)
                cur = Ring(c, "f_cu", 1, [128, 512], F32)
                t1r = Ring(c, "f_t1", 1, [128, 512], F32)
                t2r = Ring(c, "f_t2", 1, [128, 512], F32)
                hr = Ring(c, "f_h", 1, [128, D], F32)
                jr = Ring(c, "f_j", 1, [128, D], BF16)
                ssr = Ring(c, "f_ss2", 2, [128, 4], F32)
                wuv = w_up[l].rearrange("(kc p) n -> p kc n", p=128)

                def conv(pu, bpu, ch, dst, bdst):
                    w0 = cw[:, l, ch:ch + 1]
                    w1 = cw[:, l, 44 + ch:44 + ch + 1]
                    w2 = cw[:, l, 88 + ch:88 + ch + 1]
                    c.op(act, lambda e: e.activation(dst[:], pu[:], AF.Identity, bias=cb[:, l, ch:ch + 1], scale=w2),
                         reads=[bpu, Bg], writes=[bdst])
                    c.op(dve, lambda e: e.scalar_tensor_tensor(dst[:, 1:TF], pu[:, 0:TF - 1], w1, dst[:, 1:TF], ALU.mult, ALU.add),
                         reads=[bpu, Bg], upd=[bdst])
                    c.op(dve, lambda e: e.scalar_tensor_tensor(dst[:, 2:TF], pu[:, 0:TF - 2], w0, dst[:, 2:TF], ALU.mult, ALU.add),
                         reads=[bpu, Bg], upd=[bdst])
                    c.op(dve, lambda e: e.scalar_tensor_tensor(dst[:, 0:1], halo[:, ch, 1:2], w1, dst[:, 0:1], ALU.mult, ALU.add),
                         reads=[bhalo[ch], Bg], upd=[bdst])
                    c.op(dve, lambda e: e.scalar_tensor_tensor(dst[:, 0:2], halo[:, ch, 0:2], w0, dst[:, 0:2], ALU.mult, ALU.add),
                         reads=[bhalo[ch], Bg], upd=[bdst])
                    c.op(dve, lambda e: e.tensor_copy(halo[:, ch, :], pu[:, TF - 2:TF]), reads=[bpu], writes=[bhalo[ch]])

                for tg in range(S // TF):
                    t0 = tg * TF
                    for tt in range(TF // 128):
                        norm_transpose(st, xmid[t0 + tt * 128:t0 + (tt + 1) * 128, :],
                                       xnT[:, :, tt * 128:(tt + 1) * 128], bxn[tt], True)
                    for cp in range(22):
                        wu, bwu = wur.next()
                        ws, bws = st["wst"].next()
                        c.dma(sp, ws[:, :, 0:128], wuv[:, :, cp * 128:(cp + 1) * 128], writes=[bws])
                        c.dma(sp, ws[:, :, 128:256], wuv[:, :, DFF + cp * 128:DFF + (cp + 1) * 128], adds=[bws])
                        for kc in range(8):
                            c.op(act, lambda e: e.activation(wu[:, kc, :], ws[:, kc, 0:256], AF.Identity, scale=gain[:, kc:kc + 1]),
                                 reads=[bws, Bg], **({"writes": [bwu]} if kc == 0 else {"adds": [bwu]}))
                        pg, bpg = pur.next()
                        pu, bpu = pur.next()
                        for (pp_, bpp, off) in ((pg, bpg, 0), (pu, bpu, 128)):
                            for kc in range(8):
                                c.op(pe, lambda e: e.matmul(pp_[:], wu[:, kc, off:off + 128], xnT[:, kc, :],
                                                            start=(kc == 0), stop=(kc == 7)),
                                     reads=[bwu] + bxn, **({"writes": [bpp]} if kc == 0 else {"adds": [bpp]}))
                        cg, bcg = cgr.next()
                        cu, bcu = cur.next()
                        conv(pg, bpg, cp, cg, bcg)
                        conv(pu, bpu, 22 + cp, cu, bcu)
                        t1, bt1 = t1r.next()
                        t2, bt2 = t2r.next()
                        c.op(act, lambda e: e.activation(t1[:], cg[:], AF.Square), reads=[bcg], writes=[bt1])
                        c.op(dve, lambda e: e.tensor_scalar(t1[:], t1[:], 0.044715, 1.0, ALU.mult, ALU.add), upd=[bt1])
                        c.op(pl, lambda e: e.tensor_tensor(t1[:], t1[:], cg[:], ALU.mult), reads=[bcg], upd=[bt1])
                        c.op(act, lambda e: e.activation(t2[:], t1[:], AF.Sigmoid, scale=2.0 * GC), reads=[bt1], writes=[bt2])
                        c.op(pl, lambda e: e.tensor_tensor(t2[:], t2[:], cg[:], ALU.mult), reads=[bcg], upd=[bt2])
                        c.op(dve, lambda e: e.tensor_tensor(aT[:, cp, :], t2[:], cu[:], ALU.mult), reads=[bt2, bcu],
                             **({"writes": [baT]} if cp == 0 else {"adds": [baT]}))
                    for j in range(TF // 128):
                        tj = t0 + j * 128
                        ph, bph = phr.next()
                        for half in range(2):
                            for kc in range(22):
                                c.op(pe, lambda e: e.matmul(ph[:, half * 512:(half + 1) * 512], aT[:, kc, j * 128:(j + 1) * 128],
                                                            wd[:, kc, half * 512:(half + 1) * 512],
                                                            start=(kc == 0), stop=(kc == 21)),
                                     reads=[baT, bwd], **({"writes": [bph]} if (kc == 0 and half == 0) else {"adds": [bph]}))
                        ss, bs = ssr.next()
                        c.op(dve, lambda e: e.memset(ss[:], 0.0), writes=[bs])
                        jk, bj = jr.next()
                        c.op(act, lambda e: e.activation(jk[:], ph[:], AF.Square, accum_out=ss[:, 0:1]),
                             reads=[bph], writes=[bj], upd=[bs])
                        c.op(dve, lambda e: e.tensor_scalar(ss[:, 1:2], ss[:, 0:1], 1.0 / D, EPS, ALU.mult, ALU.add), upd=[bs])
                        c.op(act, lambda e: e.activation(ss[:, 2:3], ss[:, 1:2], AF.Sqrt), upd=[bs])
                        c.op(dve, lambda e: e.reciprocal(ss[:, 2:3], ss[:, 2:3]), upd=[bs])
                        hh, bh = hr.next()
                        c.op(dve, lambda e: e.scalar_tensor_tensor(hh[:], ph[:], ss[:, 2:3], gp[:], ALU.mult, ALU.mult),
                             reads=[bph, bs, bgp], writes=[bh])
                        xt, bx = st["xr"].next()
                        c.dma(sp, xt[:], xmid[tj:tj + 128, :], writes=[bx])
                        c.op(pl, lambda e: e.tensor_tensor(hh[:], hh[:], xt[:], ALU.add), reads=[bx], upd=[bh])
                        c.dma(pool, xout[tj:tj + 128, :], hh[:], reads=[bh])
                c.barrier()
            c.stack = gstack

        cur = x_in
        for l in range(depth):
            last = (l == depth - 1)
            if "1" in phases:
                phase1(l, cur)
            if "A" in phases:
                phaseA(l)
            if "b" in phases:
                phaseB1(l)
            if "B" in phases:
                phaseB2(l)
            if "C" in phases:
                phaseC(l)
            if "M" in phases:
                phaseM(l, cur, X1)
            if "F" in phases:
                phaseF(l, X1, y_out if last else X2)
            cur = X2
        c.barrier()
    return nc


def make_in_maps(inputs, S):
    cst = make_consts()
    f = lambda a: np.ascontiguousarray(np.asarray(a, dtype=np.float32))
    shared = {
        "w_in": f(inputs["w_in"]), "w_br_a": f(inputs["w_br_a"]), "w_br_b": f(inputs["w_br_b"]),
        "w_br_c": f(inputs["w_br_c"]), "w_out": f(inputs["w_out"]),
        "lam": np.ascontiguousarray(np.stack([f(inputs["lam_q1"]), f(inputs["lam_k1"]), f(inputs["lam_q2"]),
                                              f(inputs["lam_k2"])], axis=1)),
        "subln_g": f(inputs["subln_g"]),
        "norms": np.ascontiguousarray(np.stack([f(inputs["norm_mix_pre"]), f(inputs["norm_mix_post"]),
                                                f(inputs["norm_ffn_pre"]), f(inputs["norm_ffn_post"])],
                                               axis=1).reshape(DEPTH, 32, 128)),
        "w_up": f(inputs["w_ffn_up"]),
        "conv_w": np.ascontiguousarray(f(inputs["conv_w"]).reshape(DEPTH, 132, 128)),
        "conv_b": np.ascontiguousarray(f(inputs["conv_b"]).reshape(DEPTH, 44, 128)),
        "w_down": f(inputs["w_ffn_down"]),
        "cst": cst,
    }
    x = f(inputs["x"])
    pos = np.ascontiguousarray(np.asarray(inputs["positions"], dtype=np.int32))
    maps = []
    for b in range(x.shape[0]):
        m = dict(shared)
        m["x"] = np.ascontiguousarray(x[b])
        m["pos"] = np.ascontiguousarray(pos[b].reshape(S // 128, 128))
        maps.append(m)
    return maps


def kernel(**inputs):
    x = np.asarray(inputs["x"])
    B, S, _ = x.shape
    nc = build(S)
    maps = make_in_maps(inputs, S)
    res = run_bass_kernel_spmd(nc, maps, core_ids=list(range(B)))
    out = np.stack([np.asarray(r["y"], dtype=np.float32) for r in res.results], axis=0)
    return out
```

```python
import math
import os as _os
from contextlib import ExitStack
import numpy as np
import concourse.bass as bass
import concourse.mybir as mybir
from concourse.bass_utils import run_bass_kernel_spmd

F32 = mybir.dt.float32
BF16 = mybir.dt.bfloat16
I32 = mybir.dt.int32
AF = mybir.ActivationFunctionType
ALU = mybir.AluOpType
AX = mybir.AxisListType

D = 1024
IN_W = 8264
DFF = 2816
NCORES = 8
DEPTH = 2
EPS = 1e-6
A_WB = (1, 4, 16)
A_PAIRS = ((128, 1), (512, 4), (2048, 16))
NSLOT = 17
NIT = 16
TOPK = 256
NMASK = 24
C_ID = 0
C_MASK = 128
C_NEG = C_MASK + NMASK * 128
C_POW = C_NEG + 128
C_INV = C_POW + NIT
NCST = C_INV + 8


def make_consts():
    c = np.zeros((128, NCST), np.float32)
    c[:, C_ID:C_ID + 128] = np.eye(128, dtype=np.float32)
    s = np.arange(128)[:, None]
    q = np.arange(128)[None, :]
    mi = 0
    for g, (w, d) in enumerate(A_PAIRS):
        for dl in range(A_WB[g] + 1):
            dist = 128 * dl + q - s
            m = (dist >= 0) & (dist <= w) & (dist % d == 0)
            c[:, C_MASK + mi * 128:C_MASK + (mi + 1) * 128] = m.astype(np.float32)
            mi += 1
    assert mi == NMASK
    c[:, C_NEG:C_NEG + 128] = np.where(q <= s, 0.0, -1e30).astype(np.float32)
    for k in range(NIT):
        c[:, C_POW + k] = 2.0 ** (-(k + 1))
    inv = 500000.0 ** (-np.arange(0, 16, 2, dtype=np.float32) / 16.0)
    c[:, C_INV:C_INV + 8] = inv[None, :].astype(np.float32)
    return c


def mask_index(g, dl):
    return sum(A_WB[i] + 1 for i in range(g)) + dl


class Buf:
    __slots__ = ("writers", "readers")

    def __init__(self):
        self.writers = []
        self.readers = []


def _compact(toks):
    best = {}
    for s, v in toks:
        if s.num not in best or best[s.num][1] < v:
            best[s.num] = (s, v)
    return list(best.values())


class Eng:
    def __init__(self, ctx, name, eng):
        self.ctx = ctx
        self.name = name
        self.eng = eng
        self.sem = None
        self.count = 0
        self.seen = {}
        self.own = set()

    def wait(self, tok):
        sem, val = tok
        if self.name == "pe" and sem.num in self.own:
            return
        if self.seen.get(sem.num, 0) >= val:
            return
        self.eng.wait_ge(sem, val)
        self.seen[sem.num] = val

    def last(self):
        return (self.sem, self.count) if self.sem is not None and self.count > 0 else None


class Ctx:
    SEM_ROLL = 32000

    def __init__(self, nc, semstack):
        self.nc = nc
        self.semstack = semstack
        self.stack = None
        self.nsem = 0
        self.pe = Eng(self, "pe", nc.tensor)
        self.act = Eng(self, "act", nc.scalar)
        self.dve = Eng(self, "dve", nc.vector)
        self.pool = Eng(self, "pool", nc.gpsimd)
        self.sp = Eng(self, "sp", nc.sync)
        self.engs = [self.pe, self.act, self.dve, self.pool, self.sp]
        self.dma_pool = {}
        self.old_sems = []
        self.nuniq = 0

    def alloc_sem(self, name):
        self.nsem += 1
        return self.semstack.enter_context(self.nc.semaphore(f"{name}_{self.nsem}"))

    def sb(self, name, shape, dtype):
        self.nuniq += 1
        return self.stack.enter_context(self.nc.sbuf_tensor(f"{name}_{self.nuniq}", list(shape), dtype))

    def ps(self, name, shape, dtype):
        self.nuniq += 1
        return self.stack.enter_context(self.nc.psum_tensor(f"{name}_{self.nuniq}", list(shape), dtype))

    def _pre(self, E, reads, writes, adds, upd):
        for b in reads:
            for t in b.writers:
                E.wait(t)
        for b in writes:
            for t in b.writers:
                E.wait(t)
            for t in b.readers:
                E.wait(t)
        for b in upd:
            for t in b.writers:
                E.wait(t)
            for t in b.readers:
                E.wait(t)
        for b in adds:
            for t in b.readers:
                E.wait(t)

    def _post(self, tok, reads, writes, adds, upd):
        for b in reads:
            b.readers.append(tok)
            if len(b.readers) > 16:
                b.readers = _compact(b.readers)
        for b in writes:
            b.writers = [tok]
            b.readers = []
        for b in upd:
            b.writers.append(tok)
            b.readers = []
            if len(b.writers) > 16:
                b.writers = _compact(b.writers)
        for b in adds:
            b.writers.append(tok)
            b.readers = []
            if len(b.writers) > 16:
                b.writers = _compact(b.writers)

    def op(self, E, fn, reads=(), writes=(), adds=(), upd=()):
        self._pre(E, reads, writes, adds, upd)
        if E.sem is None or E.count >= self.SEM_ROLL:
            if E.sem is not None:
                self.old_sems.append((E.sem, E.count))
            E.sem = self.alloc_sem(E.name)
            E.own.add(E.sem.num)
            E.count = 0
        ins = fn(E.eng)
        E.count += 1
        ins.then_inc(E.sem, 1)
        tok = (E.sem, E.count)
        self._post(tok, reads, writes, adds, upd)
        return tok

    def dma(self, E, out, in_, reads=(), writes=(), adds=(), upd=(), nsem=16, **kw):
        key = E.name
        if key not in self.dma_pool:
            self.dma_pool[key] = {"sems": [], "vals": [], "next": 0, "toks": []}
        P = self.dma_pool[key]
        self._pre(E, reads, writes, adds, upd)
        i = P["next"]
        if i >= len(P["sems"]):
            P["sems"].append(self.alloc_sem("dma" + key))
            P["vals"].append(0)
            P["toks"].append(None)
        else:
            E.wait(P["toks"][i])
        P["next"] = (i + 1) % nsem
        sem = P["sems"][i]
        P["vals"][i] += 16
        ins = E.eng.dma_start(out=out, in_=in_, **kw)
        ins.then_inc(sem, 16)
        tok = (sem, P["vals"][i])
        P["toks"][i] = tok
        self._post(tok, reads, writes, adds, upd)
        return tok

    def barrier(self):
        toks = []
        for E in self.engs:
            t = E.last()
            if t is not None:
                toks.append(t)
        for P in self.dma_pool.values():
            for t in P["toks"]:
                if t is not None:
                    toks.append(t)
        for E in self.engs:
            for t in toks:
                E.wait(t)


class Ring:
    def __init__(self, c, name, n, shape, dtype, psum=False):
        self.t = [(c.ps if psum else c.sb)(f"{name}{i}", shape, dtype) for i in range(n)]
        self.b = [Buf() for _ in range(n)]
        self.i = 0
        self.n = n

    def next(self):
        t, b = self.t[self.i], self.b[self.i]
        self.i = (self.i + 1) % self.n
        return t, b


def build(S, depth=DEPTH, dbg=False, phases="1AbBCMF"):
    NT = S // 128
    nc = bass.Bass("TRN2", target_bir_lowering=False)

    def din(name, shape, dt=F32):
        return nc.dram_tensor(name, list(shape), dt, kind="ExternalInput").ap()

    x_in = din("x", [S, D])
    pos_in = din("pos", [NT, 128], I32)
    w_in = din("w_in", [DEPTH, D, IN_W])
    w_br_a = din("w_br_a", [DEPTH, 256, D])
    w_br_b = din("w_br_b", [DEPTH, 256, D])
    w_br_c = din("w_br_c", [DEPTH, 512, D])
    w_out = din("w_out", [DEPTH, D, D])
    lam_in = din("lam", [DEPTH, 4, 64])
    subln = din("subln_g", [DEPTH, 128])
    norms = din("norms", [DEPTH, 32, 128])
    w_up = din("w_up", [DEPTH, D, 2 * DFF])
    conv_w = din("conv_w", [DEPTH, 132, 128])
    conv_b = din("conv_b", [DEPTH, 44, 128])
    w_down = din("w_down", [DEPTH, DFF, D])
    cst_in = din("cst", [128, NCST])
    y_out = nc.dram_tensor("y", [S, D], F32, kind="ExternalOutput").ap()

    def scr(name, shape, dt):
        return nc.dram_tensor(name, list(shape), dt).ap()

    QT = scr("QT", [58 * 64, S], BF16)
    VA_A = scr("VA_A", [S, 12 * 65], BF16)
    VA_B = scr("VA_B", [S, 4 * 65], BF16)
    VA_C = scr("VA_C", [S, 4 * 129], BF16)
    W8 = scr("W8", [S, 8], F32)
    GT = scr("GT", [3072, S], BF16)
    OT = scr("OT", [1024, S], BF16)
    MT = scr("MT", [NT, 128, NT, 128], BF16)
    X1 = scr("X1", [S, D], F32)
    X2 = scr("X2", [S, D], F32)
    QT3 = QT.rearrange("(n d) t -> d n t", d=64)

    with ExitStack() as semstack, ExitStack() as gstack:
        c = Ctx(nc, semstack)
        c.stack = gstack
        pe, act, dve, pool, sp = c.pe, c.act, c.dve, c.pool, c.sp
        pl = pool if _os.environ.get("K_POOL", "0") == "1" else dve

        cstf = c.sb("cstf", [128, NCST], F32)
        Bc = Buf()
        ident_b = c.sb("ident_b", [128, 128], BF16)
        maskA = c.sb("maskA", [128, NMASK, 128], BF16)
        cosT = c.sb("cosT", [128, NT, 8], F32)
        sinT = c.sb("sinT", [128, NT, 8], F32)
        pib = c.sb("pib", [128, 1], F32)
        Bg = Buf()
        ident_f = cstf[:, C_ID:C_ID + 128]
        negtri = cstf[:, C_NEG:C_NEG + 128]

        c.dma(sp, cstf[:], cst_in, writes=[Bc])
        c.op(dve, lambda e: e.tensor_copy(ident_b[:], cstf[:, C_ID:C_ID + 128]), reads=[Bc], adds=[Bg])
        c.op(dve, lambda e: e.tensor_copy(maskA[:].rearrange("p m q -> p (m q)"), cstf[:, C_MASK:C_MASK + NMASK * 128]),
             reads=[Bc], adds=[Bg])
        c.op(dve, lambda e: e.memset(pib[:], math.pi), adds=[Bg])

        def load_cols(dst, src_rows_ap, n, stack_tag):
            with ExitStack() as ls:
                old = c.stack
                c.stack = ls
                tmp = c.sb("lc_tmp", [128, 128], F32)
                pt = c.ps("lc_ps", [128, 128], F32)
                bt, bp = Buf(), Buf()
                c.dma(sp, tmp[0:n, :], src_rows_ap, writes=[bt])
                c.op(pe, lambda e: e.transpose(pt[:, 0:n], tmp[0:n, :], cstf[0:n, C_ID:C_ID + n]),
                     reads=[bt, Bc], writes=[bp])
                c.op(dve, lambda e: e.tensor_copy(dst, pt[:, 0:n]), reads=[bp], adds=[Bg])
                c.barrier()
                c.stack = old

        with ExitStack() as ls:
            c.stack = ls
            posi = c.sb("posi", [128, 128], I32)
            posf = c.sb("posf", [128, 128], F32)
            pt = c.ps("pos_ps", [128, 128], F32)
            posT = c.sb("posT", [128, NT], F32)
            ang = c.sb("ang", [128, NT, 8], F32)
            ang2 = c.sb("ang2", [128, NT, 8], F32)
            b1, b2, b3, b4, b5, b6 = (Buf() for _ in range(6))
            c.dma(sp, posi[0:NT, :], pos_in, writes=[b1])
            c.op(dve, lambda e: e.tensor_copy(posf[0:NT, :], posi[0:NT, :]), reads=[b1], writes=[b2])
            c.op(pe, lambda e: e.transpose(pt[:, 0:NT], posf[0:NT, :], cstf[0:NT, C_ID:C_ID + NT]),
                 reads=[b2, Bc], writes=[b3])
            c.op(dve, lambda e: e.tensor_copy(posT[:], pt[:, 0:NT]), reads=[b3], writes=[b4])
            for i in range(8):
                c.op(dve, lambda e: e.tensor_scalar(ang[:, :, i], posT[:], cstf[:, C_INV + i:C_INV + i + 1], None,
                                                    ALU.mult), reads=[b4, Bc], adds=[b5])
            TWO_PI = 2.0 * math.pi
            ki = c.sb("ki", [128, NT, 8], I32)
            kf = c.sb("kf", [128, NT, 8], F32)
            rr_ = c.sb("rr_", [128, NT, 8], F32)
            tt_ = c.sb("tt_", [128, NT, 8], F32)
            b7, b8, b9, b10 = (Buf() for _ in range(4))
            for (dst, shift) in ((sinT, 0.0), (cosT, math.pi / 2)):
                c.op(dve, lambda e: e.tensor_scalar(ang2[:], ang[:], shift, None, ALU.add), reads=[b5], writes=[b6])
                c.op(dve, lambda e: e.tensor_scalar(kf[:], ang2[:], 1.0 / TWO_PI, None, ALU.mult), reads=[b6], writes=[b8])
                c.op(dve, lambda e: e.tensor_copy(ki[:], kf[:]), reads=[b8], writes=[b7])
                c.op(dve, lambda e: e.tensor_copy(kf[:], ki[:]), reads=[b7], writes=[b8])
                c.op(dve, lambda e: e.scalar_tensor_tensor(rr_[:], kf[:], -TWO_PI, ang2[:], ALU.mult, ALU.add),
                     reads=[b8, b6], writes=[b9])
                c.op(dve, lambda e: e.tensor_scalar(tt_[:], rr_[:], math.pi, TWO_PI, ALU.is_gt, ALU.mult),
                     reads=[b9], writes=[b10])
                c.op(dve, lambda e: e.tensor_tensor(rr_[:], rr_[:], tt_[:], ALU.subtract), reads=[b10], upd=[b9])
                c.op(dve, lambda e: e.tensor_scalar(tt_[:], rr_[:], -math.pi, TWO_PI, ALU.is_lt, ALU.mult),
                     reads=[b9], upd=[b10])
                c.op(dve, lambda e: e.tensor_tensor(rr_[:], rr_[:], tt_[:], ALU.add), reads=[b10], upd=[b9])
                c.op(act, lambda e: e.activation(dst[:], rr_[:], AF.Sin), reads=[b9], adds=[Bg])
            c.barrier()
        c.stack = gstack

        gcols = c.sb("gcols", [128, DEPTH, 32], F32)
        cw = c.sb("cw", [128, DEPTH, 132], F32)
        cb = c.sb("cb", [128, DEPTH, 44], F32)
        lamt = c.sb("lamt", [128, DEPTH, 4], F32)
        for l in range(depth):
            load_cols(gcols[:, l, :], norms[l], 32, "g")
            load_cols(cw[:, l, 0:128], conv_w[l, 0:128, :], 128, "cw")
            load_cols(cw[:, l, 128:132], conv_w[l, 128:132, :], 4, "cw2")
            load_cols(cb[:, l, :], conv_b[l], 44, "cb")
        with ExitStack() as ls:
            c.stack = ls
            lt = c.sb("lt", [128, DEPTH, 4, 64], F32)
            pr = c.sb("pr", [128, DEPTH, 2, 64], F32)
            sm = c.sb("sm", [128, DEPTH, 2], F32)
            ex = c.sb("ex", [128, DEPTH, 2], F32)
            b1, b2, b3, b4 = (Buf() for _ in range(4))
            for l in range(DEPTH):
                c.dma(sp, lt[:, l, :, :].rearrange("p a b -> p (a b)"),
                      lam_in[l].rearrange("a b -> (a b)").partition_broadcast(128), adds=[b1])
            for l in range(DEPTH):
                for j in range(2):
                    c.op(dve, lambda e: e.tensor_tensor(pr[:, l, j, :], lt[:, l, 2 * j, :], lt[:, l, 2 * j + 1, :],
                                                        ALU.mult), reads=[b1], adds=[b2])
            c.op(dve, lambda e: e.tensor_reduce(sm[:].rearrange("p l j -> p (l j)"),
                                                pr[:].rearrange("p l j d -> p (l j) d"), AX.X, ALU.add),
                 reads=[b2], writes=[b3])
            c.op(act, lambda e: e.activation(ex[:], sm[:], AF.Exp), reads=[b3], writes=[b4])
            for l in range(DEPTH):
                lam_init = 0.8 - 0.6 * math.exp(-0.3 * l)
                c.op(dve, lambda e: e.tensor_tensor(lamt[:, l, 0:1], ex[:, l, 0:1], ex[:, l, 1:2], ALU.subtract),
                     reads=[b4], adds=[Bg])
                c.op(dve, lambda e: e.tensor_scalar(lamt[:, l, 0:1], lamt[:, l, 0:1], lam_init, None, ALU.add),
                     upd=[Bg])
                c.op(dve, lambda e: e.tensor_scalar(lamt[:, l, 1:2], lamt[:, l, 0:1], -1.0, None, ALU.mult),
                     upd=[Bg])
            c.barrier()
        c.stack = gstack
        c.barrier()

        def norm_transpose(st, xsrc_rows, xnT_dst, bdst, first_write):
            xt, bx = st["xr"].next()
            c.dma(sp, xt[:], xsrc_rows, writes=[bx])
            ss, bs = st["ss"].next()
            c.op(dve, lambda e: e.memset(ss[:], 0.0), writes=[bs])
            jk, bj = st["junk"].next()
            c.op(act, lambda e: e.activation(jk[:], xt[:], AF.Square, accum_out=ss[:, 0:1]),
                 reads=[bx], writes=[bj], upd=[bs])
            c.op(dve, lambda e: e.tensor_scalar(ss[:, 1:2], ss[:, 0:1], 1.0 / D, EPS, ALU.mult, ALU.add), upd=[bs])
            c.op(act, lambda e: e.activation(ss[:, 2:3], ss[:, 1:2], AF.Sqrt), upd=[bs])
            c.op(dve, lambda e: e.reciprocal(ss[:, 2:3], ss[:, 2:3]), upd=[bs])
            xb, bxb = st["xb"].next()
            c.op(dve, lambda e: e.tensor_scalar(xb[:], xt[:], ss[:, 2:3], None, ALU.mult),
                 reads=[bx, bs], writes=[bxb])
            pt, bpt = st["pT"].next()
            for kc in range(8):
                c.op(pe, lambda e: e.transpose(pt[:, kc, :], xb[:, kc * 128:(kc + 1) * 128], ident_b[:]),
                     reads=[bxb, Bg], **({"writes": [bpt]} if kc == 0 else {"adds": [bpt]}))
            c.op(act, lambda e: e.activation(xnT_dst, pt[:], AF.Copy), reads=[bpt], writes=[bdst])
            return xt, bx

        def load_w(st, dst_bf, bdst, src_ap, nk, ncols, gain_ap=None):
            cap = st["wst"].t[0].shape[1]
            for k0 in range(0, nk, cap):
                kn = min(cap, nk - k0)
                ws, bws = st["wst"].next()
                c.dma(sp, ws[:, 0:kn, 0:ncols], src_ap[:, k0:k0 + kn, :], writes=[bws])
                for kk in range(kn):
                    kc = k0 + kk
                    if gain_ap is not None:
                        c.op(act, lambda e: e.activation(dst_bf[:, kc, 0:ncols], ws[:, kk, 0:ncols], AF.Identity,
                                                         scale=gain_ap[:, kc:kc + 1]),
                             reads=[bws, Bg], **({"writes": [bdst]} if kc == 0 else {"adds": [bdst]}))
                    else:
                        c.op(act, lambda e: e.activation(dst_bf[:, kc, 0:ncols], ws[:, kk, 0:ncols], AF.Copy),
                             reads=[bws], **({"writes": [bdst]} if kc == 0 else {"adds": [bdst]}))

        def phase1(l, xin):
            TCH = min(S, 2048)
            NCH = S // TCH
            NTT = TCH // 128
            with ExitStack() as ps_:
                c.stack = ps_
                st = {
                    "xr": Ring(c, "xr", 2, [128, D], F32),
                    "ss": Ring(c, "ss", 2, [128, 4], F32),
                    "junk": Ring(c, "junk", 1, [128, D], BF16),
                    "xb": Ring(c, "xb", 2, [128, D], BF16),
                    "pT": Ring(c, "pT", 2, [128, 8, 128], BF16, psum=True),
                    "wst": Ring(c, "wst", 2, [128, 8, 512], F32),
                }
                wbr = Ring(c, "wb", 2, [128, 8, 512], BF16)
                pM = Ring(c, "pM", 2, [128, 512], F32, psum=True)
                pQ = Ring(c, "pQ", 2, [128, 4, 128], BF16, psum=True)
                xnT = c.sb("xnT", [128, 8, TCH], BF16)
                bxn = [Buf() for _ in range(NTT)]
                qbr = Ring(c, "qb", 2, [128, 512], BF16)
                tmps = [Ring(c, f"rt{i}", 2, [128, 8, 8], F32) for i in range(4)]
                rpr = Ring(c, "rp", 2, [128, 8, 16], F32)
                cos8 = c.sb("cos8", [128, NT, 8, 8], F32)
                sin8 = c.sb("sin8", [128, NT, 8, 8], F32)
                b88 = Buf()
                for (d8, src8) in ((cos8, cosT), (sin8, sinT)):
                    for hh_ in range(8):
                        c.op(dve, lambda e: e.tensor_copy(d8[:, :, hh_, :], src8[:]), reads=[Bg], adds=[b88])
                qsr = Ring(c, "qs", 3, [128, 4, 128], BF16)
                vsa = Ring(c, "vsa", 3, [128, 4, 65], BF16)
                vsc = Ring(c, "vsc", 3, [128, 4, 129], BF16)
                gsr = Ring(c, "gs", 2, [128, 4, 512], BF16)
                w8r = Ring(c, "w8", 3, [128, 8], F32)
                for r in (vsa, vsc):
                    for t, b in zip(r.t, r.b):
                        c.op(dve, lambda e: e.memset(t[:], 1.0), writes=[b])
                w_l = w_in[l].rearrange("(kc p) n -> p kc n", p=128)
                gain = gcols[:, l, 0:8]

                tiles = []
                for g in range(3):
                    tiles.append(("qk", g * 768, 8, g * 8))
                    tiles.append(("v", g * 768 + 512, 256, VA_A, g * 4 * 65, 64))
                tiles.append(("qk", 2304, 8, 24))
                tiles.append(("v", 2816, 256, VA_B, 0, 64))
                tiles.append(("qk", 3072, 8, 32))
                tiles.append(("kw", 3584, 72))
                tiles.append(("qk", 3656, 8, 41))
                tiles.append(("qk", 4168, 8, 49))
                tiles.append(("v", 4680, 512, VA_C, 0, 128))
                for i in range(6):
                    tiles.append(("gate", 5192 + i * 512, 512, i * 512))

                def rope_tile(pm, bpm, qbt, bqb, nh, tglob):
                    pmv = pm[:, 0:nh * 64].rearrange("p (h d) -> p h d", d=64)
                    qbv = qbt[:, 0:nh * 64].rearrange("p (h d) -> p h d", d=64)
                    c.op(act, lambda e: e.activation(qbv[:, :, 16:64], pmv[:, :, 16:64], AF.Copy),
                         reads=[bpm], writes=[bqb])
                    if _os.environ.get("K_NOROPE"):
                        c.op(act, lambda e: e.activation(qbv[:, :, 0:16], pmv[:, :, 0:16], AF.Copy), reads=[bpm], adds=[bqb])
                        return
                    cosb = cos8[:, tglob, 0:nh, :]
                    sinb = sin8[:, tglob, 0:nh, :]
                    (t1, bt1), (t2, bt2), (t3, bt3), (t4, bt4) = [r.next() for r in tmps]
                    rp, brp = rpr.next()
                    c.op(act, lambda e: e.activation(rp[:, 0:nh, :], pmv[:, :, 0:16], AF.Copy), reads=[bpm], writes=[brp])
                    x1 = rp[:, 0:nh, 0:8]
                    x2 = rp[:, 0:nh, 8:16]
                    c.op(dve, lambda e: e.tensor_tensor(t1[:, 0:nh, :], x1, cosb, ALU.mult), reads=[brp, b88], writes=[bt1])
                    c.op(dve, lambda e: e.tensor_tensor(t2[:, 0:nh, :], x2, sinb, ALU.mult), reads=[brp, b88], writes=[bt2])
                    c.op(dve, lambda e: e.tensor_tensor(qbv[:, :, 0:8], t1[:, 0:nh, :], t2[:, 0:nh, :], ALU.subtract),
                         reads=[bt1, bt2], adds=[bqb])
                    c.op(dve, lambda e: e.tensor_tensor(t3[:, 0:nh, :], x2, cosb, ALU.mult), reads=[brp, b88], writes=[bt3])
                    c.op(dve, lambda e: e.tensor_tensor(t4[:, 0:nh, :], x1, sinb, ALU.mult), reads=[brp, b88], writes=[bt4])
                    c.op(dve, lambda e: e.tensor_tensor(qbv[:, :, 8:16], t3[:, 0:nh, :], t4[:, 0:nh, :], ALU.add),
                         reads=[bt3, bt4], adds=[bqb])

                for ch in range(NCH):
                    for tt in range(NTT):
                        t0 = ch * TCH + tt * 128
                        norm_transpose(st, xin[t0:t0 + 128, :], xnT[:, :, tt * 128:(tt + 1) * 128], bxn[tt], True)
                    import os as _os
                    _kinds = _os.environ.get("K_KINDS", "qk,v,kw,gate").split(",")
                    for tl in tiles:
                        kind, col0 = tl[0], tl[1]
                        if kind not in _kinds:
                            continue
                        ncols = 512 if kind in ("qk", "gate") else tl[2]
                        wb, bwb = wbr.next()
                        load_w(st, wb, bwb, w_l[:, :, col0:col0 + ncols], 8, ncols, gain)
                        if kind == "gate":
                            row0 = tl[3]
                            for tg in range(TCH // 512):
                                gs, bgs = gsr.next()
                                for cbk in range(4):
                                    pm, bpm = pM.next()
                                    for kc in range(8):
                                        c.op(pe, lambda e: e.matmul(pm[:], wb[:, kc, cbk * 128:(cbk + 1) * 128],
                                                                    xnT[:, kc, tg * 512:(tg + 1) * 512],
                                                                    start=(kc == 0), stop=(kc == 7)),
                                             reads=[bwb] + bxn[tg * 4:(tg + 1) * 4],
                                             **({"writes": [bpm]} if kc == 0 else {"adds": [bpm]}))
                                    c.op(act, lambda e: e.activation(gs[:, cbk, :], pm[:], AF.Sigmoid), reads=[bpm],
                                         **({"writes": [bgs]} if cbk == 0 else {"adds": [bgs]}))
                                tg0 = ch * TCH + tg * 512
                                c.dma(pool, GT[row0:row0 + 512, tg0:tg0 + 512].rearrange("(b p) t -> p b t", p=128),
                                      gs[:], reads=[bgs])
                            continue
                        for tt in range(NTT):
                            t0 = ch * TCH + tt * 128
                            tglob = t0 // 128
                            pm, bpm = pM.next()
                            for kc in range(8):
                                c.op(pe, lambda e: e.matmul(pm[:, 0:ncols], xnT[:, kc, tt * 128:(tt + 1) * 128],
                                                            wb[:, kc, 0:ncols], start=(kc == 0), stop=(kc == 7)),
                                     reads=[bwb, bxn[tt]], **({"writes": [bpm]} if kc == 0 else {"adds": [bpm]}))
                            if kind == "v":
                                dst, coff, hd = tl[3], tl[4], tl[5]
                                vs, bvs = (vsa if hd == 64 else vsc).next()
                                c.op(act, lambda e: e.activation(vs[:, :, 0:hd],
                                                                 pm[:, 0:ncols].rearrange("p (h d) -> p h d", d=hd),
                                                                 AF.Copy), reads=[bpm], writes=[bvs])
                                c.dma(pool, dst[t0:t0 + 128, coff:coff + 4 * (hd + 1)],
                                      vs[:].rearrange("p h d -> p (h d)"), reads=[bvs])
                                continue
                            nh = 8 if kind == "qk" else 1
                            qbt, bqb = qbr.next()
                            rope_tile(pm, bpm, qbt, bqb, nh, tglob)
                            pq, bpq = pQ.next()
                            qs, bqs = qsr.next()
                            if kind == "qk":
                                head0 = tl[3]
                                for blk in range(4):
                                    c.op(pe, lambda e: e.transpose(pq[:, blk, :], qbt[:, blk * 128:(blk + 1) * 128],
                                                                   ident_b[:]),
                                         reads=[bqb, Bg], **({"writes": [bpq]} if blk == 0 else {"adds": [bpq]}))
                                c.op(dve, lambda e: e.tensor_copy(qs[:], pq[:]), reads=[bpq], writes=[bqs])
                                c.dma(pool, QT[head0 * 64:head0 * 64 + 512, t0:t0 + 128].rearrange("(b p) t -> p b t", p=128),
                                      qs[:], reads=[bqs])
                            else:
                                c.op(pe, lambda e: e.transpose(pq[0:64, 0, :], qbt[:, 0:64], ident_b[:]),
                                     reads=[bqb, Bg], writes=[bpq])
                                c.op(dve, lambda e: e.tensor_copy(qs[0:64, 0, :], pq[0:64, 0, :]), reads=[bpq], writes=[bqs])
                                c.dma(pool, QT[40 * 64:41 * 64, t0:t0 + 128], qs[0:64, 0, :], reads=[bqs])
                                w8, bw8 = w8r.next()
                                c.op(act, lambda e: e.mul(w8[:], pm[:, 64:72], 8.0 ** -0.5), reads=[bpm], writes=[bw8])
                                c.dma(pool, W8[t0:t0 + 128, :], w8[:], reads=[bw8])
                c.barrier()
            c.stack = gstack

        def phaseA(l):
            with ExitStack() as ps_:
                c.stack = ps_
                kt = c.sb("a_kt", [64, 12, NSLOT * 128], BF16)
                va = c.sb("a_va", [128, NSLOT, 12 * 65], BF16)
                bk = [Buf() for _ in range(NSLOT)]
                bv = [Buf() for _ in range(NSLOT)]
                qtr = Ring(c, "a_qt", 2, [64, 12, 128], BF16)
                er = Ring(c, "a_e", 5, [128, 512], BF16)
                pr_ = Ring(c, "a_p", 5, [128, 512], BF16)
                psr = Ring(c, "a_ps", 4, [128, 512], F32, psum=True)
                accr = Ring(c, "a_acc", 2, [128, 4, 65], F32, psum=True)
                por = Ring(c, "a_po", 2, [128, 2, 128], BF16, psum=True)
                rcr = Ring(c, "a_rc", 2, [128, 4], F32)
                obr = Ring(c, "a_ob", 2, [128, 256], BF16)
                osr = Ring(c, "a_os", 2, [128, 2, 128], BF16)
                for qb in range(NT):
                    slot = qb % NSLOT
                    tok0 = qb * 128
                    qt, bq = qtr.next()
                    for g in range(3):
                        c.dma(sp, kt[:, g * 4:(g + 1) * 4, slot * 128:(slot + 1) * 128],
                              QT3[:, g * 8 + 4:g * 8 + 8, tok0:tok0 + 128],
                              **({"writes": [bk[slot]]} if g == 0 else {"adds": [bk[slot]]}))
                        c.dma(sp, qt[:, g * 4:(g + 1) * 4, :], QT3[:, g * 8:g * 8 + 4, tok0:tok0 + 128],
                              **({"writes": [bq]} if g == 0 else {"adds": [bq]}))
                    c.dma(sp, va[:, slot, :], VA_A[tok0:tok0 + 128, :], writes=[bv[slot]])
                    acc, bacc = accr.next()
                    c.op(dve, lambda e: e.memset(acc[:], 0.0), writes=[bacc])
                    pairs = [(g, kb) for g in range(3) for kb in range(max(0, qb - A_WB[g]), qb + 1)]

                    def a_qk(i):
                        g, kb = pairs[i]
                        ks = kb % NSLOT
                        ps, bps = psr.next()
                        for h in range(4):
                            c.op(pe, lambda e: e.matmul(ps[:, h * 128:(h + 1) * 128],
                                                        kt[:, g * 4 + h, ks * 128:(ks + 1) * 128], qt[:, g * 4 + h, :],
                                                        start=True, stop=True),
                                 reads=[bk[ks], bq], **({"writes": [bps]} if h == 0 else {"adds": [bps]}))
                        return ps, bps

                    def a_mid(i, ps, bps):
                        g, kb = pairs[i]
                        ee, be = er.next()
                        c.op(act, lambda e: e.activation(ee[:], ps[:], AF.Exp, scale=0.125), reads=[bps], writes=[be])
                        pp, bp = pr_.next()
                        mk = maskA[:, mask_index(g, qb - kb), :]
                        for h in range(4):
                            c.op(dve, lambda e: e.tensor_tensor(pp[:, h * 128:(h + 1) * 128], ee[:, h * 128:(h + 1) * 128],
                                                                mk, ALU.mult),
                                 reads=[be, Bg], **({"writes": [bp]} if h == 0 else {"adds": [bp]}))
                        return pp, bp

                    def a_pv(i, pp, bp):
                        g, kb = pairs[i]
                        ks = kb % NSLOT
                        for h in range(4):
                            c.op(pe, lambda e: e.matmul(acc[:, h, :], pp[:, h * 128:(h + 1) * 128],
                                                        va[:, ks, (g * 4 + h) * 65:(g * 4 + h + 1) * 65],
                                                        start=False, stop=False, skip_group_check=True),
                                 reads=[bp, bv[ks]], upd=[bacc])

                    LA = 3
                    pend = []
                    for i in range(len(pairs)):
                        ps, bps = a_qk(i)
                        pend.append((i,) + a_mid(i, ps, bps))
                        if len(pend) > LA:
                            a_pv(*pend.pop(0))
                    for it in pend:
                        a_pv(*it)
                    rc, brc = rcr.next()
                    c.op(dve, lambda e: e.reciprocal(rc[:], acc[:, :, 64]), reads=[bacc], writes=[brc])
                    ob, bob = obr.next()
                    for h in range(4):
                        c.op(dve, lambda e: e.tensor_scalar(ob[:, h * 64:(h + 1) * 64], acc[:, h, 0:64], rc[:, h:h + 1],
                                                            None, ALU.mult),
                             reads=[bacc, brc], **({"writes": [bob]} if h == 0 else {"adds": [bob]}))
                    po, bpo = por.next()
                    for j in range(2):
                        c.op(pe, lambda e: e.transpose(po[:, j, :], ob[:, j * 128:(j + 1) * 128], ident_b[:]),
                             reads=[bob, Bg], **({"writes": [bpo]} if j == 0 else {"adds": [bpo]}))
                    os_, bos = osr.next()
                    c.op(act, lambda e: e.activation(os_[:], po[:], AF.Copy), reads=[bpo], writes=[bos])
                    c.dma(pool, OT[0:256, tok0:tok0 + 128].rearrange("(b p) t -> p b t", p=128), os_[:], reads=[bos])
                c.barrier()
            c.stack = gstack

        def phaseB1(l):
            with ExitStack() as ps_:
                c.stack = ps_
                kx = c.sb("b_kx", [64, S], BF16)
                bkx = Buf()
                c.dma(sp, kx[:], QT3[:, 40, :], writes=[bkx])
                qxr = Ring(c, "b_qx", 2, [64, 8, 128], BF16)
                wr = Ring(c, "b_w", 2, [128, 8], F32)
                dgr = Ring(c, "b_dg", 2, [128, 8, 128], BF16)
                Ir = Ring(c, "b_I", 2, [128, S], F32)
                rr = Ring(c, "b_r", 4, [128, 512], BF16)
                psr = Ring(c, "b_ps", 4, [128, 512], F32, psum=True)
                pacc = Ring(c, "b_pa", 2, [128, 512], F32, psum=True)
                Mr = Ring(c, "b_M", 2, [128, S], BF16)
                scr_ = Ring(c, "b_sc", 2, [128, 8], F32)
                hsr = Ring(c, "b_hs", 2, [128, NIT], F32)
                ptr = Ring(c, "b_pt", 2, [128, 4, 128], BF16, psum=True)
                mtr = Ring(c, "b_mt", 1, [128, NT, 128], BF16)
                def b1_finish(qb, M, bM):
                        mt, bmt = mtr.next()
                        nkb = qb + 1
                        for k0 in range(0, nkb, 4):
                            kn = min(4, nkb - k0)
                            pt, bpt = ptr.next()
                            for j in range(kn):
                                c.op(pe, lambda e: e.transpose(pt[:, j, :], M[:, (k0 + j) * 128:(k0 + j + 1) * 128], ident_b[:]),
                                     reads=[bM, Bg], **({"writes": [bpt]} if j == 0 else {"adds": [bpt]}))
                            c.op(act, lambda e: e.activation(mt[:, k0:k0 + kn, :], pt[:, 0:kn, :], AF.Copy), reads=[bpt],
                                 **({"writes": [bmt]} if k0 == 0 else {"adds": [bmt]}))
                        c.dma(pool, MT[qb, :, 0:nkb, :], mt[:, 0:nkb, :], reads=[bmt])

                prev_fin = None
                for qb in range(NT):
                    tok0 = qb * 128
                    nv = tok0 + 128
                    qx, bqx = qxr.next()
                    c.dma(sp, qx[:], QT3[:, 32:40, tok0:tok0 + 128], writes=[bqx])
                    w, bw = wr.next()
                    c.dma(sp, w[:], W8[tok0:tok0 + 128, :], writes=[bw])
                    dg, bdg = dgr.next()
                    for h in range(8):
                        c.op(act, lambda e: e.activation(dg[:, h, :], cstf[:, C_ID:C_ID + 128], AF.Identity, scale=w[:, h:h + 1]),
                             reads=[bw, Bc], **({"writes": [bdg]} if h == 0 else {"adds": [bdg]}))
                    I_, bI = Ir.next()
                    nst = (nv + 511) // 512
                    firstI = True
                    for s_t in range(nst):
                        s0 = s_t * 512
                        wd = min(512, nv - s0)
                        pa, bpa = pacc.next()

                        def lg(h):
                            ps, bps = psr.next()
                            c.op(pe, lambda e: e.matmul(ps[:, 0:wd], qx[:, h, :], kx[:, s0:s0 + wd], start=True, stop=True),
                                 reads=[bqx, bkx], writes=[bps])
                            r, br = rr.next()
                            c.op(act, lambda e: e.activation(r[:, 0:wd], ps[:, 0:wd], AF.Relu, scale=0.125),
                                 reads=[bps], writes=[br])
                            return r, br

                        def dgm(h, r, br):
                            c.op(pe, lambda e: e.matmul(pa[:, 0:wd], dg[:, h, :], r[:, 0:wd], start=(h == 0), stop=(h == 7)),
                                 reads=[bdg, br], **({"writes": [bpa]} if h == 0 else {"adds": [bpa]}))

                        pend = []
                        for h in range(8):
                            pend.append((h,) + lg(h))
                            if len(pend) > 2:
                                dgm(*pend.pop(0))
                        for it in pend:
                            dgm(*it)
                        c.op(act, lambda e: e.activation(I_[:, s0:s0 + wd], pa[:, 0:wd], AF.Copy), reads=[bpa],
                             **({"writes": [bI]} if firstI else {"adds": [bI]}))
                        firstI = False
                    M, bM = Mr.next()
                    sc, bsc = scr_.next()
                    hs, bhs = hsr.next()
                    if qb >= 2:
                        c.op(dve, lambda e: e.tensor_reduce(sc[:, 0:1], I_[:, 0:nv], AX.X, ALU.max,
                                                            apply_absolute_value=True), reads=[bI], writes=[bsc])
                    c.op(dve, lambda e: e.tensor_tensor(I_[:, tok0:nv], I_[:, tok0:nv], negtri, ALU.add),
                         reads=[Bc], upd=[bI])
                    if qb < 2:
                        c.op(dve, lambda e: e.tensor_scalar(M[:, 0:nv], I_[:, 0:nv], -1e29, None, ALU.is_ge),
                             reads=[bI], writes=[bM])
                    else:
                        c.op(dve, lambda e: e.tensor_scalar(sc[:, 0:1], sc[:, 0:1], 1.0, 1e-3, ALU.mult, ALU.add), upd=[bsc])
                        c.op(dve, lambda e: e.tensor_scalar(sc[:, 1:2], sc[:, 0:1], -1.0, None, ALU.mult), upd=[bsc])
                        c.op(dve, lambda e: e.tensor_scalar(sc[:, 4:5], sc[:, 0:1], 2.0, None, ALU.mult), upd=[bsc])
                        c.op(dve, lambda e: e.tensor_scalar(hs[:], cstf[:, C_POW:C_POW + NIT], sc[:, 4:5], None, ALU.mult),
                             reads=[bsc, Bc], writes=[bhs])
                        for k in range(NIT):
                            c.op(dve, lambda e: e.tensor_tensor(sc[:, 2:3], sc[:, 1:2], hs[:, k:k + 1], ALU.add),
                                 reads=[bhs], upd=[bsc])
                            c.op(dve, lambda e: e.tensor_scalar(M[:, 0:nv], I_[:, 0:nv], sc[:, 2:3], None, ALU.is_ge,
                                                                ALU.add, accum_out=sc[:, 3:4]),
                                 reads=[bI], writes=[bM], upd=[bsc])
                            c.op(dve, lambda e: e.scalar_tensor_tensor(sc[:, 4:5], sc[:, 3:4], TOPK - 0.5, hs[:, k:k + 1],
                                                                       ALU.is_ge, ALU.mult), reads=[bhs], upd=[bsc])
                            c.op(dve, lambda e: e.tensor_tensor(sc[:, 1:2], sc[:, 1:2], sc[:, 4:5], ALU.add), upd=[bsc])
                        c.op(dve, lambda e: e.tensor_scalar(M[:, 0:nv], I_[:, 0:nv], sc[:, 1:2], None, ALU.is_ge),
                             reads=[bI, bsc], writes=[bM])
                    if prev_fin is not None:
                        b1_finish(*prev_fin)
                    prev_fin = (qb, M, bM)
                b1_finish(*prev_fin)
                c.barrier()
            c.stack = gstack

        def phaseB2(l):
            with ExitStack() as ps_:
                c.stack = ps_
                kt = c.sb("b2_kt", [64, 4, S], BF16)
                va = c.sb("b2_va", [128, NT, 4 * 65], BF16)
                bkt, bva = Buf(), Buf()
                c.dma(sp, kt[:], QT3[:, 28:32, :], writes=[bkt])
                vbv = VA_B.rearrange("(n p) c -> p n c", p=128)
                for n0 in range(0, NT, 8):
                    c.dma(sp, va[:, n0:n0 + 8, :], vbv[:, n0:n0 + 8, :], **({"writes": [bva]} if n0 == 0 else {"adds": [bva]}))
                qtr = Ring(c, "b2_qt", 2, [64, 4, 128], BF16)
                mtr = Ring(c, "b2_mt", 2, [128, NT, 128], BF16)
                er = Ring(c, "b2_e", 5, [128, 512], BF16)
                pr_ = Ring(c, "b2_p", 5, [128, 512], BF16)
                psr = Ring(c, "b2_ps", 4, [128, 512], F32, psum=True)
                accr = Ring(c, "b2_acc", 2, [128, 4, 65], F32, psum=True)
                por = Ring(c, "b2_po", 2, [128, 2, 128], BF16, psum=True)
                rcr = Ring(c, "b2_rc", 2, [128, 4], F32)
                obr = Ring(c, "b2_ob", 2, [128, 256], BF16)
                osr = Ring(c, "b2_os", 2, [128, 2, 128], BF16)
                for qb in range(NT):
                    tok0 = qb * 128
                    nkb = qb + 1
                    qt, bq = qtr.next()
                    c.dma(sp, qt[:], QT3[:, 24:28, tok0:tok0 + 128], writes=[bq])
                    mt, bmt = mtr.next()
                    c.dma(sp, mt[:, 0:nkb, :], MT[qb, :, 0:nkb, :], writes=[bmt])
                    acc, bacc = accr.next()
                    c.op(dve, lambda e: e.memset(acc[:], 0.0), writes=[bacc])
                    def b_qk(kb):
                        ps, bps = psr.next()
                        for h in range(4):
                            c.op(pe, lambda e: e.matmul(ps[:, h * 128:(h + 1) * 128], kt[:, h, kb * 128:(kb + 1) * 128],
                                                        qt[:, h, :], start=True, stop=True),
                                 reads=[bkt, bq], **({"writes": [bps]} if h == 0 else {"adds": [bps]}))
                        return ps, bps

                    def b_mid(kb, ps, bps):
                        ee, be = er.next()
                        c.op(act, lambda e: e.activation(ee[:], ps[:], AF.Exp, scale=0.125), reads=[bps], writes=[be])
                        pp, bp = pr_.next()
                        mk = mt[:, kb, :]
                        for h in range(4):
                            c.op(dve, lambda e: e.tensor_tensor(pp[:, h * 128:(h + 1) * 128], ee[:, h * 128:(h + 1) * 128],
                                                                mk, ALU.mult),
                                 reads=[be, bmt], **({"writes": [bp]} if h == 0 else {"adds": [bp]}))
                        return pp, bp

                    def b_pv(kb, pp, bp):
                        for h in range(4):
                            c.op(pe, lambda e: e.matmul(acc[:, h, :], pp[:, h * 128:(h + 1) * 128],
                                                        va[:, kb, h * 65:(h + 1) * 65],
                                                        start=False, stop=False, skip_group_check=True),
                                 reads=[bp, bva], upd=[bacc])

                    LA = 3
                    pend = []
                    for kb in range(nkb):
                        ps, bps = b_qk(kb)
                        pend.append((kb,) + b_mid(kb, ps, bps))
                        if len(pend) > LA:
                            b_pv(*pend.pop(0))
                    for it in pend:
                        b_pv(*it)
                    rc, brc = rcr.next()
                    c.op(dve, lambda e: e.reciprocal(rc[:], acc[:, :, 64]), reads=[bacc], writes=[brc])
                    ob, bob = obr.next()
                    for h in range(4):
                        c.op(dve, lambda e: e.tensor_scalar(ob[:, h * 64:(h + 1) * 64], acc[:, h, 0:64], rc[:, h:h + 1],
                                                            None, ALU.mult),
                             reads=[bacc, brc], **({"writes": [bob]} if h == 0 else {"adds": [bob]}))
                    po, bpo = por.next()
                    for j in range(2):
                        c.op(pe, lambda e: e.transpose(po[:, j, :], ob[:, j * 128:(j + 1) * 128], ident_b[:]),
                             reads=[bob, Bg], **({"writes": [bpo]} if j == 0 else {"adds": [bpo]}))
                    os_, bos = osr.next()
                    c.op(act, lambda e: e.activation(os_[:], po[:], AF.Copy), reads=[bpo], writes=[bos])
                    c.dma(pool, OT[256:512, tok0:tok0 + 128].rearrange("(b p) t -> p b t", p=128), os_[:], reads=[bos])
                c.barrier()
            c.stack = gstack

        def phaseC(l):
            lam_init = 0.8 - 0.6 * math.exp(-0.3 * l)
            with ExitStack() as ps_:
                c.stack = ps_
                ktr = Ring(c, "c_kt", 2, [64, 2, S], BF16)
                var = Ring(c, "c_va", 2, [128, NT, 129], BF16)
                qtr = Ring(c, "c_qt", 2, [64, 2, 512], BF16)
                er = Ring(c, "c_e", 4, [128, 512], BF16)
                psr = Ring(c, "c_ps", 3, [128, 512], F32, psum=True)
                accs = [c.ps(f"c_acc{i}", [128, 3, 129], F32) for i in range(3)]
                bacc = Buf()
                por = Ring(c, "c_po", 2, [128, 128], BF16, psum=True)
                sg = c.sb("c_sg", [128, 128], F32)
                bsg = Buf()
                c.dma(sp, sg[:], subln[l].partition_broadcast(128), writes=[bsg])
                tri = maskA[:, 0, :]
                smr = Ring(c, "c_sm", 2, [128, 8], F32)
                o1r = Ring(c, "c_o1", 2, [128, 128], F32)
                o2r = Ring(c, "c_o2", 2, [128, 128], F32)
                jr = Ring(c, "c_j", 2, [128, 128], F32)
                obr = Ring(c, "c_ob", 2, [128, 128], BF16)
                osr = Ring(c, "c_os", 2, [128, 512], BF16)

                def accv(cc, j):
                    i = cc * 4 + j
                    return accs[i // 3][:, i % 3, :]

                for h in range(4):
                    kt, bkt = ktr.next()
                    va, bva = var.next()
                    c.dma(sp, kt[:], QT3[:, 49 + 2 * h:51 + 2 * h, :], writes=[bkt])
                    vcv = VA_C[:, h * 129:(h + 1) * 129].rearrange("(n p) c -> p n c", p=128)
                    for n0 in range(0, NT, 8):
                        c.dma(sp, va[:, n0:n0 + 8, :], vcv[:, n0:n0 + 8, :], **({"writes": [bva]} if n0 == 0 else {"adds": [bva]}))
                    for qt_i in range(S // 512):
                        q0 = qt_i * 512
                        qt, bq = qtr.next()
                        c.dma(sp, qt[:], QT3[:, 41 + 2 * h:43 + 2 * h, q0:q0 + 512], writes=[bq])
                        for a in accs:
                            c.op(dve, lambda e: e.memset(a[:], 0.0), **({"writes": [bacc]} if a is accs[0] else {"upd": [bacc]}))
                        nkb = (q0 + 512) // 128
                        steps = [(kb, cc) for kb in range(nkb) for cc in range(2)]

                        def c_qk(kb, cc):
                            ps, bps = psr.next()
                            c.op(pe, lambda e: e.matmul(ps[:], kt[:, cc, kb * 128:(kb + 1) * 128], qt[:, cc, :],
                                                        start=True, stop=True), reads=[bkt, bq], writes=[bps])
                            return ps, bps

                        def c_mid(kb, cc, ps, bps):
                            jk = kb - q0 // 128
                            ee, be = er.next()
                            c.op(act, lambda e: e.activation(ee[:], ps[:], AF.Exp, scale=0.125), reads=[bps], writes=[be])
                            if jk >= 0:
                                c.op(dve, lambda e: e.tensor_tensor(ee[:, jk * 128:(jk + 1) * 128],
                                                                    ee[:, jk * 128:(jk + 1) * 128], tri, ALU.mult),
                                     reads=[Bg], upd=[be])
                            return ee, be

                        def c_pv(kb, cc, ee, be):
                            jk = kb - q0 // 128
                            for j in range(max(jk, 0), 4):
                                c.op(pe, lambda e: e.matmul(accv(cc, j), ee[:, j * 128:(j + 1) * 128], va[:, kb, :],
                                                            start=False, stop=False, skip_group_check=True),
                                     reads=[be, bva], upd=[bacc])

                        LA = 2
                        pend = []
                        for (kb, cc) in steps:
                            ps, bps = c_qk(kb, cc)
                            pend.append((kb, cc) + c_mid(kb, cc, ps, bps))
                            if len(pend) > LA:
                                c_pv(*pend.pop(0))
                        for it in pend:
                            c_pv(*it)
                        os_, bos = osr.next()
                        for j in range(4):
                            sm, bsm = smr.next()
                            a1 = accv(0, j)
                            a2 = accv(1, j)
                            c.op(dve, lambda e: e.reciprocal(sm[:, 0:1], a1[:, 128:129]), reads=[bacc], writes=[bsm])
                            c.op(dve, lambda e: e.reciprocal(sm[:, 1:2], a2[:, 128:129]), reads=[bacc], upd=[bsm])
                            c.op(dve, lambda e: e.tensor_tensor(sm[:, 1:2], sm[:, 1:2], lamt[:, l, 1:2], ALU.mult),
                                 reads=[Bg], upd=[bsm])
                            o1, bo1 = o1r.next()
                            c.op(dve, lambda e: e.tensor_scalar(o1[:], a1[:, 0:128], sm[:, 0:1], None, ALU.mult),
                                 reads=[bacc, bsm], writes=[bo1])
                            o2, bo2 = o2r.next()
                            c.op(dve, lambda e: e.scalar_tensor_tensor(o2[:], a2[:, 0:128], sm[:, 1:2], o1[:],
                                                                       ALU.mult, ALU.add),
                                 reads=[bacc, bsm, bo1], writes=[bo2])
                            c.op(dve, lambda e: e.memset(sm[:, 2:3], 0.0), upd=[bsm])
                            jj, bjj = jr.next()
                            c.op(act, lambda e: e.activation(jj[:], o2[:], AF.Square, accum_out=sm[:, 2:3]),
                                 reads=[bo2], writes=[bjj], upd=[bsm])
                            c.op(dve, lambda e: e.tensor_scalar(sm[:, 3:4], sm[:, 2:3], 1.0 / 128, EPS, ALU.mult, ALU.add),
                                 upd=[bsm])
                            c.op(act, lambda e: e.activation(sm[:, 4:5], sm[:, 3:4], AF.Sqrt), upd=[bsm])
                            c.op(dve, lambda e: e.reciprocal(sm[:, 4:5], sm[:, 4:5]), upd=[bsm])
                            c.op(dve, lambda e: e.tensor_scalar(sm[:, 4:5], sm[:, 4:5], 1.0 - lam_init, None, ALU.mult),
                                 upd=[bsm])
                            ob, bob = obr.next()
                            c.op(dve, lambda e: e.scalar_tensor_tensor(ob[:], o2[:], sm[:, 4:5], sg[:], ALU.mult, ALU.mult),
                                 reads=[bo2, bsm, bsg], writes=[bob])
                            po, bpo = por.next()
                            c.op(pe, lambda e: e.transpose(po[:], ob[:], ident_b[:]), reads=[bob, Bg], writes=[bpo])
                            c.op(act, lambda e: e.activation(os_[:, j * 128:(j + 1) * 128], po[:], AF.Copy), reads=[bpo],
                                 **({"writes": [bos]} if j == 0 else {"adds": [bos]}))
                        c.dma(pool, OT[512 + h * 128:512 + (h + 1) * 128, q0:q0 + 512], os_[:], reads=[bos])
                c.barrier()
            c.stack = gstack

        def bcast_load(dst, src_vec_ap, bdst):
            c.dma(sp, dst, src_vec_ap.partition_broadcast(128), writes=[bdst])

        def phaseM(l, xin, xmid):
            with ExitStack() as ps_:
                c.stack = ps_
                st = {"wst": Ring(c, "m_wst", 2, [128, 4, 1024], F32)}
                wa = c.sb("m_wa", [128, 2, 1024], BF16)
                wbb = c.sb("m_wb", [128, 2, 1024], BF16)
                wc = c.sb("m_wc", [128, 4, 1024], BF16)
                wo = c.sb("m_wo", [128, 8, 1024], BF16)
                bwa, bwb_, bwc, bwo = Buf(), Buf(), Buf(), Buf()
                load_w(st, wa, bwa, w_br_a[l].rearrange("(kc p) n -> p kc n", p=128), 2, 1024)
                load_w(st, wbb, bwb_, w_br_b[l].rearrange("(kc p) n -> p kc n", p=128), 2, 1024)
                load_w(st, wc, bwc, w_br_c[l].rearrange("(kc p) n -> p kc n", p=128), 4, 1024)
                load_w(st, wo, bwo, w_out[l].rearrange("(kc p) n -> p kc n", p=128), 8, 1024)
                gp = c.sb("m_gp", [128, D], F32)
                bgp = Buf()
                bcast_load(gp[:], norms[l, 8:16, :].rearrange("a b -> (a b)"), bgp)
                otr = Ring(c, "m_ot", 2, [128, 8, 512], BF16)
                gtr = Ring(c, "m_gt", 2, [128, 24, 512], BF16)
                yT = c.sb("m_yT", [128, 8, 512], BF16)
                byT = Buf()
                pbr = Ring(c, "m_pb", 4, [128, 512], F32, psum=True)
                phr = Ring(c, "m_ph", 2, [128, 1024], F32, psum=True)
                t1r = Ring(c, "m_t1", 2, [128, 512], F32)
                t2r = Ring(c, "m_t2", 2, [128, 512], F32)
                xr = Ring(c, "m_x", 2, [128, D], F32)
                hr = Ring(c, "m_h", 2, [128, D], F32)
                jr = Ring(c, "m_j", 1, [128, D], BF16)
                ssr = Ring(c, "m_ss", 2, [128, 4], F32)
                OTv = OT.rearrange("(kc p) t -> p kc t", p=128)
                GTv = GT.rearrange("(kc p) t -> p kc t", p=128)
                for tg in range(S // 512):
                    t0 = tg * 512
                    ot, bot = otr.next()
                    c.dma(sp, ot[:], OTv[:, :, t0:t0 + 512], writes=[bot])
                    gt, bgt = gtr.next()
                    c.dma(sp, gt[:], GTv[:, :, t0:t0 + 512], writes=[bgt])
                    for n in range(8):
                        pa, bpa = pbr.next()
                        for kc in range(2):
                            c.op(pe, lambda e: e.matmul(pa[:], wa[:, kc, n * 128:(n + 1) * 128], ot[:, kc, :],
                                                        start=(kc == 0), stop=(kc == 1)),
                                 reads=[bwa, bot], **({"writes": [bpa]} if kc == 0 else {"adds": [bpa]}))
                        pb, bpb = pbr.next()
                        for kc in range(2):
                            c.op(pe, lambda e: e.matmul(pb[:], wbb[:, kc, n * 128:(n + 1) * 128], ot[:, 2 + kc, :],
                                                        start=(kc == 0), stop=(kc == 1)),
                                 reads=[bwb_, bot], **({"writes": [bpb]} if kc == 0 else {"adds": [bpb]}))
                        pc, bpc = pbr.next()
                        for kc in range(4):
                            c.op(pe, lambda e: e.matmul(pc[:], wc[:, kc, n * 128:(n + 1) * 128], ot[:, 4 + kc, :],
                                                        start=(kc == 0), stop=(kc == 3)),
                                 reads=[bwc, bot], **({"writes": [bpc]} if kc == 0 else {"adds": [bpc]}))
                        t1, bt1 = t1r.next()
                        t2, bt2 = t2r.next()
                        c.op(dve, lambda e: e.tensor_tensor(t1[:], pa[:], gt[:, n, :], ALU.mult), reads=[bpa, bgt], writes=[bt1])
                        c.op(dve, lambda e: e.tensor_tensor(t2[:], pb[:], gt[:, 8 + n, :], ALU.mult), reads=[bpb, bgt], writes=[bt2])
                        c.op(pl, lambda e: e.tensor_tensor(t1[:], t1[:], t2[:], ALU.add), reads=[bt2], upd=[bt1])
                        c.op(dve, lambda e: e.tensor_tensor(t2[:], pc[:], gt[:, 16 + n, :], ALU.mult), reads=[bpc, bgt], upd=[bt2])
                        c.op(pl, lambda e: e.tensor_tensor(yT[:, n, :], t1[:], t2[:], ALU.add), reads=[bt1, bt2],
                             **({"writes": [byT]} if n == 0 else {"adds": [byT]}))
                    for j in range(4):
                        tj = t0 + j * 128
                        ph, bph = phr.next()
                        for half in range(2):
                            for kc in range(8):
                                c.op(pe, lambda e: e.matmul(ph[:, half * 512:(half + 1) * 512], yT[:, kc, j * 128:(j + 1) * 128],
                                                            wo[:, kc, half * 512:(half + 1) * 512],
                                                            start=(kc == 0), stop=(kc == 7)),
                                     reads=[byT, bwo], **({"writes": [bph]} if (kc == 0 and half == 0) else {"adds": [bph]}))
                        xt, bx = xr.next()
                        c.dma(sp, xt[:], xin[tj:tj + 128, :], writes=[bx])
                        ss, bs = ssr.next()
                        c.op(dve, lambda e: e.memset(ss[:], 0.0), writes=[bs])
                        jk, bj = jr.next()
                        c.op(act, lambda e: e.activation(jk[:], ph[:], AF.Square, accum_out=ss[:, 0:1]),
                             reads=[bph], writes=[bj], upd=[bs])
                        c.op(dve, lambda e: e.tensor_scalar(ss[:, 1:2], ss[:, 0:1], 1.0 / D, EPS, ALU.mult, ALU.add), upd=[bs])
                        c.op(act, lambda e: e.activation(ss[:, 2:3], ss[:, 1:2], AF.Sqrt), upd=[bs])
                        c.op(dve, lambda e: e.reciprocal(ss[:, 2:3], ss[:, 2:3]), upd=[bs])
                        hh, bh = hr.next()
                        c.op(dve, lambda e: e.scalar_tensor_tensor(hh[:], ph[:], ss[:, 2:3], gp[:], ALU.mult, ALU.mult),
                             reads=[bph, bs, bgp], writes=[bh])
                        c.op(pl, lambda e: e.tensor_tensor(hh[:], hh[:], xt[:], ALU.add), reads=[bx], upd=[bh])
                        c.dma(pool, xmid[tj:tj + 128, :], hh[:], reads=[bh])
                c.barrier()
            c.stack = gstack

        def phaseF(l, xmid, xout):
            TF = 512
            GC = 0.7978845608028654
            with ExitStack() as ps_:
                c.stack = ps_
                st = {
                    "xr": Ring(c, "f_xr", 2, [128, D], F32),
                    "ss": Ring(c, "f_ss", 2, [128, 4], F32),
                    "junk": Ring(c, "f_junk", 1, [128, D], BF16),
                    "xb": Ring(c, "f_xb", 2, [128, D], BF16),
                    "pT": Ring(c, "f_pT", 2, [128, 8, 128], BF16, psum=True),
                    "wst": Ring(c, "f_wst", 2, [128, 8, 512], F32),
                }
                wd = c.sb("f_wd", [128, 22, 1024], BF16)
                bwd = Buf()
                wdv = w_down[l].rearrange("(kc p) n -> p kc n", p=128)
                firstw = True
                for k0 in range(0, 22, 8):
                    kn = min(8, 22 - k0)
                    for half in range(2):
                        ws, bws = st["wst"].next()
                        c.dma(sp, ws[:, 0:kn, :], wdv[:, k0:k0 + kn, half * 512:(half + 1) * 512], writes=[bws])
                        for kc in range(kn):
                            c.op(act, lambda e: e.activation(wd[:, k0 + kc, half * 512:(half + 1) * 512], ws[:, kc, :], AF.Copy),
                                 reads=[bws], **({"writes": [bwd]} if firstw else {"adds": [bwd]}))
                            firstw = False
                gp = c.sb("f_gp", [128, D], F32)
                bgp = Buf()
                bcast_load(gp[:], norms[l, 24:32, :].rearrange("a b -> (a b)"), bgp)
                gain = gcols[:, l, 16:24]
                wur = Ring(c, "f_wu", 2, [128, 8, 256], BF16)
                xnT = c.sb("f_xnT", [128, 8, TF], BF16)
                bxn = [Buf() for _ in range(TF // 128)]
                halo = c.sb("f_halo", [128, 44, 2], F32)
                bhalo = [Buf() for _ in range(44)]
                c.op(dve, lambda e: e.memset(halo[:], 0.0), writes=bhalo)
                aT = c.sb("f_aT", [128, 22, TF], BF16)
                baT = Buf()
                pur = Ring(c, "f_pu", 4, [128, 512], F32, psum=True)
                phr = Ring(c, "f_ph", 1, [128, 1024], F32, psum=True)
                cgr = Ring(c, "f_cg", 2, [128, 512], F32)
                cur = Ring(c, "f_cu", 2, [128, 512], F32)
                t1r = Ring(c, "f_t1", 2, [128, 512], F32)
                t2r = Ring(c, "f_t2", 2, [128, 512], F32)
                hr = Ring(c, "f_h", 2, [128, D], F32)
                jr = Ring(c, "f_j", 1, [128, D], BF16)
                ssr = Ring(c, "f_ss2", 2, [128, 4], F32)
                wuv = w_up[l].rearrange("(kc p) n -> p kc n", p=128)

                def conv(pu, bpu, ch, dst, bdst):
                    w0 = cw[:, l, ch:ch + 1]
                    w1 = cw[:, l, 44 + ch:44 + ch + 1]
                    w2 = cw[:, l, 88 + ch:88 + ch + 1]
                    c.op(act, lambda e: e.activation(dst[:], pu[:], AF.Identity, bias=cb[:, l, ch:ch + 1], scale=w2),
                         reads=[bpu, Bg], writes=[bdst])
                    c.op(dve, lambda e: e.scalar_tensor_tensor(dst[:, 1:TF], pu[:, 0:TF - 1], w1, dst[:, 1:TF], ALU.mult, ALU.add),
                         reads=[bpu, Bg], upd=[bdst])
                    c.op(dve, lambda e: e.scalar_tensor_tensor(dst[:, 2:TF], pu[:, 0:TF - 2], w0, dst[:, 2:TF], ALU.mult, ALU.add),
                         reads=[bpu, Bg], upd=[bdst])
                    c.op(dve, lambda e: e.scalar_tensor_tensor(dst[:, 0:1], halo[:, ch, 1:2], w1, dst[:, 0:1], ALU.mult, ALU.add),
                         reads=[bhalo[ch], Bg], upd=[bdst])
                    c.op(dve, lambda e: e.scalar_tensor_tensor(dst[:, 0:2], halo[:, ch, 0:2], w0, dst[:, 0:2], ALU.mult, ALU.add),
                         reads=[bhalo[ch], Bg], upd=[bdst])
                    c.op(dve, lambda e: e.tensor_copy(halo[:, ch, :], pu[:, TF - 2:TF]), reads=[bpu], writes=[bhalo[ch]])

                for tg in range(S // TF):
                    t0 = tg * TF
                    for tt in range(TF // 128):
                        norm_transpose(st, xmid[t0 + tt * 128:t0 + (tt + 1) * 128, :],
                                       xnT[:, :, tt * 128:(tt + 1) * 128], bxn[tt], True)
                    for cp in range(22):
                        wu, bwu = wur.next()
                        ws, bws = st["wst"].next()
                        c.dma(sp, ws[:, :, 0:128], wuv[:, :, cp * 128:(cp + 1) * 128], writes=[bws])
                        c.dma(sp, ws[:, :, 128:256], wuv[:, :, DFF + cp * 128:DFF + (cp + 1) * 128], adds=[bws])
                        for kc in range(8):
                            c.op(act, lambda e: e.activation(wu[:, kc, :], ws[:, kc, 0:256], AF.Identity, scale=gain[:, kc:kc + 1]),
                                 reads=[bws, Bg], **({"writes": [bwu]} if kc == 0 else {"adds": [bwu]}))
                        pg, bpg = pur.next()
                        pu, bpu = pur.next()
                        for (pp_, bpp, off) in ((pg, bpg, 0), (pu, bpu, 128)):
                            for kc in range(8):
                                c.op(pe, lambda e: e.matmul(pp_[:], wu[:, kc, off:off + 128], xnT[:, kc, :],
                                                            start=(kc == 0), stop=(kc == 7)),
                                     reads=[bwu] + bxn, **({"writes": [bpp]} if kc == 0 else {"adds": [bpp]}))
                        cg, bcg = cgr.next()
                        cu, bcu = cur.next()
                        conv(pg, bpg, cp, cg, bcg)
                        conv(pu, bpu, 22 + cp, cu, bcu)
                        t1, bt1 = t1r.next()
                        t2, bt2 = t2r.next()
                        c.op(act, lambda e: e.activation(t1[:], cg[:], AF.Square), reads=[bcg], writes=[bt1])
                        c.op(dve, lambda e: e.tensor_scalar(t1[:], t1[:], 0.044715, 1.0, ALU.mult, ALU.add), upd=[bt1])
                        c.op(pl, lambda e: e.tensor_tensor(t1[:], t1[:], cg[:], ALU.mult), reads=[bcg], upd=[bt1])
                        c.op(act, lambda e: e.activation(t2[:], t1[:], AF.Sigmoid, scale=2.0 * GC), reads=[bt1], writes=[bt2])
                        c.op(pl, lambda e: e.tensor_tensor(t2[:], t2[:], cg[:], ALU.mult), reads=[bcg], upd=[bt2])
                        c.op(dve, lambda e: e.tensor_tensor(aT[:, cp, :], t2[:], cu[:], ALU.mult), reads=[bt2, bcu],
                             **({"writes": [baT]} if cp == 0 else {"adds": [baT]}))
                    for j in range(TF // 128):
                        tj = t0 + j * 128
                        ph, bph = phr.next()
                        for half in range(2):
                            for kc in range(22):
                                c.op(pe, lambda e: e.matmul(ph[:, half * 512:(half + 1) * 512], aT[:, kc, j * 128:(j + 1) * 128],
                                                            wd[:, kc, half * 512:(half + 1) * 512],
                                                            start=(kc == 0), stop=(kc == 21)),
                                     reads=[baT, bwd], **({"writes": [bph]} if (kc == 0 and half == 0) else {"adds": [bph]}))
                        ss, bs = ssr.next()
                        c.op(dve, lambda e: e.memset(ss[:], 0.0), writes=[bs])
                        jk, bj = jr.next()
                        c.op(act, lambda e: e.activation(jk[:], ph[:], AF.Square, accum_out=ss[:, 0:1]),
                             reads=[bph], writes=[bj], upd=[bs])
                        c.op(dve, lambda e: e.tensor_scalar(ss[:, 1:2], ss[:, 0:1], 1.0 / D, EPS, ALU.mult, ALU.add), upd=[bs])
                        c.op(act, lambda e: e.activation(ss[:, 2:3], ss[:, 1:2], AF.Sqrt), upd=[bs])
                        c.op(dve, lambda e: e.reciprocal(ss[:, 2:3], ss[:, 2:3]), upd=[bs])
                        hh, bh = hr.next()
                        c.op(dve, lambda e: e.scalar_tensor_tensor(hh[:], ph[:], ss[:, 2:3], gp[:], ALU.mult, ALU.mult),
                             reads=[bph, bs, bgp], writes=[bh])
                        xt, bx = st["xr"].next()
                        c.dma(sp, xt[:], xmid[tj:tj + 128, :], writes=[bx])
                        c.op(pl, lambda e: e.tensor_tensor(hh[:], hh[:], xt[:], ALU.add), reads=[bx], upd=[bh])
                        c.dma(pool, xout[tj:tj + 128, :], hh[:], reads=[bh])
                c.barrier()
            c.stack = gstack

        cur = x_in
        for l in range(depth):
            last = (l == depth - 1)
            if "1" in phases:
                phase1(l, cur)
            if "A" in phases:
                phaseA(l)
            if "b" in phases:
                phaseB1(l)
            if "B" in phases:
                phaseB2(l)
            if "C" in phases:
                phaseC(l)
            if "M" in phases:
                phaseM(l, cur, X1)
            if "F" in phases:
                phaseF(l, X1, y_out if last else X2)
            cur = X2
        c.barrier()
    return nc


def make_in_maps(inputs, S):
    cst = make_consts()
    f = lambda a: np.ascontiguousarray(np.asarray(a, dtype=np.float32))
    shared = {
        "w_in": f(inputs["w_in"]), "w_br_a": f(inputs["w_br_a"]), "w_br_b": f(inputs["w_br_b"]),
        "w_br_c": f(inputs["w_br_c"]), "w_out": f(inputs["w_out"]),
        "lam": np.ascontiguousarray(np.stack([f(inputs["lam_q1"]), f(inputs["lam_k1"]), f(inputs["lam_q2"]),
                                              f(inputs["lam_k2"])], axis=1)),
        "subln_g": f(inputs["subln_g"]),
        "norms": np.ascontiguousarray(np.stack([f(inputs["norm_mix_pre"]), f(inputs["norm_mix_post"]),
                                                f(inputs["norm_ffn_pre"]), f(inputs["norm_ffn_post"])],
                                               axis=1).reshape(DEPTH, 32, 128)),
        "w_up": f(inputs["w_ffn_up"]),
        "conv_w": np.ascontiguousarray(f(inputs["conv_w"]).reshape(DEPTH, 132, 128)),
        "conv_b": np.ascontiguousarray(f(inputs["conv_b"]).reshape(DEPTH, 44, 128)),
        "w_down": f(inputs["w_ffn_down"]),
        "cst": cst,
    }
    x = f(inputs["x"])
    pos = np.ascontiguousarray(np.asarray(inputs["positions"], dtype=np.int32))
    maps = []
    for b in range(x.shape[0]):
        m = dict(shared)
        m["x"] = np.ascontiguousarray(x[b])
        m["pos"] = np.ascontiguousarray(pos[b].reshape(S // 128, 128))
        maps.append(m)
    return maps


def kernel(**inputs):
    x = np.asarray(inputs["x"])
    B, S, _ = x.shape
    nc = build(S)
    maps = make_in_maps(inputs, S)
    res = run_bass_kernel_spmd(nc, maps, core_ids=list(range(B)))
    out = np.stack([np.asarray(r["y"], dtype=np.float32) for r in res.results], axis=0)
    return out
```

```python
import math
import os as _os
from contextlib import ExitStack
import numpy as np
import concourse.bass as bass
import concourse.mybir as mybir
from concourse.bass_utils import run_bass_kernel_spmd

F32 = mybir.dt.float32
BF16 = mybir.dt.bfloat16
I32 = mybir.dt.int32
AF = mybir.ActivationFunctionType
ALU = mybir.AluOpType
AX = mybir.AxisListType

D = 1024
IN_W = 8264
DFF = 2816
NCORES = 8
DEPTH = 2
EPS = 1e-6
A_WB = (1, 4, 16)
A_PAIRS = ((128, 1), (512, 4), (2048, 16))
NSLOT = 17
NIT = 16
TOPK = 256
NMASK = 24
C_ID = 0
C_MASK = 128
C_NEG = C_MASK + NMASK * 128
C_POW = C_NEG + 128
C_INV = C_POW + NIT
NCST = C_INV + 8


def make_consts():
    c = np.zeros((128, NCST), np.float32)
    c[:, C_ID:C_ID + 128] = np.eye(128, dtype=np.float32)
    s = np.arange(128)[:, None]
    q = np.arange(128)[None, :]
    mi = 0
    for g, (w, d) in enumerate(A_PAIRS):
        for dl in range(A_WB[g] + 1):
            dist = 128 * dl + q - s
            m = (dist >= 0) & (dist <= w) & (dist % d == 0)
            c[:, C_MASK + mi * 128:C_MASK + (mi + 1) * 128] = m.astype(np.float32)
            mi += 1
    assert mi == NMASK
    c[:, C_NEG:C_NEG + 128] = np.where(q <= s, 0.0, -1e30).astype(np.float32)
    for k in range(NIT):
        c[:, C_POW + k] = 2.0 ** (-(k + 1))
    inv = 500000.0 ** (-np.arange(0, 16, 2, dtype=np.float32) / 16.0)
    c[:, C_INV:C_INV + 8] = inv[None, :].astype(np.float32)
    return c


def mask_index(g, dl):
    return sum(A_WB[i] + 1 for i in range(g)) + dl


class Buf:
    __slots__ = ("writers", "readers")

    def __init__(self):
        self.writers = []
        self.readers = []


def _compact(toks):
    best = {}
    for s, v in toks:
        if s.num not in best or best[s.num][1] < v:
            best[s.num] = (s, v)
    return list(best.values())


class Eng:
    def __init__(self, ctx, name, eng):
        self.ctx = ctx
        self.name = name
        self.eng = eng
        self.sem = None
        self.count = 0
        self.seen = {}
        self.own = set()

    def wait(self, tok):
        sem, val = tok
        if self.name == "pe" and sem.num in self.own:
            return
        if self.seen.get(sem.num, 0) >= val:
            return
        self.eng.wait_ge(sem, val)
        self.seen[sem.num] = val

    def last(self):
        return (self.sem, self.count) if self.sem is not None and self.count > 0 else None


class Ctx:
    SEM_ROLL = 32000

    def __init__(self, nc, semstack):
        self.nc = nc
        self.semstack = semstack
        self.stack = None
        self.nsem = 0
        self.pe = Eng(self, "pe", nc.tensor)
        self.act = Eng(self, "act", nc.scalar)
        self.dve = Eng(self, "dve", nc.vector)
        self.pool = Eng(self, "pool", nc.gpsimd)
        self.sp = Eng(self, "sp", nc.sync)
        self.engs = [self.pe, self.act, self.dve, self.pool, self.sp]
        self.dma_pool = {}
        self.old_sems = []
        self.nuniq = 0

    def alloc_sem(self, name):
        self.nsem += 1
        return self.semstack.enter_context(self.nc.semaphore(f"{name}_{self.nsem}"))

    def sb(self, name, shape, dtype):
        self.nuniq += 1
        return self.stack.enter_context(self.nc.sbuf_tensor(f"{name}_{self.nuniq}", list(shape), dtype))

    def ps(self, name, shape, dtype):
        self.nuniq += 1
        return self.stack.enter_context(self.nc.psum_tensor(f"{name}_{self.nuniq}", list(shape), dtype))

    def _pre(self, E, reads, writes, adds, upd):
        for b in reads:
            for t in b.writers:
                E.wait(t)
        for b in writes:
            for t in b.writers:
                E.wait(t)
            for t in b.readers:
                E.wait(t)
        for b in upd:
            for t in b.writers:
                E.wait(t)
            for t in b.readers:
                E.wait(t)
        for b in adds:
            for t in b.readers:
                E.wait(t)

    def _post(self, tok, reads, writes, adds, upd):
        for b in reads:
            b.readers.append(tok)
            if len(b.readers) > 16:
                b.readers = _compact(b.readers)
        for b in writes:
            b.writers = [tok]
            b.readers = []
        for b in upd:
            b.writers.append(tok)
            b.readers = []
            if len(b.writers) > 16:
                b.writers = _compact(b.writers)
        for b in adds:
            b.writers.append(tok)
            b.readers = []
            if len(b.writers) > 16:
                b.writers = _compact(b.writers)

    def op(self, E, fn, reads=(), writes=(), adds=(), upd=()):
        self._pre(E, reads, writes, adds, upd)
        if E.sem is None or E.count >= self.SEM_ROLL:
            if E.sem is not None:
                self.old_sems.append((E.sem, E.count))
            E.sem = self.alloc_sem(E.name)
            E.own.add(E.sem.num)
            E.count = 0
        ins = fn(E.eng)
        E.count += 1
        ins.then_inc(E.sem, 1)
        tok = (E.sem, E.count)
        self._post(tok, reads, writes, adds, upd)
        return tok

    def dma(self, E, out, in_, reads=(), writes=(), adds=(), upd=(), nsem=16, **kw):
        key = E.name
        if key not in self.dma_pool:
            self.dma_pool[key] = {"sems": [], "vals": [], "next": 0, "toks": []}
        P = self.dma_pool[key]
        self._pre(E, reads, writes, adds, upd)
        i = P["next"]
        if i >= len(P["sems"]):
            P["sems"].append(self.alloc_sem("dma" + key))
            P["vals"].append(0)
            P["toks"].append(None)
        else:
            E.wait(P["toks"][i])
        P["next"] = (i + 1) % nsem
        sem = P["sems"][i]
        P["vals"][i] += 16
        ins = E.eng.dma_start(out=out, in_=in_, **kw)
        ins.then_inc(sem, 16)
        tok = (sem, P["vals"][i])
        P["toks"][i] = tok
        self._post(tok, reads, writes, adds, upd)
        return tok

    def barrier(self):
        toks = []
        for E in self.engs:
            t = E.last()
            if t is not None:
                toks.append(t)
        for P in self.dma_pool.values():
            for t in P["toks"]:
                if t is not None:
                    toks.append(t)
        for E in self.engs:
            for t in toks:
                E.wait(t)


class Ring:
    def __init__(self, c, name, n, shape, dtype, psum=False):
        self.t = [(c.ps if psum else c.sb)(f"{name}{i}", shape, dtype) for i in range(n)]
        self.b = [Buf() for _ in range(n)]
        self.i = 0
        self.n = n

    def next(self):
        t, b = self.t[self.i], self.b[self.i]
        self.i = (self.i + 1) % self.n
        return t, b


def build(S, depth=DEPTH, dbg=False, phases="1AbBCMF"):
    NT = S // 128
    nc = bass.Bass("TRN2", target_bir_lowering=False)

    def din(name, shape, dt=F32):
        return nc.dram_tensor(name, list(shape), dt, kind="ExternalInput").ap()

    x_in = din("x", [S, D])
    pos_in = din("pos", [NT, 128], I32)
    w_in = din("w_in", [DEPTH, D, IN_W])
    w_br_a = din("w_br_a", [DEPTH, 256, D])
    w_br_b = din("w_br_b", [DEPTH, 256, D])
    w_br_c = din("w_br_c", [DEPTH, 512, D])
    w_out = din("w_out", [DEPTH, D, D])
    lam_in = din("lam", [DEPTH, 4, 64])
    subln = din("subln_g", [DEPTH, 128])
    norms = din("norms", [DEPTH, 32, 128])
    w_up = din("w_up", [DEPTH, D, 2 * DFF])
    conv_w = din("conv_w", [DEPTH, 132, 128])
    conv_b = din("conv_b", [DEPTH, 44, 128])
    w_down = din("w_down", [DEPTH, DFF, D])
    cst_in = din("cst", [128, NCST])
    y_out = nc.dram_tensor("y", [S, D], F32, kind="ExternalOutput").ap()

    def scr(name, shape, dt):
        return nc.dram_tensor(name, list(shape), dt).ap()

    QT = scr("QT", [58 * 64, S], BF16)
    VA_A = scr("VA_A", [S, 12 * 65], BF16)
    VA_B = scr("VA_B", [S, 4 * 65], BF16)
    VA_C = scr("VA_C", [S, 4 * 129], BF16)
    W8 = scr("W8", [S, 8], F32)
    GT = scr("GT", [3072, S], BF16)
    OT = scr("OT", [1024, S], BF16)
    MT = scr("MT", [NT, 128, NT, 128], BF16)
    X1 = scr("X1", [S, D], F32)
    X2 = scr("X2", [S, D], F32)
    WU = scr("WU", [22, 128, 8, 256], BF16)
    QT3 = QT.rearrange("(n d) t -> d n t", d=64)

    with ExitStack() as semstack, ExitStack() as gstack:
        c = Ctx(nc, semstack)
        c.stack = gstack
        pe, act, dve, pool, sp = c.pe, c.act, c.dve, c.pool, c.sp
        pl = pool if _os.environ.get("K_POOL", "0") == "1" else dve

        cstf = c.sb("cstf", [128, NCST], F32)
        Bc = Buf()
        ident_b = c.sb("ident_b", [128, 128], BF16)
        maskA = c.sb("maskA", [128, NMASK, 128], BF16)
        cosT = c.sb("cosT", [128, NT, 8], F32)
        sinT = c.sb("sinT", [128, NT, 8], F32)
        pib = c.sb("pib", [128, 1], F32)
        Bg = Buf()
        ident_f = cstf[:, C_ID:C_ID + 128]
        negtri = cstf[:, C_NEG:C_NEG + 128]

        c.dma(sp, cstf[:], cst_in, writes=[Bc])
        c.op(dve, lambda e: e.tensor_copy(ident_b[:], cstf[:, C_ID:C_ID + 128]), reads=[Bc], adds=[Bg])
        c.op(dve, lambda e: e.tensor_copy(maskA[:].rearrange("p m q -> p (m q)"), cstf[:, C_MASK:C_MASK + NMASK * 128]),
             reads=[Bc], adds=[Bg])
        c.op(dve, lambda e: e.memset(pib[:], math.pi), adds=[Bg])

        def load_cols(dst, src_rows_ap, n, stack_tag):
            with ExitStack() as ls:
                old = c.stack
                c.stack = ls
                tmp = c.sb("lc_tmp", [128, 128], F32)
                pt = c.ps("lc_ps", [128, 128], F32)
                bt, bp = Buf(), Buf()
                c.dma(sp, tmp[0:n, :], src_rows_ap, writes=[bt])
                c.op(pe, lambda e: e.transpose(pt[:, 0:n], tmp[0:n, :], cstf[0:n, C_ID:C_ID + n]),
                     reads=[bt, Bc], writes=[bp])
                c.op(dve, lambda e: e.tensor_copy(dst, pt[:, 0:n]), reads=[bp], adds=[Bg])
                c.barrier()
                c.stack = old

        with ExitStack() as ls:
            c.stack = ls
            posi = c.sb("posi", [128, 128], I32)
            posf = c.sb("posf", [128, 128], F32)
            pt = c.ps("pos_ps", [128, 128], F32)
            posT = c.sb("posT", [128, NT], F32)
            ang = c.sb("ang", [128, NT, 8], F32)
            ang2 = c.sb("ang2", [128, NT, 8], F32)
            b1, b2, b3, b4, b5, b6 = (Buf() for _ in range(6))
            c.dma(sp, posi[0:NT, :], pos_in, writes=[b1])
            c.op(dve, lambda e: e.tensor_copy(posf[0:NT, :], posi[0:NT, :]), reads=[b1], writes=[b2])
            c.op(pe, lambda e: e.transpose(pt[:, 0:NT], posf[0:NT, :], cstf[0:NT, C_ID:C_ID + NT]),
                 reads=[b2, Bc], writes=[b3])
            c.op(dve, lambda e: e.tensor_copy(posT[:], pt[:, 0:NT]), reads=[b3], writes=[b4])
            for i in range(8):
                c.op(dve, lambda e: e.tensor_scalar(ang[:, :, i], posT[:], cstf[:, C_INV + i:C_INV + i + 1], None,
                                                    ALU.mult), reads=[b4, Bc], adds=[b5])
            TWO_PI = 2.0 * math.pi
            ki = c.sb("ki", [128, NT, 8], I32)
            kf = c.sb("kf", [128, NT, 8], F32)
            rr_ = c.sb("rr_", [128, NT, 8], F32)
            tt_ = c.sb("tt_", [128, NT, 8], F32)
            b7, b8, b9, b10 = (Buf() for _ in range(4))
            for (dst, shift) in ((sinT, 0.0), (cosT, math.pi / 2)):
                c.op(dve, lambda e: e.tensor_scalar(ang2[:], ang[:], shift, None, ALU.add), reads=[b5], writes=[b6])
                c.op(dve, lambda e: e.tensor_scalar(kf[:], ang2[:], 1.0 / TWO_PI, None, ALU.mult), reads=[b6], writes=[b8])
                c.op(dve, lambda e: e.tensor_copy(ki[:], kf[:]), reads=[b8], writes=[b7])
                c.op(dve, lambda e: e.tensor_copy(kf[:], ki[:]), reads=[b7], writes=[b8])
                c.op(dve, lambda e: e.scalar_tensor_tensor(rr_[:], kf[:], -TWO_PI, ang2[:], ALU.mult, ALU.add),
                     reads=[b8, b6], writes=[b9])
                c.op(dve, lambda e: e.tensor_scalar(tt_[:], rr_[:], math.pi, TWO_PI, ALU.is_gt, ALU.mult),
                     reads=[b9], writes=[b10])
                c.op(dve, lambda e: e.tensor_tensor(rr_[:], rr_[:], tt_[:], ALU.subtract), reads=[b10], upd=[b9])
                c.op(dve, lambda e: e.tensor_scalar(tt_[:], rr_[:], -math.pi, TWO_PI, ALU.is_lt, ALU.mult),
                     reads=[b9], upd=[b10])
                c.op(dve, lambda e: e.tensor_tensor(rr_[:], rr_[:], tt_[:], ALU.add), reads=[b10], upd=[b9])
                c.op(act, lambda e: e.activation(dst[:], rr_[:], AF.Sin), reads=[b9], adds=[Bg])
            c.barrier()
        c.stack = gstack

        gcols = c.sb("gcols", [128, DEPTH, 32], F32)
        cw = c.sb("cw", [128, DEPTH, 132], F32)
        cb = c.sb("cb", [128, DEPTH, 44], F32)
        lamt = c.sb("lamt", [128, DEPTH, 4], F32)
        for l in range(depth):
            load_cols(gcols[:, l, :], norms[l], 32, "g")
            load_cols(cw[:, l, 0:128], conv_w[l, 0:128, :], 128, "cw")
            load_cols(cw[:, l, 128:132], conv_w[l, 128:132, :], 4, "cw2")
            load_cols(cb[:, l, :], conv_b[l], 44, "cb")
        with ExitStack() as ls:
            c.stack = ls
            lt = c.sb("lt", [128, DEPTH, 4, 64], F32)
            pr = c.sb("pr", [128, DEPTH, 2, 64], F32)
            sm = c.sb("sm", [128, DEPTH, 2], F32)
            ex = c.sb("ex", [128, DEPTH, 2], F32)
            b1, b2, b3, b4 = (Buf() for _ in range(4))
            for l in range(DEPTH):
                c.dma(sp, lt[:, l, :, :].rearrange("p a b -> p (a b)"),
                      lam_in[l].rearrange("a b -> (a b)").partition_broadcast(128), adds=[b1])
            for l in range(DEPTH):
                for j in range(2):
                    c.op(dve, lambda e: e.tensor_tensor(pr[:, l, j, :], lt[:, l, 2 * j, :], lt[:, l, 2 * j + 1, :],
                                                        ALU.mult), reads=[b1], adds=[b2])
            c.op(dve, lambda e: e.tensor_reduce(sm[:].rearrange("p l j -> p (l j)"),
                                                pr[:].rearrange("p l j d -> p (l j) d"), AX.X, ALU.add),
                 reads=[b2], writes=[b3])
            c.op(act, lambda e: e.activation(ex[:], sm[:], AF.Exp), reads=[b3], writes=[b4])
            for l in range(DEPTH):
                lam_init = 0.8 - 0.6 * math.exp(-0.3 * l)
                c.op(dve, lambda e: e.tensor_tensor(lamt[:, l, 0:1], ex[:, l, 0:1], ex[:, l, 1:2], ALU.subtract),
                     reads=[b4], adds=[Bg])
                c.op(dve, lambda e: e.tensor_scalar(lamt[:, l, 0:1], lamt[:, l, 0:1], lam_init, None, ALU.add),
                     upd=[Bg])
                c.op(dve, lambda e: e.tensor_scalar(lamt[:, l, 1:2], lamt[:, l, 0:1], -1.0, None, ALU.mult),
                     upd=[Bg])
            c.barrier()
        c.stack = gstack
        c.barrier()

        def norm_transpose(st, xsrc_rows, xnT_dst, bdst, first_write):
            xt, bx = st["xr"].next()
            c.dma(sp, xt[:], xsrc_rows, writes=[bx])
            ss, bs = st["ss"].next()
            c.op(dve, lambda e: e.memset(ss[:], 0.0), writes=[bs])
            jk, bj = st["junk"].next()
            c.op(act, lambda e: e.activation(jk[:], xt[:], AF.Square, accum_out=ss[:, 0:1]),
                 reads=[bx], writes=[bj], upd=[bs])
            c.op(dve, lambda e: e.tensor_scalar(ss[:, 1:2], ss[:, 0:1], 1.0 / D, EPS, ALU.mult, ALU.add), upd=[bs])
            c.op(act, lambda e: e.activation(ss[:, 2:3], ss[:, 1:2], AF.Sqrt), upd=[bs])
            c.op(dve, lambda e: e.reciprocal(ss[:, 2:3], ss[:, 2:3]), upd=[bs])
            xb, bxb = st["xb"].next()
            c.op(dve, lambda e: e.tensor_scalar(xb[:], xt[:], ss[:, 2:3], None, ALU.mult),
                 reads=[bx, bs], writes=[bxb])
            pt, bpt = st["pT"].next()
            for kc in range(8):
                c.op(pe, lambda e: e.transpose(pt[:, kc, :], xb[:, kc * 128:(kc + 1) * 128], ident_b[:]),
                     reads=[bxb, Bg], **({"writes": [bpt]} if kc == 0 else {"adds": [bpt]}))
            c.op(act, lambda e: e.activation(xnT_dst, pt[:], AF.Copy), reads=[bpt], writes=[bdst])
            return xt, bx

        def load_w(st, dst_bf, bdst, src_ap, nk, ncols, gain_ap=None):
            cap = st["wst"].t[0].shape[1]
            for k0 in range(0, nk, cap):
                kn = min(cap, nk - k0)
                ws, bws = st["wst"].next()
                c.dma(sp, ws[:, 0:kn, 0:ncols], src_ap[:, k0:k0 + kn, :], writes=[bws])
                for kk in range(kn):
                    kc = k0 + kk
                    if gain_ap is not None:
                        c.op(act, lambda e: e.activation(dst_bf[:, kc, 0:ncols], ws[:, kk, 0:ncols], AF.Identity,
                                                         scale=gain_ap[:, kc:kc + 1]),
                             reads=[bws, Bg], **({"writes": [bdst]} if kc == 0 else {"adds": [bdst]}))
                    else:
                        c.op(act, lambda e: e.activation(dst_bf[:, kc, 0:ncols], ws[:, kk, 0:ncols], AF.Copy),
                             reads=[bws], **({"writes": [bdst]} if kc == 0 else {"adds": [bdst]}))

        def phase1(l, xin):
            TCH = min(S, 2048)
            NCH = S // TCH
            NTT = TCH // 128
            with ExitStack() as ps_:
                c.stack = ps_
                st = {
                    "xr": Ring(c, "xr", 2, [128, D], F32),
                    "ss": Ring(c, "ss", 2, [128, 4], F32),
                    "junk": Ring(c, "junk", 1, [128, D], BF16),
                    "xb": Ring(c, "xb", 2, [128, D], BF16),
                    "pT": Ring(c, "pT", 2, [128, 8, 128], BF16, psum=True),
                    "wst": Ring(c, "wst", 2, [128, 8, 512], F32),
                }
                wbr = Ring(c, "wb", 2, [128, 8, 512], BF16)
                pM = Ring(c, "pM", 2, [128, 512], F32, psum=True)
                pQ = Ring(c, "pQ", 2, [128, 4, 128], BF16, psum=True)
                xnT = c.sb("xnT", [128, 8, TCH], BF16)
                bxn = [Buf() for _ in range(NTT)]
                qbr = Ring(c, "qb", 2, [128, 512], BF16)
                tmps = [Ring(c, f"rt{i}", 2, [128, 8, 8], F32) for i in range(4)]
                rpr = Ring(c, "rp", 2, [128, 8, 16], F32)
                cos8 = c.sb("cos8", [128, NT, 8, 8], F32)
                sin8 = c.sb("sin8", [128, NT, 8, 8], F32)
                b88 = Buf()
                for (d8, src8) in ((cos8, cosT), (sin8, sinT)):
                    for hh_ in range(8):
                        c.op(dve, lambda e: e.tensor_copy(d8[:, :, hh_, :], src8[:]), reads=[Bg], adds=[b88])
                qsr = Ring(c, "qs", 3, [128, 4, 128], BF16)
                vsa = Ring(c, "vsa", 3, [128, 4, 65], BF16)
                vsc = Ring(c, "vsc", 3, [128, 4, 129], BF16)
                gsr = Ring(c, "gs", 2, [128, 4, 512], BF16)
                w8r = Ring(c, "w8", 3, [128, 8], F32)
                for r in (vsa, vsc):
                    for t, b in zip(r.t, r.b):
                        c.op(dve, lambda e: e.memset(t[:], 1.0), writes=[b])
                w_l = w_in[l].rearrange("(kc p) n -> p kc n", p=128)
                gain = gcols[:, l, 0:8]

                tiles = []
                for g in range(3):
                    tiles.append(("qk", g * 768, 8, g * 8))
                    tiles.append(("v", g * 768 + 512, 256, VA_A, g * 4 * 65, 64))
                tiles.append(("qk", 2304, 8, 24))
                tiles.append(("v", 2816, 256, VA_B, 0, 64))
                tiles.append(("qk", 3072, 8, 32))
                tiles.append(("kw", 3584, 72))
                tiles.append(("qk", 3656, 8, 41))
                tiles.append(("qk", 4168, 8, 49))
                tiles.append(("v", 4680, 512, VA_C, 0, 128))
                for i in range(6):
                    tiles.append(("gate", 5192 + i * 512, 512, i * 512))

                def rope_tile(pm, bpm, qbt, bqb, nh, tglob):
                    pmv = pm[:, 0:nh * 64].rearrange("p (h d) -> p h d", d=64)
                    qbv = qbt[:, 0:nh * 64].rearrange("p (h d) -> p h d", d=64)
                    c.op(act, lambda e: e.activation(qbv[:, :, 16:64], pmv[:, :, 16:64], AF.Copy),
                         reads=[bpm], writes=[bqb])
                    if _os.environ.get("K_NOROPE"):
                        c.op(act, lambda e: e.activation(qbv[:, :, 0:16], pmv[:, :, 0:16], AF.Copy), reads=[bpm], adds=[bqb])
                        return
                    cosb = cos8[:, tglob, 0:nh, :]
                    sinb = sin8[:, tglob, 0:nh, :]
                    (t1, bt1), (t2, bt2), (t3, bt3), (t4, bt4) = [r.next() for r in tmps]
                    rp, brp = rpr.next()
                    c.op(act, lambda e: e.activation(rp[:, 0:nh, :], pmv[:, :, 0:16], AF.Copy), reads=[bpm], writes=[brp])
                    x1 = rp[:, 0:nh, 0:8]
                    x2 = rp[:, 0:nh, 8:16]
                    c.op(dve, lambda e: e.tensor_tensor(t1[:, 0:nh, :], x1, cosb, ALU.mult), reads=[brp, b88], writes=[bt1])
                    c.op(dve, lambda e: e.tensor_tensor(t2[:, 0:nh, :], x2, sinb, ALU.mult), reads=[brp, b88], writes=[bt2])
                    c.op(dve, lambda e: e.tensor_tensor(qbv[:, :, 0:8], t1[:, 0:nh, :], t2[:, 0:nh, :], ALU.subtract),
                         reads=[bt1, bt2], adds=[bqb])
                    c.op(dve, lambda e: e.tensor_tensor(t3[:, 0:nh, :], x2, cosb, ALU.mult), reads=[brp, b88], writes=[bt3])
                    c.op(dve, lambda e: e.tensor_tensor(t4[:, 0:nh, :], x1, sinb, ALU.mult), reads=[brp, b88], writes=[bt4])
                    c.op(dve, lambda e: e.tensor_tensor(qbv[:, :, 8:16], t3[:, 0:nh, :], t4[:, 0:nh, :], ALU.add),
                         reads=[bt3, bt4], adds=[bqb])

                for ch in range(NCH):
                    for tt in range(NTT):
                        t0 = ch * TCH + tt * 128
                        norm_transpose(st, xin[t0:t0 + 128, :], xnT[:, :, tt * 128:(tt + 1) * 128], bxn[tt], True)
                    import os as _os
                    _kinds = _os.environ.get("K_KINDS", "qk,v,kw,gate").split(",")
                    for tl in tiles:
                        kind, col0 = tl[0], tl[1]
                        if kind not in _kinds:
                            continue
                        ncols = 512 if kind in ("qk", "gate") else tl[2]
                        wb, bwb = wbr.next()
                        load_w(st, wb, bwb, w_l[:, :, col0:col0 + ncols], 8, ncols, gain)
                        if kind == "gate":
                            row0 = tl[3]
                            for tg in range(TCH // 512):
                                gs, bgs = gsr.next()
                                for cbk in range(4):
                                    pm, bpm = pM.next()
                                    for kc in range(8):
                                        c.op(pe, lambda e: e.matmul(pm[:], wb[:, kc, cbk * 128:(cbk + 1) * 128],
                                                                    xnT[:, kc, tg * 512:(tg + 1) * 512],
                                                                    start=(kc == 0), stop=(kc == 7)),
                                             reads=[bwb] + bxn[tg * 4:(tg + 1) * 4],
                                             **({"writes": [bpm]} if kc == 0 else {"adds": [bpm]}))
                                    c.op(act, lambda e: e.activation(gs[:, cbk, :], pm[:], AF.Sigmoid), reads=[bpm],
                                         **({"writes": [bgs]} if cbk == 0 else {"adds": [bgs]}))
                                tg0 = ch * TCH + tg * 512
                                c.dma(pool, GT[row0:row0 + 512, tg0:tg0 + 512].rearrange("(b p) t -> p b t", p=128),
                                      gs[:], reads=[bgs])
                            continue
                        for tt in range(NTT):
                            t0 = ch * TCH + tt * 128
                            tglob = t0 // 128
                            pm, bpm = pM.next()
                            for kc in range(8):
                                c.op(pe, lambda e: e.matmul(pm[:, 0:ncols], xnT[:, kc, tt * 128:(tt + 1) * 128],
                                                            wb[:, kc, 0:ncols], start=(kc == 0), stop=(kc == 7)),
                                     reads=[bwb, bxn[tt]], **({"writes": [bpm]} if kc == 0 else {"adds": [bpm]}))
                            if kind == "v":
                                dst, coff, hd = tl[3], tl[4], tl[5]
                                vs, bvs = (vsa if hd == 64 else vsc).next()
                                c.op(act, lambda e: e.activation(vs[:, :, 0:hd],
                                                                 pm[:, 0:ncols].rearrange("p (h d) -> p h d", d=hd),
                                                                 AF.Copy), reads=[bpm], writes=[bvs])
                                c.dma(pool, dst[t0:t0 + 128, coff:coff + 4 * (hd + 1)],
                                      vs[:].rearrange("p h d -> p (h d)"), reads=[bvs])
                                continue
                            nh = 8 if kind == "qk" else 1
                            qbt, bqb = qbr.next()
                            rope_tile(pm, bpm, qbt, bqb, nh, tglob)
                            pq, bpq = pQ.next()
                            qs, bqs = qsr.next()
                            if kind == "qk":
                                head0 = tl[3]
                                for blk in range(4):
                                    c.op(pe, lambda e: e.transpose(pq[:, blk, :], qbt[:, blk * 128:(blk + 1) * 128],
                                                                   ident_b[:]),
                                         reads=[bqb, Bg], **({"writes": [bpq]} if blk == 0 else {"adds": [bpq]}))
                                c.op(dve, lambda e: e.tensor_copy(qs[:], pq[:]), reads=[bpq], writes=[bqs])
                                c.dma(pool, QT[head0 * 64:head0 * 64 + 512, t0:t0 + 128].rearrange("(b p) t -> p b t", p=128),
                                      qs[:], reads=[bqs])
                            else:
                                c.op(pe, lambda e: e.transpose(pq[0:64, 0, :], qbt[:, 0:64], ident_b[:]),
                                     reads=[bqb, Bg], writes=[bpq])
                                c.op(dve, lambda e: e.tensor_copy(qs[0:64, 0, :], pq[0:64, 0, :]), reads=[bpq], writes=[bqs])
                                c.dma(pool, QT[40 * 64:41 * 64, t0:t0 + 128], qs[0:64, 0, :], reads=[bqs])
                                w8, bw8 = w8r.next()
                                c.op(act, lambda e: e.mul(w8[:], pm[:, 64:72], 8.0 ** -0.5), reads=[bpm], writes=[bw8])
                                c.dma(pool, W8[t0:t0 + 128, :], w8[:], reads=[bw8])
                c.barrier()
            c.stack = gstack

        def phaseA(l):
            with ExitStack() as ps_:
                c.stack = ps_
                kt = c.sb("a_kt", [64, 12, NSLOT * 128], BF16)
                va = c.sb("a_va", [128, NSLOT, 12 * 65], BF16)
                bk = [Buf() for _ in range(NSLOT)]
                bv = [Buf() for _ in range(NSLOT)]
                qtr = Ring(c, "a_qt", 2, [64, 12, 128], BF16)
                er = Ring(c, "a_e", 5, [128, 512], BF16)
                pr_ = Ring(c, "a_p", 5, [128, 512], BF16)
                psr = Ring(c, "a_ps", 4, [128, 512], F32, psum=True)
                accr = Ring(c, "a_acc", 2, [128, 4, 65], F32, psum=True)
                por = Ring(c, "a_po", 2, [128, 2, 128], BF16, psum=True)
                rcr = Ring(c, "a_rc", 2, [128, 4], F32)
                obr = Ring(c, "a_ob", 2, [128, 256], BF16)
                osr = Ring(c, "a_os", 2, [128, 2, 128], BF16)
                for qb in range(NT):
                    slot = qb % NSLOT
                    tok0 = qb * 128
                    qt, bq = qtr.next()
                    for g in range(3):
                        c.dma(sp, kt[:, g * 4:(g + 1) * 4, slot * 128:(slot + 1) * 128],
                              QT3[:, g * 8 + 4:g * 8 + 8, tok0:tok0 + 128],
                              **({"writes": [bk[slot]]} if g == 0 else {"adds": [bk[slot]]}))
                        c.dma(sp, qt[:, g * 4:(g + 1) * 4, :], QT3[:, g * 8:g * 8 + 4, tok0:tok0 + 128],
                              **({"writes": [bq]} if g == 0 else {"adds": [bq]}))
                    c.dma(sp, va[:, slot, :], VA_A[tok0:tok0 + 128, :], writes=[bv[slot]])
                    acc, bacc = accr.next()
                    c.op(dve, lambda e: e.memset(acc[:], 0.0), writes=[bacc])
                    pairs = [(g, kb) for g in range(3) for kb in range(max(0, qb - A_WB[g]), qb + 1)]

                    def a_qk(i):
                        g, kb = pairs[i]
                        ks = kb % NSLOT
                        ps, bps = psr.next()
                        for h in range(4):
                            c.op(pe, lambda e: e.matmul(ps[:, h * 128:(h + 1) * 128],
                                                        kt[:, g * 4 + h, ks * 128:(ks + 1) * 128], qt[:, g * 4 + h, :],
                                                        start=True, stop=True),
                                 reads=[bk[ks], bq], **({"writes": [bps]} if h == 0 else {"adds": [bps]}))
                        return ps, bps

                    def a_mid(i, ps, bps):
                        g, kb = pairs[i]
                        ee, be = er.next()
                        c.op(act, lambda e: e.activation(ee[:], ps[:], AF.Exp, scale=0.125), reads=[bps], writes=[be])
                        pp, bp = pr_.next()
                        mk = maskA[:, mask_index(g, qb - kb), :]
                        for h in range(4):
                            c.op(dve, lambda e: e.tensor_tensor(pp[:, h * 128:(h + 1) * 128], ee[:, h * 128:(h + 1) * 128],
                                                                mk, ALU.mult),
                                 reads=[be, Bg], **({"writes": [bp]} if h == 0 else {"adds": [bp]}))
                        return pp, bp

                    def a_pv(i, pp, bp):
                        g, kb = pairs[i]
                        ks = kb % NSLOT
                        for h in range(4):
                            c.op(pe, lambda e: e.matmul(acc[:, h, :], pp[:, h * 128:(h + 1) * 128],
                                                        va[:, ks, (g * 4 + h) * 65:(g * 4 + h + 1) * 65],
                                                        start=False, stop=False, skip_group_check=True),
                                 reads=[bp, bv[ks]], upd=[bacc])

                    LA = 3
                    pend = []
                    for i in range(len(pairs)):
                        ps, bps = a_qk(i)
                        pend.append((i,) + a_mid(i, ps, bps))
                        if len(pend) > LA:
                            a_pv(*pend.pop(0))
                    for it in pend:
                        a_pv(*it)
                    rc, brc = rcr.next()
                    c.op(dve, lambda e: e.reciprocal(rc[:], acc[:, :, 64]), reads=[bacc], writes=[brc])
                    ob, bob = obr.next()
                    for h in range(4):
                        c.op(dve, lambda e: e.tensor_scalar(ob[:, h * 64:(h + 1) * 64], acc[:, h, 0:64], rc[:, h:h + 1],
                                                            None, ALU.mult),
                             reads=[bacc, brc], **({"writes": [bob]} if h == 0 else {"adds": [bob]}))
                    po, bpo = por.next()
                    for j in range(2):
                        c.op(pe, lambda e: e.transpose(po[:, j, :], ob[:, j * 128:(j + 1) * 128], ident_b[:]),
                             reads=[bob, Bg], **({"writes": [bpo]} if j == 0 else {"adds": [bpo]}))
                    os_, bos = osr.next()
                    c.op(act, lambda e: e.activation(os_[:], po[:], AF.Copy), reads=[bpo], writes=[bos])
                    c.dma(pool, OT[0:256, tok0:tok0 + 128].rearrange("(b p) t -> p b t", p=128), os_[:], reads=[bos])
                c.barrier()
            c.stack = gstack

        def phaseB1(l):
            with ExitStack() as ps_:
                c.stack = ps_
                kx = c.sb("b_kx", [64, S], BF16)
                bkx = Buf()
                c.dma(sp, kx[:], QT3[:, 40, :], writes=[bkx])
                qxr = Ring(c, "b_qx", 2, [64, 8, 128], BF16)
                wr = Ring(c, "b_w", 2, [128, 8], F32)
                dgr = Ring(c, "b_dg", 2, [128, 8, 128], BF16)
                Ir = Ring(c, "b_I", 2, [128, S], F32)
                rr = Ring(c, "b_r", 4, [128, 512], BF16)
                psr = Ring(c, "b_ps", 4, [128, 512], F32, psum=True)
                pacc = Ring(c, "b_pa", 2, [128, 512], F32, psum=True)
                Mr = Ring(c, "b_M", 2, [128, S], BF16)
                scr_ = Ring(c, "b_sc", 2, [128, 8], F32)
                hsr = Ring(c, "b_hs", 2, [128, NIT], F32)
                ptr = Ring(c, "b_pt", 2, [128, 4, 128], BF16, psum=True)
                mtr = Ring(c, "b_mt", 1, [128, NT, 128], BF16)
                def b1_finish(qb, M, bM):
                        mt, bmt = mtr.next()
                        nkb = qb + 1
                        for k0 in range(0, nkb, 4):
                            kn = min(4, nkb - k0)
                            pt, bpt = ptr.next()
                            for j in range(kn):
                                c.op(pe, lambda e: e.transpose(pt[:, j, :], M[:, (k0 + j) * 128:(k0 + j + 1) * 128], ident_b[:]),
                                     reads=[bM, Bg], **({"writes": [bpt]} if j == 0 else {"adds": [bpt]}))
                            c.op(act, lambda e: e.activation(mt[:, k0:k0 + kn, :], pt[:, 0:kn, :], AF.Copy), reads=[bpt],
                                 **({"writes": [bmt]} if k0 == 0 else {"adds": [bmt]}))
                        c.dma(pool, MT[qb, :, 0:nkb, :], mt[:, 0:nkb, :], reads=[bmt])

                prev_fin = None
                for qb in range(NT):
                    tok0 = qb * 128
                    nv = tok0 + 128
                    qx, bqx = qxr.next()
                    c.dma(sp, qx[:], QT3[:, 32:40, tok0:tok0 + 128], writes=[bqx])
                    w, bw = wr.next()
                    c.dma(sp, w[:], W8[tok0:tok0 + 128, :], writes=[bw])
                    dg, bdg = dgr.next()
                    for h in range(8):
                        c.op(act, lambda e: e.activation(dg[:, h, :], cstf[:, C_ID:C_ID + 128], AF.Identity, scale=w[:, h:h + 1]),
                             reads=[bw, Bc], **({"writes": [bdg]} if h == 0 else {"adds": [bdg]}))
                    I_, bI = Ir.next()
                    nst = (nv + 511) // 512
                    firstI = True
                    for s_t in range(nst):
                        s0 = s_t * 512
                        wd = min(512, nv - s0)
                        pa, bpa = pacc.next()

                        def lg(h):
                            ps, bps = psr.next()
                            c.op(pe, lambda e: e.matmul(ps[:, 0:wd], qx[:, h, :], kx[:, s0:s0 + wd], start=True, stop=True),
                                 reads=[bqx, bkx], writes=[bps])
                            r, br = rr.next()
                            c.op(act, lambda e: e.activation(r[:, 0:wd], ps[:, 0:wd], AF.Relu, scale=0.125),
                                 reads=[bps], writes=[br])
                            return r, br

                        def dgm(h, r, br):
                            c.op(pe, lambda e: e.matmul(pa[:, 0:wd], dg[:, h, :], r[:, 0:wd], start=(h == 0), stop=(h == 7)),
                                 reads=[bdg, br], **({"writes": [bpa]} if h == 0 else {"adds": [bpa]}))

                        pend = []
                        for h in range(8):
                            pend.append((h,) + lg(h))
                            if len(pend) > 2:
                                dgm(*pend.pop(0))
                        for it in pend:
                            dgm(*it)
                        c.op(act, lambda e: e.activation(I_[:, s0:s0 + wd], pa[:, 0:wd], AF.Copy), reads=[bpa],
                             **({"writes": [bI]} if firstI else {"adds": [bI]}))
                        firstI = False
                    M, bM = Mr.next()
                    sc, bsc = scr_.next()
                    hs, bhs = hsr.next()
                    if qb >= 2:
                        c.op(dve, lambda e: e.tensor_reduce(sc[:, 0:1], I_[:, 0:nv], AX.X, ALU.max,
                                                            apply_absolute_value=True), reads=[bI], writes=[bsc])
                    c.op(dve, lambda e: e.tensor_tensor(I_[:, tok0:nv], I_[:, tok0:nv], negtri, ALU.add),
                         reads=[Bc], upd=[bI])
                    if qb < 2:
                        c.op(dve, lambda e: e.tensor_scalar(M[:, 0:nv], I_[:, 0:nv], -1e29, None, ALU.is_ge),
                             reads=[bI], writes=[bM])
                    else:
                        c.op(dve, lambda e: e.tensor_scalar(sc[:, 0:1], sc[:, 0:1], 1.0, 1e-3, ALU.mult, ALU.add), upd=[bsc])
                        c.op(dve, lambda e: e.tensor_scalar(sc[:, 1:2], sc[:, 0:1], -1.0, None, ALU.mult), upd=[bsc])
                        c.op(dve, lambda e: e.tensor_scalar(sc[:, 4:5], sc[:, 0:1], 2.0, None, ALU.mult), upd=[bsc])
                        c.op(dve, lambda e: e.tensor_scalar(hs[:], cstf[:, C_POW:C_POW + NIT], sc[:, 4:5], None, ALU.mult),
                             reads=[bsc, Bc], writes=[bhs])
                        for k in range(NIT):
                            c.op(dve, lambda e: e.tensor_tensor(sc[:, 2:3], sc[:, 1:2], hs[:, k:k + 1], ALU.add),
                                 reads=[bhs], upd=[bsc])
                            c.op(dve, lambda e: e.tensor_scalar(M[:, 0:nv], I_[:, 0:nv], sc[:, 2:3], None, ALU.is_ge,
                                                                ALU.add, accum_out=sc[:, 3:4]),
                                 reads=[bI], writes=[bM], upd=[bsc])
                            c.op(dve, lambda e: e.scalar_tensor_tensor(sc[:, 4:5], sc[:, 3:4], TOPK - 0.5, hs[:, k:k + 1],
                                                                       ALU.is_ge, ALU.mult), reads=[bhs], upd=[bsc])
                            c.op(dve, lambda e: e.tensor_tensor(sc[:, 1:2], sc[:, 1:2], sc[:, 4:5], ALU.add), upd=[bsc])
                        c.op(dve, lambda e: e.tensor_scalar(M[:, 0:nv], I_[:, 0:nv], sc[:, 1:2], None, ALU.is_ge),
                             reads=[bI, bsc], writes=[bM])
                    if prev_fin is not None:
                        b1_finish(*prev_fin)
                    prev_fin = (qb, M, bM)
                b1_finish(*prev_fin)
                c.barrier()
            c.stack = gstack

        def phaseB2(l):
            with ExitStack() as ps_:
                c.stack = ps_
                kt = c.sb("b2_kt", [64, 4, S], BF16)
                va = c.sb("b2_va", [128, NT, 4 * 65], BF16)
                bkt, bva = Buf(), Buf()
                c.dma(sp, kt[:], QT3[:, 28:32, :], writes=[bkt])
                vbv = VA_B.rearrange("(n p) c -> p n c", p=128)
                for n0 in range(0, NT, 8):
                    c.dma(sp, va[:, n0:n0 + 8, :], vbv[:, n0:n0 + 8, :], **({"writes": [bva]} if n0 == 0 else {"adds": [bva]}))
                qtr = Ring(c, "b2_qt", 2, [64, 4, 128], BF16)
                mtr = Ring(c, "b2_mt", 2, [128, NT, 128], BF16)
                er = Ring(c, "b2_e", 5, [128, 512], BF16)
                pr_ = Ring(c, "b2_p", 5, [128, 512], BF16)
                psr = Ring(c, "b2_ps", 4, [128, 512], F32, psum=True)
                accr = Ring(c, "b2_acc", 2, [128, 4, 65], F32, psum=True)
                por = Ring(c, "b2_po", 2, [128, 2, 128], BF16, psum=True)
                rcr = Ring(c, "b2_rc", 2, [128, 4], F32)
                obr = Ring(c, "b2_ob", 2, [128, 256], BF16)
                osr = Ring(c, "b2_os", 2, [128, 2, 128], BF16)
                for qb in range(NT):
                    tok0 = qb * 128
                    nkb = qb + 1
                    qt, bq = qtr.next()
                    c.dma(sp, qt[:], QT3[:, 24:28, tok0:tok0 + 128], writes=[bq])
                    mt, bmt = mtr.next()
                    c.dma(sp, mt[:, 0:nkb, :], MT[qb, :, 0:nkb, :], writes=[bmt])
                    acc, bacc = accr.next()
                    c.op(dve, lambda e: e.memset(acc[:], 0.0), writes=[bacc])
                    def b_qk(kb):
                        ps, bps = psr.next()
                        for h in range(4):
                            c.op(pe, lambda e: e.matmul(ps[:, h * 128:(h + 1) * 128], kt[:, h, kb * 128:(kb + 1) * 128],
                                                        qt[:, h, :], start=True, stop=True),
                                 reads=[bkt, bq], **({"writes": [bps]} if h == 0 else {"adds": [bps]}))
                        return ps, bps

                    def b_mid(kb, ps, bps):
                        ee, be = er.next()
                        c.op(act, lambda e: e.activation(ee[:], ps[:], AF.Exp, scale=0.125), reads=[bps], writes=[be])
                        pp, bp = pr_.next()
                        mk = mt[:, kb, :]
                        for h in range(4):
                            c.op(dve, lambda e: e.tensor_tensor(pp[:, h * 128:(h + 1) * 128], ee[:, h * 128:(h + 1) * 128],
                                                                mk, ALU.mult),
                                 reads=[be, bmt], **({"writes": [bp]} if h == 0 else {"adds": [bp]}))
                        return pp, bp

                    def b_pv(kb, pp, bp):
                        for h in range(4):
                            c.op(pe, lambda e: e.matmul(acc[:, h, :], pp[:, h * 128:(h + 1) * 128],
                                                        va[:, kb, h * 65:(h + 1) * 65],
                                                        start=False, stop=False, skip_group_check=True),
                                 reads=[bp, bva], upd=[bacc])

                    LA = 3
                    pend = []
                    for kb in range(nkb):
                        ps, bps = b_qk(kb)
                        pend.append((kb,) + b_mid(kb, ps, bps))
                        if len(pend) > LA:
                            b_pv(*pend.pop(0))
                    for it in pend:
                        b_pv(*it)
                    rc, brc = rcr.next()
                    c.op(dve, lambda e: e.reciprocal(rc[:], acc[:, :, 64]), reads=[bacc], writes=[brc])
                    ob, bob = obr.next()
                    for h in range(4):
                        c.op(dve, lambda e: e.tensor_scalar(ob[:, h * 64:(h + 1) * 64], acc[:, h, 0:64], rc[:, h:h + 1],
                                                            None, ALU.mult),
                             reads=[bacc, brc], **({"writes": [bob]} if h == 0 else {"adds": [bob]}))
                    po, bpo = por.next()
                    for j in range(2):
                        c.op(pe, lambda e: e.transpose(po[:, j, :], ob[:, j * 128:(j + 1) * 128], ident_b[:]),
                             reads=[bob, Bg], **({"writes": [bpo]} if j == 0 else {"adds": [bpo]}))
                    os_, bos = osr.next()
                    c.op(act, lambda e: e.activation(os_[:], po[:], AF.Copy), reads=[bpo], writes=[bos])
                    c.dma(pool, OT[256:512, tok0:tok0 + 128].rearrange("(b p) t -> p b t", p=128), os_[:], reads=[bos])
                c.barrier()
            c.stack = gstack

        def phaseC(l):
            lam_init = 0.8 - 0.6 * math.exp(-0.3 * l)
            with ExitStack() as ps_:
                c.stack = ps_
                ktr = Ring(c, "c_kt", 2, [64, 2, S], BF16)
                var = Ring(c, "c_va", 2, [128, NT, 129], BF16)
                qtr = Ring(c, "c_qt", 2, [64, 2, 512], BF16)
                er = Ring(c, "c_e", 4, [128, 512], BF16)
                psr = Ring(c, "c_ps", 3, [128, 512], F32, psum=True)
                accs = [c.ps(f"c_acc{i}", [128, 3, 129], F32) for i in range(3)]
                bacc = Buf()
                por = Ring(c, "c_po", 2, [128, 128], BF16, psum=True)
                sg = c.sb("c_sg", [128, 128], F32)
                bsg = Buf()
                c.dma(sp, sg[:], subln[l].partition_broadcast(128), writes=[bsg])
                tri = maskA[:, 0, :]
                smr = Ring(c, "c_sm", 2, [128, 8], F32)
                o1r = Ring(c, "c_o1", 2, [128, 128], F32)
                o2r = Ring(c, "c_o2", 2, [128, 128], F32)
                jr = Ring(c, "c_j", 2, [128, 128], F32)
                obr = Ring(c, "c_ob", 2, [128, 128], BF16)
                osr = Ring(c, "c_os", 2, [128, 512], BF16)

                def accv(cc, j):
                    i = cc * 4 + j
                    return accs[i // 3][:, i % 3, :]

                for h in range(4):
                    kt, bkt = ktr.next()
                    va, bva = var.next()
                    c.dma(sp, kt[:], QT3[:, 49 + 2 * h:51 + 2 * h, :], writes=[bkt])
                    vcv = VA_C[:, h * 129:(h + 1) * 129].rearrange("(n p) c -> p n c", p=128)
                    for n0 in range(0, NT, 8):
                        c.dma(sp, va[:, n0:n0 + 8, :], vcv[:, n0:n0 + 8, :], **({"writes": [bva]} if n0 == 0 else {"adds": [bva]}))
                    for qt_i in range(S // 512):
                        q0 = qt_i * 512
                        qt, bq = qtr.next()
                        c.dma(sp, qt[:], QT3[:, 41 + 2 * h:43 + 2 * h, q0:q0 + 512], writes=[bq])
                        for a in accs:
                            c.op(dve, lambda e: e.memset(a[:], 0.0), **({"writes": [bacc]} if a is accs[0] else {"upd": [bacc]}))
                        nkb = (q0 + 512) // 128
                        steps = [(kb, cc) for kb in range(nkb) for cc in range(2)]

                        def c_qk(kb, cc):
                            ps, bps = psr.next()
                            c.op(pe, lambda e: e.matmul(ps[:], kt[:, cc, kb * 128:(kb + 1) * 128], qt[:, cc, :],
                                                        start=True, stop=True), reads=[bkt, bq], writes=[bps])
                            return ps, bps

                        def c_mid(kb, cc, ps, bps):
                            jk = kb - q0 // 128
                            ee, be = er.next()
                            c.op(act, lambda e: e.activation(ee[:], ps[:], AF.Exp, scale=0.125), reads=[bps], writes=[be])
                            if jk >= 0:
                                c.op(dve, lambda e: e.tensor_tensor(ee[:, jk * 128:(jk + 1) * 128],
                                                                    ee[:, jk * 128:(jk + 1) * 128], tri, ALU.mult),
                                     reads=[Bg], upd=[be])
                            return ee, be

                        def c_pv(kb, cc, ee, be):
                            jk = kb - q0 // 128
                            for j in range(max(jk, 0), 4):
                                c.op(pe, lambda e: e.matmul(accv(cc, j), ee[:, j * 128:(j + 1) * 128], va[:, kb, :],
                                                            start=False, stop=False, skip_group_check=True),
                                     reads=[be, bva], upd=[bacc])

                        LA = 2
                        pend = []
                        for (kb, cc) in steps:
                            ps, bps = c_qk(kb, cc)
                            pend.append((kb, cc) + c_mid(kb, cc, ps, bps))
                            if len(pend) > LA:
                                c_pv(*pend.pop(0))
                        for it in pend:
                            c_pv(*it)
                        os_, bos = osr.next()
                        for j in range(4):
                            sm, bsm = smr.next()
                            a1 = accv(0, j)
                            a2 = accv(1, j)
                            c.op(dve, lambda e: e.reciprocal(sm[:, 0:1], a1[:, 128:129]), reads=[bacc], writes=[bsm])
                            c.op(dve, lambda e: e.reciprocal(sm[:, 1:2], a2[:, 128:129]), reads=[bacc], upd=[bsm])
                            c.op(dve, lambda e: e.tensor_tensor(sm[:, 1:2], sm[:, 1:2], lamt[:, l, 1:2], ALU.mult),
                                 reads=[Bg], upd=[bsm])
                            o1, bo1 = o1r.next()
                            c.op(dve, lambda e: e.tensor_scalar(o1[:], a1[:, 0:128], sm[:, 0:1], None, ALU.mult),
                                 reads=[bacc, bsm], writes=[bo1])
                            o2, bo2 = o2r.next()
                            c.op(dve, lambda e: e.scalar_tensor_tensor(o2[:], a2[:, 0:128], sm[:, 1:2], o1[:],
                                                                       ALU.mult, ALU.add),
                                 reads=[bacc, bsm, bo1], writes=[bo2])
                            c.op(dve, lambda e: e.memset(sm[:, 2:3], 0.0), upd=[bsm])
                            jj, bjj = jr.next()
                            c.op(act, lambda e: e.activation(jj[:], o2[:], AF.Square, accum_out=sm[:, 2:3]),
                                 reads=[bo2], writes=[bjj], upd=[bsm])
                            c.op(dve, lambda e: e.tensor_scalar(sm[:, 3:4], sm[:, 2:3], 1.0 / 128, EPS, ALU.mult, ALU.add),
                                 upd=[bsm])
                            c.op(act, lambda e: e.activation(sm[:, 4:5], sm[:, 3:4], AF.Sqrt), upd=[bsm])
                            c.op(dve, lambda e: e.reciprocal(sm[:, 4:5], sm[:, 4:5]), upd=[bsm])
                            c.op(dve, lambda e: e.tensor_scalar(sm[:, 4:5], sm[:, 4:5], 1.0 - lam_init, None, ALU.mult),
                                 upd=[bsm])
                            ob, bob = obr.next()
                            c.op(dve, lambda e: e.scalar_tensor_tensor(ob[:], o2[:], sm[:, 4:5], sg[:], ALU.mult, ALU.mult),
                                 reads=[bo2, bsm, bsg], writes=[bob])
                            po, bpo = por.next()
                            c.op(pe, lambda e: e.transpose(po[:], ob[:], ident_b[:]), reads=[bob, Bg], writes=[bpo])
                            c.op(act, lambda e: e.activation(os_[:, j * 128:(j + 1) * 128], po[:], AF.Copy), reads=[bpo],
                                 **({"writes": [bos]} if j == 0 else {"adds": [bos]}))
                        c.dma(pool, OT[512 + h * 128:512 + (h + 1) * 128, q0:q0 + 512], os_[:], reads=[bos])
                c.barrier()
            c.stack = gstack

        def bcast_load(dst, src_vec_ap, bdst):
            c.dma(sp, dst, src_vec_ap.partition_broadcast(128), writes=[bdst])

        def phaseM(l, xin, xmid):
            with ExitStack() as ps_:
                c.stack = ps_
                st = {"wst": Ring(c, "m_wst", 2, [128, 4, 1024], F32)}
                wa = c.sb("m_wa", [128, 2, 1024], BF16)
                wbb = c.sb("m_wb", [128, 2, 1024], BF16)
                wc = c.sb("m_wc", [128, 4, 1024], BF16)
                wo = c.sb("m_wo", [128, 8, 1024], BF16)
                bwa, bwb_, bwc, bwo = Buf(), Buf(), Buf(), Buf()
                load_w(st, wa, bwa, w_br_a[l].rearrange("(kc p) n -> p kc n", p=128), 2, 1024)
                load_w(st, wbb, bwb_, w_br_b[l].rearrange("(kc p) n -> p kc n", p=128), 2, 1024)
                load_w(st, wc, bwc, w_br_c[l].rearrange("(kc p) n -> p kc n", p=128), 4, 1024)
                load_w(st, wo, bwo, w_out[l].rearrange("(kc p) n -> p kc n", p=128), 8, 1024)
                gp = c.sb("m_gp", [128, D], F32)
                bgp = Buf()
                bcast_load(gp[:], norms[l, 8:16, :].rearrange("a b -> (a b)"), bgp)
                otr = Ring(c, "m_ot", 2, [128, 8, 512], BF16)
                gtr = Ring(c, "m_gt", 2, [128, 24, 512], BF16)
                yT = c.sb("m_yT", [128, 8, 512], BF16)
                byT = Buf()
                pbr = Ring(c, "m_pb", 4, [128, 512], F32, psum=True)
                phr = Ring(c, "m_ph", 2, [128, 1024], F32, psum=True)
                t1r = Ring(c, "m_t1", 2, [128, 512], F32)
                t2r = Ring(c, "m_t2", 2, [128, 512], F32)
                xr = Ring(c, "m_x", 2, [128, D], F32)
                hr = Ring(c, "m_h", 2, [128, D], F32)
                jr = Ring(c, "m_j", 1, [128, D], BF16)
                ssr = Ring(c, "m_ss", 2, [128, 4], F32)
                OTv = OT.rearrange("(kc p) t -> p kc t", p=128)
                GTv = GT.rearrange("(kc p) t -> p kc t", p=128)
                for tg in range(S // 512):
                    t0 = tg * 512
                    ot, bot = otr.next()
                    c.dma(sp, ot[:], OTv[:, :, t0:t0 + 512], writes=[bot])
                    gt, bgt = gtr.next()
                    c.dma(sp, gt[:], GTv[:, :, t0:t0 + 512], writes=[bgt])
                    for n in range(8):
                        pa, bpa = pbr.next()
                        for kc in range(2):
                            c.op(pe, lambda e: e.matmul(pa[:], wa[:, kc, n * 128:(n + 1) * 128], ot[:, kc, :],
                                                        start=(kc == 0), stop=(kc == 1)),
                                 reads=[bwa, bot], **({"writes": [bpa]} if kc == 0 else {"adds": [bpa]}))
                        pb, bpb = pbr.next()
                        for kc in range(2):
                            c.op(pe, lambda e: e.matmul(pb[:], wbb[:, kc, n * 128:(n + 1) * 128], ot[:, 2 + kc, :],
                                                        start=(kc == 0), stop=(kc == 1)),
                                 reads=[bwb_, bot], **({"writes": [bpb]} if kc == 0 else {"adds": [bpb]}))
                        pc, bpc = pbr.next()
                        for kc in range(4):
                            c.op(pe, lambda e: e.matmul(pc[:], wc[:, kc, n * 128:(n + 1) * 128], ot[:, 4 + kc, :],
                                                        start=(kc == 0), stop=(kc == 3)),
                                 reads=[bwc, bot], **({"writes": [bpc]} if kc == 0 else {"adds": [bpc]}))
                        t1, bt1 = t1r.next()
                        t2, bt2 = t2r.next()
                        c.op(dve, lambda e: e.tensor_tensor(t1[:], pa[:], gt[:, n, :], ALU.mult), reads=[bpa, bgt], writes=[bt1])
                        c.op(dve, lambda e: e.tensor_tensor(t2[:], pb[:], gt[:, 8 + n, :], ALU.mult), reads=[bpb, bgt], writes=[bt2])
                        c.op(pl, lambda e: e.tensor_tensor(t1[:], t1[:], t2[:], ALU.add), reads=[bt2], upd=[bt1])
                        c.op(dve, lambda e: e.tensor_tensor(t2[:], pc[:], gt[:, 16 + n, :], ALU.mult), reads=[bpc, bgt], upd=[bt2])
                        c.op(pl, lambda e: e.tensor_tensor(yT[:, n, :], t1[:], t2[:], ALU.add), reads=[bt1, bt2],
                             **({"writes": [byT]} if n == 0 else {"adds": [byT]}))
                    for j in range(4):
                        tj = t0 + j * 128
                        ph, bph = phr.next()
                        for half in range(2):
                            for kc in range(8):
                                c.op(pe, lambda e: e.matmul(ph[:, half * 512:(half + 1) * 512], yT[:, kc, j * 128:(j + 1) * 128],
                                                            wo[:, kc, half * 512:(half + 1) * 512],
                                                            start=(kc == 0), stop=(kc == 7)),
                                     reads=[byT, bwo], **({"writes": [bph]} if (kc == 0 and half == 0) else {"adds": [bph]}))
                        xt, bx = xr.next()
                        c.dma(sp, xt[:], xin[tj:tj + 128, :], writes=[bx])
                        ss, bs = ssr.next()
                        c.op(dve, lambda e: e.memset(ss[:], 0.0), writes=[bs])
                        jk, bj = jr.next()
                        c.op(act, lambda e: e.activation(jk[:], ph[:], AF.Square, accum_out=ss[:, 0:1]),
                             reads=[bph], writes=[bj], upd=[bs])
                        c.op(dve, lambda e: e.tensor_scalar(ss[:, 1:2], ss[:, 0:1], 1.0 / D, EPS, ALU.mult, ALU.add), upd=[bs])
                        c.op(act, lambda e: e.activation(ss[:, 2:3], ss[:, 1:2], AF.Sqrt), upd=[bs])
                        c.op(dve, lambda e: e.reciprocal(ss[:, 2:3], ss[:, 2:3]), upd=[bs])
                        hh, bh = hr.next()
                        c.op(dve, lambda e: e.scalar_tensor_tensor(hh[:], ph[:], ss[:, 2:3], gp[:], ALU.mult, ALU.mult),
                             reads=[bph, bs, bgp], writes=[bh])
                        c.op(pl, lambda e: e.tensor_tensor(hh[:], hh[:], xt[:], ALU.add), reads=[bx], upd=[bh])
                        c.dma(pool, xmid[tj:tj + 128, :], hh[:], reads=[bh])
                c.barrier()
            c.stack = gstack

        def phaseF(l, xmid, xout):
            TF = 512
            GC = 0.7978845608028654
            with ExitStack() as ps_:
                c.stack = ps_
                st = {
                    "xr": Ring(c, "f_xr", 2, [128, D], F32),
                    "ss": Ring(c, "f_ss", 2, [128, 4], F32),
                    "junk": Ring(c, "f_junk", 1, [128, D], BF16),
                    "xb": Ring(c, "f_xb", 2, [128, D], BF16),
                    "pT": Ring(c, "f_pT", 2, [128, 8, 128], BF16, psum=True),
                    "wst": Ring(c, "f_wst", 2, [128, 8, 512], F32),
                }
                wd = c.sb("f_wd", [128, 22, 1024], BF16)
                bwd = Buf()
                wdv = w_down[l].rearrange("(kc p) n -> p kc n", p=128)
                firstw = True
                for k0 in range(0, 22, 8):
                    kn = min(8, 22 - k0)
                    for half in range(2):
                        ws, bws = st["wst"].next()
                        c.dma(sp, ws[:, 0:kn, :], wdv[:, k0:k0 + kn, half * 512:(half + 1) * 512], writes=[bws])
                        for kc in range(kn):
                            c.op(act, lambda e: e.activation(wd[:, k0 + kc, half * 512:(half + 1) * 512], ws[:, kc, :], AF.Copy),
                                 reads=[bws], **({"writes": [bwd]} if firstw else {"adds": [bwd]}))
                            firstw = False
                gp = c.sb("f_gp", [128, D], F32)
                bgp = Buf()
                bcast_load(gp[:], norms[l, 24:32, :].rearrange("a b -> (a b)"), bgp)
                gain = gcols[:, l, 16:24]
                wur = Ring(c, "f_wu", 3, [128, 8, 256], BF16)
                xnT = c.sb("f_xnT", [128, 8, TF], BF16)
                bxn = [Buf() for _ in range(TF // 128)]
                halo = c.sb("f_halo", [128, 44, 2], F32)
                bhalo = [Buf() for _ in range(44)]
                c.op(dve, lambda e: e.memset(halo[:], 0.0), writes=bhalo)
                aT = c.sb("f_aT", [128, 22, TF], BF16)
                baT = Buf()
                pur = Ring(c, "f_pu", 4, [128, 512], F32, psum=True)
                phr = Ring(c, "f_ph", 1, [128, 1024], F32, psum=True)
                cgr = Ring(c, "f_cg", 2, [128, 512], F32)
                cur = Ring(c, "f_cu", 2, [128, 512], F32)
                t1r = Ring(c, "f_t1", 2, [128, 512], F32)
                t2r = Ring(c, "f_t2", 2, [128, 512], F32)
                hr = Ring(c, "f_h", 2, [128, D], F32)
                jr = Ring(c, "f_j", 1, [128, D], BF16)
                ssr = Ring(c, "f_ss2", 2, [128, 4], F32)
                wuv = w_up[l].rearrange("(kc p) n -> p kc n", p=128)
                bWU = Buf()
                wcr = Ring(c, "f_wc", 2, [128, 8, 512], BF16)
                for half in range(2):
                    for c4 in range(0, 22, 4):
                        ncp = min(4, 22 - c4)
                        nco = ncp * 128
                        col0 = half * DFF + c4 * 128
                        ws, bws = st["wst"].next()
                        c.dma(sp, ws[:, :, 0:nco], wuv[:, :, col0:col0 + nco], writes=[bws])
                        wcb, bwc = wcr.next()
                        for kc in range(8):
                            c.op(act, lambda e: e.activation(wcb[:, kc, 0:nco], ws[:, kc, 0:nco], AF.Identity,
                                                             scale=gain[:, kc:kc + 1]),
                                 reads=[bws, Bg], **({"writes": [bwc]} if kc == 0 else {"adds": [bwc]}))
                        for ci in range(ncp):
                            c.dma(pool, WU[c4 + ci, :, :, half * 128:(half + 1) * 128], wcb[:, :, ci * 128:(ci + 1) * 128],
                                  reads=[bwc], adds=[bWU])

                def conv(pu, bpu, ch, dst, bdst):
                    w0 = cw[:, l, ch:ch + 1]
                    w1 = cw[:, l, 44 + ch:44 + ch + 1]
                    w2 = cw[:, l, 88 + ch:88 + ch + 1]
                    c.op(act, lambda e: e.activation(dst[:], pu[:], AF.Identity, bias=cb[:, l, ch:ch + 1], scale=w2),
                         reads=[bpu, Bg], writes=[bdst])
                    c.op(dve, lambda e: e.scalar_tensor_tensor(dst[:, 1:TF], pu[:, 0:TF - 1], w1, dst[:, 1:TF], ALU.mult, ALU.add),
                         reads=[bpu, Bg], upd=[bdst])
                    c.op(dve, lambda e: e.scalar_tensor_tensor(dst[:, 2:TF], pu[:, 0:TF - 2], w0, dst[:, 2:TF], ALU.mult, ALU.add),
                         reads=[bpu, Bg], upd=[bdst])
                    c.op(dve, lambda e: e.scalar_tensor_tensor(dst[:, 0:1], halo[:, ch, 1:2], w1, dst[:, 0:1], ALU.mult, ALU.add),
                         reads=[bhalo[ch], Bg], upd=[bdst])
                    c.op(dve, lambda e: e.scalar_tensor_tensor(dst[:, 0:2], halo[:, ch, 0:2], w0, dst[:, 0:2], ALU.mult, ALU.add),
                         reads=[bhalo[ch], Bg], upd=[bdst])
                    c.op(dve, lambda e: e.tensor_copy(halo[:, ch, :], pu[:, TF - 2:TF]), reads=[bpu], writes=[bhalo[ch]])

                for tg in range(S // TF):
                    t0 = tg * TF
                    for tt in range(TF // 128):
                        norm_transpose(st, xmid[t0 + tt * 128:t0 + (tt + 1) * 128, :],
                                       xnT[:, :, tt * 128:(tt + 1) * 128], bxn[tt], True)
                    for cp in range(22):
                        wu, bwu = wur.next()
                        c.dma(sp, wu[:], WU[cp], reads=[bWU], writes=[bwu])
                        pg, bpg = pur.next()
                        pu, bpu = pur.next()
                        for (pp_, bpp, off) in ((pg, bpg, 0), (pu, bpu, 128)):
                            for kc in range(8):
                                c.op(pe, lambda e: e.matmul(pp_[:], wu[:, kc, off:off + 128], xnT[:, kc, :],
                                                            start=(kc == 0), stop=(kc == 7)),
                                     reads=[bwu] + bxn, **({"writes": [bpp]} if kc == 0 else {"adds": [bpp]}))
                        cg, bcg = cgr.next()
                        cu, bcu = cur.next()
                        conv(pg, bpg, cp, cg, bcg)
                        conv(pu, bpu, 22 + cp, cu, bcu)
                        t1, bt1 = t1r.next()
                        t2, bt2 = t2r.next()
                        c.op(act, lambda e: e.activation(t1[:], cg[:], AF.Square), reads=[bcg], writes=[bt1])
                        c.op(dve, lambda e: e.tensor_scalar(t1[:], t1[:], 0.044715, 1.0, ALU.mult, ALU.add), upd=[bt1])
                        c.op(pl, lambda e: e.tensor_tensor(t1[:], t1[:], cg[:], ALU.mult), reads=[bcg], upd=[bt1])
                        c.op(act, lambda e: e.activation(t2[:], t1[:], AF.Sigmoid, scale=2.0 * GC), reads=[bt1], writes=[bt2])
                        c.op(pl, lambda e: e.tensor_tensor(t2[:], t2[:], cg[:], ALU.mult), reads=[bcg], upd=[bt2])
                        c.op(dve, lambda e: e.tensor_tensor(aT[:, cp, :], t2[:], cu[:], ALU.mult), reads=[bt2, bcu],
                             **({"writes": [baT]} if cp == 0 else {"adds": [baT]}))
                    for j in range(TF // 128):
                        tj = t0 + j * 128
                        ph, bph = phr.next()
                        for half in range(2):
                            for kc in range(22):
                                c.op(pe, lambda e: e.matmul(ph[:, half * 512:(half + 1) * 512], aT[:, kc, j * 128:(j + 1) * 128],
                                                            wd[:, kc, half * 512:(half + 1) * 512],
                                                            start=(kc == 0), stop=(kc == 21)),
                                     reads=[baT, bwd], **({"writes": [bph]} if (kc == 0 and half == 0) else {"adds": [bph]}))
                        ss, bs = ssr.next()
                        c.op(dve, lambda e: e.memset(ss[:], 0.0), writes=[bs])
                        jk, bj = jr.next()
                        c.op(act, lambda e: e.activation(jk[:], ph[:], AF.Square, accum_out=ss[:, 0:1]),
                             reads=[bph], writes=[bj], upd=[bs])
                        c.op(dve, lambda e: e.tensor_scalar(ss[:, 1:2], ss[:, 0:1], 1.0 / D, EPS, ALU.mult, ALU.add), upd=[bs])
                        c.op(act, lambda e: e.activation(ss[:, 2:3], ss[:, 1:2], AF.Sqrt), upd=[bs])
                        c.op(dve, lambda e: e.reciprocal(ss[:, 2:3], ss[:, 2:3]), upd=[bs])
                        hh, bh = hr.next()
                        c.op(dve, lambda e: e.scalar_tensor_tensor(hh[:], ph[:], ss[:, 2:3], gp[:], ALU.mult, ALU.mult),
                             reads=[bph, bs, bgp], writes=[bh])
                        xt, bx = st["xr"].next()
                        c.dma(sp, xt[:], xmid[tj:tj + 128, :], writes=[bx])
                        c.op(pl, lambda e: e.tensor_tensor(hh[:], hh[:], xt[:], ALU.add), reads=[bx], upd=[bh])
                        c.dma(pool, xout[tj:tj + 128, :], hh[:], reads=[bh])
                c.barrier()
            c.stack = gstack

        cur = x_in
        for l in range(depth):
            last = (l == depth - 1)
            if "1" in phases:
                phase1(l, cur)
            if "A" in phases:
                phaseA(l)
            if "b" in phases:
                phaseB1(l)
            if "B" in phases:
                phaseB2(l)
            if "C" in phases:
                phaseC(l)
            if "M" in phases:
                phaseM(l, cur, X1)
            if "F" in phases:
                phaseF(l, X1, y_out if last else X2)
            cur = X2
        c.barrier()
    return nc


def make_in_maps(inputs, S):
    cst = make_consts()
    f = lambda a: np.ascontiguousarray(np.asarray(a, dtype=np.float32))
    shared = {
        "w_in": f(inputs["w_in"]), "w_br_a": f(inputs["w_br_a"]), "w_br_b": f(inputs["w_br_b"]),
        "w_br_c": f(inputs["w_br_c"]), "w_out": f(inputs["w_out"]),
        "lam": np.ascontiguousarray(np.stack([f(inputs["lam_q1"]), f(inputs["lam_k1"]), f(inputs["lam_q2"]),
                                              f(inputs["lam_k2"])], axis=1)),
        "subln_g": f(inputs["subln_g"]),
        "norms": np.ascontiguousarray(np.stack([f(inputs["norm_mix_pre"]), f(inputs["norm_mix_post"]),
                                                f(inputs["norm_ffn_pre"]), f(inputs["norm_ffn_post"])],
                                               axis=1).reshape(DEPTH, 32, 128)),
        "w_up": f(inputs["w_ffn_up"]),
        "conv_w": np.ascontiguousarray(f(inputs["conv_w"]).reshape(DEPTH, 132, 128)),
        "conv_b": np.ascontiguousarray(f(inputs["conv_b"]).reshape(DEPTH, 44, 128)),
        "w_down": f(inputs["w_ffn_down"]),
        "cst": cst,
    }
    x = f(inputs["x"])
    pos = np.ascontiguousarray(np.asarray(inputs["positions"], dtype=np.int32))
    maps = []
    for b in range(x.shape[0]):
        m = dict(shared)
        m["x"] = np.ascontiguousarray(x[b])
        m["pos"] = np.ascontiguousarray(pos[b].reshape(S // 128, 128))
        maps.append(m)
    return maps


def kernel(**inputs):
    x = np.asarray(inputs["x"])
    B, S, _ = x.shape
    nc = build(S)
    maps = make_in_maps(inputs, S)
    res = run_bass_kernel_spmd(nc, maps, core_ids=list(range(B)))
    out = np.stack([np.asarray(r["y"], dtype=np.float32) for r in res.results], axis=0)
    return out
```
